# Optimizing a Trainium2 kernel written in Bass

```python
import math
import jax
import jax.numpy as jnp
from jax import lax
import numpy as np

D_MODEL = 4096
BATCH = 2
SEQ = 8192
DEPTH = 2

GRID_W = 64
CTX_LEN = 256

CONV_W = D_MODEL // 4
CONV_K = 3
ATT_HEAD_DIM = 128
ATT_V_DIM = 2 * ATT_HEAD_DIM
ATT_W = D_MODEL // 2
ATT_HEADS = ATT_W // ATT_V_DIM
CFM_W = D_MODEL - CONV_W - ATT_W
CFM_K = 31
MIX_W = CONV_W + ATT_W + CFM_W

QK_W = ATT_HEADS * 2 * ATT_HEAD_DIM
OFF_Q = 3 * CONV_W
OFF_K = OFF_Q + QK_W
OFF_V = OFF_K + QK_W
OFF_C = OFF_V + ATT_W
PROJ_W = OFF_C + 2 * CFM_W

MOE_GROUPS = 4
EXPERTS_PER_GROUP = 8
MOE_TOP_K = 2
EXPERT_FF = D_MODEL // 8

Q_BLOCK = 128
ROPE_BASE = 10000.0
NORM_EPS = 1e-6

kernel_name = 'hybrid_dit_shortconv_diffattn_conformer_hmoe'


def rmsnorm(x, g):
    xf = x.astype(jnp.float32)
    y = xf * lax.rsqrt(jnp.mean(xf * xf, axis=-1, keepdims=True) + NORM_EPS)
    return (y * g.astype(jnp.float32)).astype(x.dtype)


def layernorm(x, g, b):
    xf = x.astype(jnp.float32)
    mu = jnp.mean(xf, axis=-1, keepdims=True)
    var = jnp.mean(jnp.square(xf - mu), axis=-1, keepdims=True)
    y = (xf - mu) * lax.rsqrt(var + NORM_EPS)
    return (y * g.astype(jnp.float32) + b.astype(jnp.float32)).astype(x.dtype)


def modulate(u, shift, scale):
    return u * (1 + scale) + shift


def axial_rope_tables(n):
    rows = n // GRID_W
    row = jnp.repeat(jnp.arange(rows), GRID_W).astype(jnp.float32)
    col = jnp.tile(jnp.arange(GRID_W), rows).astype(jnp.float32)
    axis_dim = ATT_HEAD_DIM // 2
    inv = ROPE_BASE ** (-jnp.arange(0, axis_dim, 2, dtype=jnp.float32) / axis_dim)
    ang_r = row[:, None] * inv[None, :]
    ang_c = col[:, None] * inv[None, :]
    ang = jnp.concatenate([ang_r, ang_r, ang_c, ang_c], axis=-1)
    return jnp.cos(ang), jnp.sin(ang)


def apply_rope(x, cos, sin):
    r1, r2, c1, c2 = jnp.split(x, 4, axis=-1)
    rot = jnp.concatenate([-r2, r1, -c2, c1], axis=-1)
    cb = cos[:, None, None, :]
    sb = sin[:, None, None, :]
    return (x * cb + rot * sb).astype(x.dtype)


def depthwise_conv(x, w):
    k = w.shape[0]
    return lax.conv_general_dilated(
        x, w[:, None, :].astype(x.dtype), window_strides=(1,),
        padding=[(k // 2, k // 2)], dimension_numbers=('NWC', 'WIO', 'NWC'),
        feature_group_count=x.shape[-1])


def split_kv(pkv):
    b, l = pkv.shape[:2]
    k = pkv[..., :QK_W].reshape(b, l, ATT_HEADS, 2, ATT_HEAD_DIM)
    v = pkv[..., QK_W:].reshape(b, l, ATT_HEADS, ATT_V_DIM)
    return k, v


def split_q(p):
    b, l = p.shape[:2]
    return p[..., OFF_Q:OFF_K].reshape(b, l, ATT_HEADS, 2, ATT_HEAD_DIM)


def diff_lambda(lam_qk, lam_init):
    lf = lam_qk.astype(jnp.float32)
    return jnp.exp(jnp.sum(lf[0] * lf[1])) - jnp.exp(jnp.sum(lf[2] * lf[3])) + lam_init


def diff_attend(q, k, v, lam):
    s = jnp.einsum('bqhmd,bkhmd->bhmqk', q, k).astype(jnp.float32) * (ATT_HEAD_DIM ** -0.5)
    p = jax.nn.softmax(s, axis=-1)
    a = p[:, :, 0] - lam * p[:, :, 1]
    return jnp.einsum('bhqk,bkhe->bqhe', a.astype(v.dtype), v)


def blocked_diff_attention(q, k_all, v_all, lam):
    b, n = q.shape[:2]
    qb = q.reshape(b, n // Q_BLOCK, Q_BLOCK, ATT_HEADS, 2, ATT_HEAD_DIM).swapaxes(0, 1)
    out = lax.map(lambda qq: diff_attend(qq, k_all, v_all, lam), qb)
    return out.swapaxes(0, 1).reshape(b, n, ATT_HEADS, ATT_V_DIM)


def mixer_out(p, att, lam_init, subln_g, w_sc, w_cf, b_cf, ln_g, ln_b, w_o):
    xa = p[..., :CONV_W]
    gb = p[..., CONV_W:2 * CONV_W]
    gc = p[..., 2 * CONV_W:OFF_Q]
    y_short = gb * depthwise_conv(gc * xa, w_sc)
    b, l = att.shape[:2]
    y_att = (rmsnorm(att, subln_g) * (1 - lam_init)).reshape(b, l, ATT_W)
    ga = p[..., OFF_C:OFF_C + CFM_W]
    gg = p[..., OFF_C + CFM_W:]
    z = depthwise_conv(ga * jax.nn.sigmoid(gg), w_cf) + b_cf
    y_cfm = jax.nn.silu(layernorm(z, ln_g, ln_b))
    return jnp.concatenate([y_short, y_att, y_cfm], axis=-1) @ w_o


def hier_moe(t, w_rg, b_rg, w_re, b_re, w_gate, w_up, w_down):
    n_tok = t.shape[0]
    pg = jax.nn.softmax((t @ w_rg).astype(jnp.float32) + b_rg.astype(jnp.float32), axis=-1)
    g_star = jnp.argmax(pg, axis=-1)
    pg_star = jnp.max(pg, axis=-1)
    le = ((t @ w_re).astype(jnp.float32) + b_re.astype(jnp.float32)).reshape(n_tok, MOE_GROUPS, EXPERTS_PER_GROUP)
    le_sel = jnp.take_along_axis(le, g_star[:, None, None], axis=1)[:, 0]
    pe = jax.nn.softmax(le_sel, axis=-1)
    top_v, top_i = lax.top_k(pe, MOE_TOP_K)
    top_w = top_v / jnp.sum(top_v, axis=-1, keepdims=True) * pg_star[:, None]
    within = jnp.sum(jax.nn.one_hot(top_i, EXPERTS_PER_GROUP, dtype=jnp.float32) * top_w[..., None], axis=1)
    gates = (jax.nn.one_hot(g_star, MOE_GROUPS, dtype=jnp.float32)[:, :, None] * within[:, None, :]).astype(t.dtype)
    out = jnp.zeros_like(t)
    for gi in range(MOE_GROUPS):
        hid = jax.nn.silu(jnp.einsum('td,edf->tef', t, w_gate[gi])) * jnp.einsum('td,edf->tef', t, w_up[gi])
        out = out + jnp.einsum('tef,efd->td', hid * gates[:, gi, :, None], w_down[gi])
    return out


def setup_inputs(seed: int = 0) -> dict:
    key = jax.random.key(seed)
    ks = jax.random.split(key, 25)
    f32 = jnp.float32

    def nrm(k, shape, s):
        return jax.random.normal(k, shape, f32) * s

    n_exp = MOE_GROUPS * EXPERTS_PER_GROUP
    return {
        'x': nrm(ks[0], (BATCH, SEQ, D_MODEL), 1.0),
        'c': nrm(ks[1], (BATCH, D_MODEL), 1.0),
        'ctx': nrm(ks[2], (BATCH, CTX_LEN, D_MODEL), 1.0),
        'c_ctx': nrm(ks[3], (D_MODEL,), 1.0),
        'w_ada': nrm(ks[4], (DEPTH, D_MODEL, 6 * D_MODEL), 0.5 * D_MODEL ** -0.5),
        'b_ada': nrm(ks[5], (DEPTH, 6 * D_MODEL), 0.02),
        'g_mix': 1.0 + nrm(ks[6], (DEPTH, D_MODEL), 0.02),
        'g_ffn': 1.0 + nrm(ks[7], (DEPTH, D_MODEL), 0.02),
        'w_in': nrm(ks[8], (DEPTH, D_MODEL, PROJ_W), D_MODEL ** -0.5),
        'w_out': nrm(ks[9], (DEPTH, MIX_W, D_MODEL), MIX_W ** -0.5),
        'short_conv_w': nrm(ks[10], (DEPTH, CONV_K, CONV_W), CONV_K ** -0.5),
        'cfm_conv_w': nrm(ks[11], (DEPTH, CFM_K, CFM_W), CFM_K ** -0.5),
        'cfm_conv_b': nrm(ks[12], (DEPTH, CFM_W), 0.02),
        'cfm_ln_g': 1.0 + nrm(ks[13], (DEPTH, CFM_W), 0.02),
        'cfm_ln_b': nrm(ks[14], (DEPTH, CFM_W), 0.02),
        'lam_qk': nrm(ks[15], (DEPTH, 4, ATT_HEAD_DIM), 0.1),
        'subln_g': 1.0 + nrm(ks[16], (DEPTH, ATT_V_DIM), 0.02),
        'w_route_group': nrm(ks[17], (DEPTH, D_MODEL, MOE_GROUPS), D_MODEL ** -0.5),
        'b_route_group': nrm(ks[18], (DEPTH, MOE_GROUPS), 0.01),
        'w_route_expert': nrm(ks[19], (DEPTH, D_MODEL, n_exp), D_MODEL ** -0.5),
        'b_route_expert': nrm(ks[20], (DEPTH, n_exp), 0.01),
        'w_gate': nrm(ks[21], (DEPTH, MOE_GROUPS, EXPERTS_PER_GROUP, D_MODEL, EXPERT_FF), D_MODEL ** -0.5),
        'w_up': nrm(ks[22], (DEPTH, MOE_GROUPS, EXPERTS_PER_GROUP, D_MODEL, EXPERT_FF), D_MODEL ** -0.5),
        'w_down': nrm(ks[23], (DEPTH, MOE_GROUPS, EXPERTS_PER_GROUP, EXPERT_FF, D_MODEL), EXPERT_FF ** -0.5),
        'g_final': 1.0 + nrm(ks[24], (D_MODEL,), 0.02),
    }


def reference(x, c, ctx, c_ctx, w_ada, b_ada, g_mix, g_ffn, w_in, w_out, short_conv_w, cfm_conv_w,
              cfm_conv_b, cfm_ln_g, cfm_ln_b, lam_qk, subln_g, w_route_group, b_route_group,
              w_route_expert, b_route_expert, w_gate, w_up, w_down, g_final):
    bsz, n, d = x.shape
    cos, sin = axial_rope_tables(n)
    silu_c = jax.nn.silu(c)
    silu_cc = jax.nn.silu(c_ctx)
    h, hc = x, ctx
    for l in range(DEPTH):
        last = l == DEPTH - 1
        lam_init = 0.8 - 0.6 * math.exp(-0.3 * l)
        lam = diff_lambda(lam_qk[l], lam_init)
        mod = silu_c @ w_ada[l] + b_ada[l]
        sh1, sc1, gt1, sh2, sc2, gt2 = jnp.split(mod[:, None, :], 6, axis=-1)
        mod_c = silu_cc @ w_ada[l] + b_ada[l]
        csh1, csc1, cgt1, csh2, csc2, cgt2 = jnp.split(mod_c, 6, axis=-1)

        u = modulate(rmsnorm(h, g_mix[l]), sh1, sc1)
        uc = modulate(rmsnorm(hc, g_mix[l]), csh1, csc1)
        p = u @ w_in[l]
        q = apply_rope(split_q(p), cos, sin)
        k, v = split_kv(p[..., OFF_K:OFF_C])
        k = apply_rope(k, cos, sin)
        if last:
            kc, vc = split_kv(uc @ w_in[l][:, OFF_K:OFF_C])
        else:
            pc = uc @ w_in[l]
            kc, vc = split_kv(pc[..., OFF_K:OFF_C])
        k_all = jnp.concatenate([k, kc], axis=1)
        v_all = jnp.concatenate([v, vc], axis=1)
        att = blocked_diff_attention(q, k_all, v_all, lam)
        mix = mixer_out(p, att, lam_init, subln_g[l], short_conv_w[l], cfm_conv_w[l], cfm_conv_b[l],
                        cfm_ln_g[l], cfm_ln_b[l], w_out[l])
        h = h + gt1 * mix
        if not last:
            att_c = diff_attend(split_q(pc), kc, vc, lam)
            mix_c = mixer_out(pc, att_c, lam_init, subln_g[l], short_conv_w[l], cfm_conv_w[l], cfm_conv_b[l],
                              cfm_ln_g[l], cfm_ln_b[l], w_out[l])
            hc = hc + cgt1 * mix_c

        f = modulate(rmsnorm(h, g_ffn[l]), sh2, sc2).reshape(-1, d)
        moe_args = (w_route_group[l], b_route_group[l], w_route_expert[l], b_route_expert[l],
                    w_gate[l], w_up[l], w_down[l])
        if last:
            h = h + gt2 * hier_moe(f, *moe_args).reshape(bsz, n, d)
        else:
            fc = modulate(rmsnorm(hc, g_ffn[l]), csh2, csc2).reshape(-1, d)
            ff = hier_moe(jnp.concatenate([f, fc], axis=0), *moe_args)
            h = h + gt2 * ff[:bsz * n].reshape(bsz, n, d)
            hc = hc + cgt2 * ff[bsz * n:].reshape(bsz, -1, d)
    return rmsnorm(h, g_final)
```

```python
import math
from contextlib import ExitStack
import numpy as np
import concourse.bass as bass
import concourse.mybir as mybir
from concourse.bass_utils import run_bass_kernel_spmd

F32 = mybir.dt.float32
BF16 = mybir.dt.bfloat16
ALU = mybir.AluOpType
AF = mybir.ActivationFunctionType
EPS = 1e-6
ENGS = ('pe', 'act', 'dve', 'pool', 'sp')


class Cfg:
    def __init__(self, D, SEQ, CTX=256, L=2):
        self.D, self.SEQ, self.CTX, self.L = D, SEQ, CTX, L
        self.NC = 8
        self.KC = D // 128
        self.NTh = SEQ // 8
        self.NT = 2 * self.NTh
        self.NTOT = self.NT + 2 * CTX
        self.CONV_W = D // 4
        self.CG = self.CONV_W // 128
        self.ATT_W = D // 2
        self.H = self.ATT_W // 256
        self.H2 = 2 * self.H
        self.CFM_W = D - self.CONV_W - self.ATT_W
        self.QK_W = self.H * 256
        self.OFF_Q = 3 * self.CONV_W
        self.OFF_K = self.OFF_Q + self.QK_W
        self.OFF_V = self.OFF_K + self.QK_W
        self.OFF_C = self.OFF_V + self.ATT_W
        self.PROJ_W = self.OFF_C + 2 * self.CFM_W
        self.FF = D // 8
        self.FC = self.FF // 128
        self.NE = 32
        self.NS = 6 * D // 8
        self.NSC = self.NS // 128
        self.TB = min(512, self.NTh)
        self.PW = 256
        self.GRID_W = 64
        self.KVN = self.H2 * 128 * self.NT + self.NT * self.ATT_W


class StopBuild(Exception):
    pass


class Prog:
    CH = 16000
    K = 8
    PU = 1000

    def __init__(self):
        self.ops = []
        self.lw = {}
        self.rd = {}
        self.last_on = {}
        self.pending_st = []
        self.dead = False

    def op(self, eng, fn, reads=(), writes=(), kind='c', extra=()):
        if self.dead:
            return None
        i = len(self.ops)
        deps = set(extra)
        for r in reads:
            w = self.lw.get(r)
            if w:
                deps.update(w)
        for r in writes:
            rr = self.rd.get(r)
            if rr and (rr[0] or rr[1]):
                deps.update(rr[0].values())
                deps.update(rr[1])
        for r in reads:
            rr = self.rd.get(r)
            if rr is None:
                rr = self.rd[r] = ({}, [])
            if kind == 'c':
                rr[0][eng] = i
            else:
                rr[1].append(i)
        for r in writes:
            rr = self.rd.get(r)
            if rr and (rr[0] or rr[1]):
                self.lw[r] = [i]
                self.rd[r] = ({}, [])
            else:
                self.lw.setdefault(r, []).append(i)
                if r in reads:
                    pass
        deps.discard(i)
        self.ops.append([eng, fn, kind, deps])
        self.last_on[(eng, kind)] = i
        return i

    def barrier(self, full=False):
        if self.dead:
            return
        if full:
            hasdep = set()
            for o in self.ops:
                hasdep.update(o[3])
            extra = {i for i, o in enumerate(self.ops) if o[2] in ('cc', 'd') and i not in hasdep}
            self.pending_st = list(set(self.pending_st) | extra)
        deps = {v for (eng, kind), v in self.last_on.items() if kind == 'c'} | set(self.pending_st)
        b = self.op('sp', None, extra=deps)
        for e in ('pe', 'act', 'dve', 'pool'):
            self.op(e, None, extra={b})
        self.pending_st = []

    def emit(self, nc, es):
        ops = self.ops
        n = len(ops)
        need = [False] * n
        for (eng, fn, kind, deps) in ops:
            for d in deps:
                de, _, dk, _ = ops[d]
                if dk == 'c' and kind == 'c' and de == 'pe' and eng == 'pe':
                    continue
                need[d] = True
        sig = [None] * n
        ccount = {e: 0 for e in ENGS}
        csems = {e: [] for e in ENGS}
        dcount = {'sp': 0, 'pool': 0}
        dsems = {'sp': [], 'pool': []}
        slot_prev = {}
        ncc = 0
        for i, (eng, fn, kind, deps) in enumerate(ops):
            if kind == 'c':
                if need[i]:
                    k = ccount[eng]
                    ccount[eng] += 1
                    ch = k // self.CH
                    while len(csems[eng]) <= ch:
                        csems[eng].append(es.enter_context(nc.semaphore("c_%s_%d" % (eng, len(csems[eng])))))
                    sig[i] = (csems[eng][ch], k % self.CH + 1, 1, ('c', eng, ch))
            elif kind == 'd':
                j = dcount[eng]
                dcount[eng] += 1
                s = j % self.K
                m = j // self.K
                pool = m // self.PU
                val = 16 * ((m % self.PU) + 1)
                while len(dsems[eng]) <= pool:
                    pi = len(dsems[eng])
                    dsems[eng].append([es.enter_context(nc.semaphore("d_%s_%d_%d" % (eng, pi, t)))
                                       for t in range(self.K)])
                sig[i] = (dsems[eng][pool][s], val, 16, ('d', eng, pool, s))
                prev = slot_prev.get((eng, pool, s))
                if prev is not None:
                    deps.add(prev)
                slot_prev[(eng, pool, s)] = i
            else:
                sem = es.enter_context(nc.semaphore("cc_%d" % ncc))
                ncc += 1
                sig[i] = (sem, 1, 1, ('cc', ncc))
        self.nsig = dict(ccount)
        self.ndma = dict(dcount)

        def run(engname, e):
            waited = {}
            cwait = {}
            for i, (eng, fn, kind, deps) in enumerate(ops):
                if eng != engname:
                    continue
                for d in sorted(deps):
                    sg = sig[d]
                    if sg is None:
                        continue
                    de, _, dk, _ = ops[d]
                    if dk == 'c' and kind == 'c' and de == 'pe' and eng == 'pe':
                        continue
                    sem, val, _, key = sg
                    if key[0] == 'c':
                        cw = cwait.get(key[1])
                        if cw is not None and (cw[0] > key[2] or (cw[0] == key[2] and cw[1] >= val)):
                            continue
                        cwait[key[1]] = (key[2], val)
                    else:
                        if waited.get(key, 0) >= val:
                            continue
                        waited[key] = val
                    e.wait_ge(sem, val)
                if fn is None:
                    if sig[i] is not None:
                        e.nop().then_inc(sig[i][0], sig[i][2])
                else:
                    ins = fn(e)
                    if sig[i] is not None:
                        if kind == 'cc':
                            ins.then_inc(sig[i][0])
                        else:
                            ins.then_inc(sig[i][0], sig[i][2])

        with nc.Block() as block:
            @block.tensor
            def _(e):
                run('pe', e)

            @block.vector
            def _(e):
                run('dve', e)

            @block.scalar
            def _(e):
                run('act', e)

            @block.gpsimd
            def _(e):
                run('pool', e)

            @block.sync
            def _(e):
                run('sp', e)


def MM(P, out, lhsT, rhs, start, stop, rd, wr):
    P.op('pe', lambda e: e.matmul(out, lhsT, rhs, start=start, stop=stop), rd, wr)


def TR(P, out, in_, ident, rd, wr):
    P.op('pe', lambda e: e.transpose(out, in_, ident), rd, wr)


def ACTV(P, out, in_, func, rd, wr, bias=None, scale=None, accum=None):
    kw = {}
    if bias is not None:
        kw['bias'] = bias
    if scale is not None:
        kw['scale'] = scale
    if accum is not None:
        kw['accum_out'] = accum
    P.op('act', lambda e: e.activation(out, in_, func, **kw), rd, wr)


def TS(P, eng, out, in0, s1, s2, op0, op1, rd, wr):
    if op1 is None:
        P.op(eng, lambda e: e.tensor_scalar(out, in0, s1, None, op0), rd, wr)
    else:
        P.op(eng, lambda e: e.tensor_scalar(out, in0, s1, s2, op0, op1), rd, wr)


def TT(P, eng, out, in0, in1, op, rd, wr):
    P.op(eng, lambda e: e.tensor_tensor(out, in0, in1, op), rd, wr)


def STT(P, out, in0, scalar, in1, op0, op1, rd, wr):
    P.op('dve', lambda e: e.scalar_tensor_tensor(out, in0, scalar, in1, op0, op1), rd, wr)


def RECIP(P, out, in_, rd, wr):
    ACTV(P, out, in_, AF.Ln, rd, wr)
    ACTV(P, out, out, AF.Exp, list(wr), wr, scale=-1.0)


def RSQ(P, out, rd, wr):
    ACTV(P, out, out, AF.Exp, rd, wr, scale=-0.5)


def RMAX(P, out, in_, rd, wr):
    P.op('dve', lambda e: e.reduce_max(out, in_, mybir.AxisListType.X), rd, wr)


def COPY(P, eng, out, in_, rd, wr):
    P.op(eng, lambda e: e.tensor_copy(out, in_), rd, wr)


def MEMSET(P, eng, ap, val, wr):
    P.op(eng, lambda e: e.memset(ap, val), (), wr)


def DMA(P, q, out, in_, rd, wr, store=False, **kw):
    i = P.op(q, lambda e: e.dma_start(out=out, in_=in_, **kw), rd, wr, kind='d')
    if store and i is not None:
        P.pending_st.append(i)
    return i


def flat(ap):
    nd = len(ap.shape)
    names = " ".join("a%d" % i for i in range(nd))
    return ap.rearrange("%s -> (%s)" % (names, names))


def build(c):
    nc = bass.Bass("TRN2", target_bir_lowering=False)
    P = Prog()
    D, KC, L, NT, NTh, NTOT, CTX = c.D, c.KC, c.L, c.NT, c.NTh, c.NTOT, c.CTX
    CG, H, H2, TB, PW, FC, NE = c.CG, c.H, c.H2, c.TB, c.PW, c.FC, c.NE
    NSC = c.NSC
    TBM = max(TB, CTX)

    def din(name, shape, dt=F32):
        return nc.dram_tensor(name, list(shape), dt, kind="ExternalInput")

    def dscr(name, shape, dt):
        return nc.dram_tensor(name, list(shape), dt, kind="Internal")

    x_sh = din("x_sh", [NT, D])
    ctx_in = din("ctx_in", [2 * CTX, D])
    cvec = din("cvec", [3, D])
    selLR = din("selLR", [128, 16])
    w_ada_sh = din("w_ada_sh", [L, D, c.NS])
    b_ada_sh = din("b_ada_sh", [L, c.NS])
    g_mix = din("g_mix", [L, D])
    g_ffn = din("g_ffn", [L, D])
    g_final = din("g_final", [D])
    w_in_sh = din("w_in_sh", [L, D // 8, c.PROJ_W])
    w_out_sh = din("w_out_sh", [L, D // 8, D])
    w_gate_sh = din("w_gate_sh", [L, 4 * D, c.FF])
    w_up_sh = din("w_up_sh", [L, 4 * D, c.FF])
    w_down_sh = din("w_down_sh", [L, 4 * c.FF, D])
    sc_w = din("sc_w", [L, 3, c.CONV_W])
    cf_w = din("cf_w", [L, 31, c.CFM_W])
    cf_b = din("cf_b", [L, c.CFM_W])
    ln_g = din("ln_g", [L, c.CFM_W])
    ln_b = din("ln_b", [L, c.CFM_W])
    lam_qk = din("lam_qk", [L, 4, 128])
    subln_g = din("subln_g", [L, 256])
    w_rg = din("w_rg", [L, D, 4])
    b_rg = din("b_rg", [L, 4])
    w_re = din("w_re", [L, D, 32])
    b_re = din("b_re", [L, 32])
    cosT_d = din("cosT", [128, NTh])
    sinT_d = din("sinT", [128, NTh])
    ident_d = din("ident", [128, 128])
    pm_d = din("pm", [128, 128])
    out_sh = nc.dram_tensor("out_sh", [NT, D], F32, kind="ExternalOutput")

    wspec = {'in': (w_in_sh, (D // 8) * c.PROJ_W), 'out': (w_out_sh, (D // 8) * D),
             'gate': (w_gate_sh, 4 * D * c.FF), 'up': (w_up_sh, 4 * D * c.FF), 'down': (w_down_sh, 4 * c.FF * D)}
    wl = {}
    wg = {}
    for nm, (src, nel) in wspec.items():
        for l in range(L):
            wl[(nm, l)] = dscr("wl_%s_%d" % (nm, l), [nel // 2048, 2048], BF16)
            wg[(nm, l)] = dscr("wg_%s_%d" % (nm, l), [8 * nel // 2048, 2048], BF16)
    h_scr = dscr("h_scr", [NTOT, D], F32)
    h1_scr = dscr("h1_scr", [NTOT, D], F32)
    s_scr = dscr("s_scr", [c.CONV_W, NTOT], F32)
    gb_scr = dscr("gb_scr", [c.CONV_W, NTOT], F32)
    g_scr = dscr("g_scr", [c.CFM_W, NTOT], F32)
    q_scr = dscr("q_scr", [H2 * 128, NTOT], BF16)
    kv_loc = dscr("kv_loc", [c.KVN // 2048, 2048], BF16)
    kvg = dscr("kvg", [8 * c.KVN // 2048, 2048], BF16)
    kc_scr = dscr("kc_scr", [H2 * 128, 2 * CTX], BF16)
    vc_scr = dscr("vc_scr", [2 * CTX, c.ATT_W], BF16)
    halo_loc = dscr("halo_loc", [c.CFM_W, 128], F32)
    halo_g = dscr("halo_g", [8 * c.CFM_W, 128], F32)
    mix_scr = dscr("mix_scr", [D, NTOT], BF16)
    MODN = L * 3 * 128 * NSC
    mod_loc = dscr("mod_loc", [1, MODN], F32)
    mod_all = dscr("mod_all", [8, MODN], F32)

    NK = H2 * 128 * NT
    kvl_flat = flat(kv_loc.ap())
    k_loc = kvl_flat[0:NK].rearrange("(r t) -> r t", t=NT)
    v_loc = kvl_flat[NK:c.KVN].rearrange("(t a) -> t a", a=c.ATT_W)
    kvg_flat = flat(kvg.ap())

    def kg_rank(r):
        return kvg_flat[r * c.KVN:r * c.KVN + NK].rearrange("(r t) -> r t", t=NT)

    def vg_rank(r):
        return kvg_flat[r * c.KVN + NK:(r + 1) * c.KVN].rearrange("(t a) -> t a", a=c.ATT_W)

    def wview(nm, l, cols):
        return flat(wg[(nm, l)].ap()).rearrange("(r x) -> r x", x=cols)

    RG = [list(range(8))]

    def AG(src_ap, dst_ap, rd, wr):
        P.op('pool', lambda e: e.collective_compute("AllGather", ALU.bypass, replica_groups=RG,
                                                    ins=[src_ap], outs=[dst_ap]), rd, wr, kind='cc')

    blocks = []
    for b in range(2):
        for k in range(NTh // TB):
            blocks.append((b * NTh + k * TB, TB, b, b, False, k * TB))
    for b in range(2):
        blocks.append((NT + b * CTX, CTX, 2, b, True, 0))

    cur_l = [0]

    def cut(name):
        st = getattr(c, 'stop', None)
        if st is None or P.dead:
            return
        sl = 0
        if '@' in st:
            st, sl = st.split('@')
            sl = int(sl)
        if st == name and cur_l[0] == sl:
            P.barrier()
            P.dead = True

    es = ExitStack()
    with es:
        uid = [0]

        def sb(name, shape, dt, stack=None):
            uid[0] += 1
            return (stack or es).enter_context(nc.sbuf_tensor("s%d_%s" % (uid[0], name), list(shape), dt))

        ps = es.enter_context(nc.psum_tensor("ps", [128, 8, 512], F32))

        def PSR(b):
            return ('ps', b)

        ident = sb("ident", [128, 128], F32)
        pm_f = sb("pm_f", [128, 128], F32)
        pm_b = sb("pm_b", [128, 128], BF16)
        ones_f = sb("ones_f", [128, 128], F32)
        ones_b = sb("ones_b", [128, 128], BF16)
        eps_t = sb("eps_t", [128, 1], F32)
        cosT = sb("cosT_s", [128, NTh], F32)
        sinT = sb("sinT_s", [128, NTh], F32)
        sel_t = sb("sel_t", [128, 16], F32)
        gmix_c = sb("gmix_c", [128, L, KC], F32)
        gffn_c = sb("gffn_c", [128, L, KC], F32)
        gfin_c = sb("gfin_c", [128, KC], F32)
        wsc_c = sb("wsc_c", [128, L, CG, 3], F32)
        wcf_c = sb("wcf_c", [128, L, CG, 31], F32)
        bcf_c = sb("bcf_c", [128, L, CG], F32)
        lng_c = sb("lng_c", [128, L, CG], F32)
        lnb_c = sb("lnb_c", [128, L, CG], F32)
        subg_c = sb("subg_c", [128, L, 2], F32)
        lam_c = sb("lam_c", [128, L, 4], F32)
        lamp = sb("lamp", [128, L, 2], F32)
        neglam = sb("neglam", [128, L], F32)
        modT = sb("modT", [128, L * 3, 6 * KC], F32)
        a1 = sb("a1", [128, L * 3, KC], F32)
        a2 = sb("a2", [128, L * 3, KC], F32)
        brow = sb("brow", [1, L, 36], F32)
        zpad = sb("zpad", [128, 60], F32)

        nc_ctx = nc.allow_non_contiguous_dma(reason="small column-layout parameter loads")
        nc_ctx.__enter__()
        try:

            DMA(P, 'sp', ident[:], ident_d.ap(), (), ['ident'])
            DMA(P, 'sp', pm_f[:], pm_d.ap(), (), ['pm_f'])
            COPY(P, 'dve', pm_b[:], pm_f[:], ['pm_f'], ['pm_b'])
            MEMSET(P, 'dve', ones_f[:], 1.0, ['ones_f'])
            MEMSET(P, 'dve', ones_b[:], 1.0, ['ones_b'])
            MEMSET(P, 'dve', eps_t[:], EPS, ['eps_t'])
            DMA(P, 'sp', cosT[:], cosT_d.ap(), (), ['cosT'])
            DMA(P, 'sp', sinT[:], sinT_d.ap(), (), ['sinT'])
            DMA(P, 'sp', sel_t[:], selLR.ap(), (), ['sel'])
            for l in range(L):
                DMA(P, 'sp', gmix_c[:, l, :], g_mix.ap()[l].rearrange("(k p) -> p k", p=128), (), ['gmix'])
                DMA(P, 'sp', gffn_c[:, l, :], g_ffn.ap()[l].rearrange("(k p) -> p k", p=128), (), ['gffn'])
                for g in range(CG):
                    DMA(P, 'sp', wsc_c[:, l, g, :], sc_w.ap()[l][:, g * 128:(g + 1) * 128].rearrange("k p -> p k"), (), ['wsc'])
                    DMA(P, 'sp', wcf_c[:, l, g, :], cf_w.ap()[l][:, g * 128:(g + 1) * 128].rearrange("k p -> p k"), (), ['wcf'])
                DMA(P, 'sp', bcf_c[:, l, :], cf_b.ap()[l].rearrange("(g p) -> p g", p=128), (), ['bcf'])
                DMA(P, 'sp', lng_c[:, l, :], ln_g.ap()[l].rearrange("(g p) -> p g", p=128), (), ['lng'])
                DMA(P, 'sp', lnb_c[:, l, :], ln_b.ap()[l].rearrange("(g p) -> p g", p=128), (), ['lnb'])
                DMA(P, 'sp', subg_c[:, l, :], subln_g.ap()[l].rearrange("(g p) -> p g", p=128), (), ['subg'])
                DMA(P, 'sp', lam_c[:, l, :], lam_qk.ap()[l].rearrange("k p -> p k"), (), ['lamc'])
                DMA(P, 'sp', brow[0:1, l, 0:4], b_rg.ap()[l:l + 1, :], (), ['brow'])
                DMA(P, 'sp', brow[0:1, l, 4:36], b_re.ap()[l:l + 1, :], (), ['brow'])
            DMA(P, 'sp', gfin_c[:], g_final.ap().rearrange("(k p) -> p k", p=128), (), ['gfin'])

            lam_init = [0.8 - 0.6 * math.exp(-0.3 * l) for l in range(L)]
            for l in range(L):
                TT(P, 'dve', lamp[:, l, 0:1], lam_c[:, l, 0:1], lam_c[:, l, 1:2], ALU.mult, ['lamc'], ['lamp'])
                TT(P, 'dve', lamp[:, l, 1:2], lam_c[:, l, 2:3], lam_c[:, l, 3:4], ALU.mult, ['lamc', 'lamp'], ['lamp'])
            lamflat = lamp[:].rearrange("p l k -> p (l k)")
            MM(P, ps[:, 7, 0:2 * L], ones_f[:], lamflat, True, True, ['ones_f', 'lamp'], [PSR(7)])
            ACTV(P, lamp[:].rearrange("p l k -> p (l k)"), ps[:, 7, 0:2 * L], AF.Exp, [PSR(7)], ['lamp'])
            for l in range(L):
                STT(P, neglam[:, l:l + 1], lamp[:, l, 1:2], -lam_init[l], lamp[:, l, 0:1], ALU.add, ALU.subtract,
                    ['lamp'], ['neglam'])
                TS(P, 'dve', subg_c[:, l, :], subg_c[:, l, :], 1.0 - lam_init[l], None, ALU.mult, None, ['subg'], ['subg'])

            for l in range(L):
                for nm in ('in', 'out', 'gate', 'up', 'down'):
                    src, nel = wspec[nm]
                    sv = flat(src.ap()[l]).rearrange("(a x) -> a x", x=2048)
                    DMA(P, 'pool', wl[(nm, l)].ap(), sv, (), [('wl', nm, l)])

            def gather_w(nm, l):
                AG(wl[(nm, l)].ap(), wg[(nm, l)].ap(), [('wl', nm, l)], [('wg', nm, l)])

            with ExitStack() as s1:
                crow = sb("crow", [3, D], F32, s1)
                cT = sb("cT", [128, KC, 4], F32, s1)
                modl = sb("modl", [128, L * 3, NSC], F32, s1)
                bcol = sb("bcol", [128, L, NSC], F32, s1)
                wad = [sb("wad%d" % i, [128, KC, 256], F32, s1) for i in range(2)]
                DMA(P, 'sp', crow[:], cvec.ap(), (), ['crow'])
                ACTV(P, crow[:], crow[:], AF.Silu, ['crow'], ['crow'])
                for l in range(L):
                    DMA(P, 'sp', bcol[:, l, :], b_ada_sh.ap()[l].rearrange("(n p) -> p n", p=128), (), ['bcol'])
                for k in range(KC):
                    TR(P, ps[:, 7, 0:3], crow[0:3, k * 128:(k + 1) * 128], ident[0:3, 0:3], ['crow', 'ident'], [PSR(7)])
                    COPY(P, 'dve', cT[:, k, 0:3], ps[:, 7, 0:3], [PSR(7)], ['cT'])
                npan = c.NS // 256
                pi = 0
                for l in range(L):
                    for pn in range(npan):
                        wt = wad[pi % 2]
                        wr_ = ('wad', pi % 2)
                        pi += 1
                        DMA(P, 'sp', wt[:], w_ada_sh.ap()[l][:, pn * 256:(pn + 1) * 256].rearrange("(k p) x -> p k x", p=128),
                            (), [wr_])
                        for j2 in range(2):
                            n = pn * 2 + j2
                            bk = 4 + (n % 2)
                            for k in range(KC):
                                MM(P, ps[:, bk, 0:3], wt[:, k, j2 * 128:(j2 + 1) * 128], cT[:, k, 0:3], k == 0, k == KC - 1,
                                   [wr_, 'cT'], [PSR(bk)])
                            TS(P, 'dve', modl[:, l * 3:(l + 1) * 3, n], ps[:, bk, 0:3], bcol[:, l, n:n + 1], None, ALU.add, None,
                               [PSR(bk), 'bcol'], ['modl'])
                for l in range(L):
                    dst = mod_loc.ap()[0, l * 3 * 128 * NSC:(l + 1) * 3 * 128 * NSC].rearrange("(j p n) -> p j n", j=3, p=128)
                    DMA(P, 'sp', dst, modl[:, l * 3:(l + 1) * 3, :], ['modl'], ['mod_loc'], store=True)
                AG(mod_loc.ap(), mod_all.ap(), ['mod_loc'], ['mod_all'])
                for l in range(L):
                    for j in range(3):
                        o0 = (l * 3 + j) * 128 * NSC
                        src = mod_all.ap()[:, o0:o0 + 128 * NSC].rearrange("r (p n) -> p r n", p=128)
                        DMA(P, 'sp', modT[:, l * 3 + j, :].rearrange("p (r n) -> p r n", r=8), src, ['mod_all'], ['modT'])
                for l in range(L):
                    for j in range(3):
                        i = l * 3 + j
                        TS(P, 'dve', a1[:, i, :], modT[:, i, KC:2 * KC], 1.0, None, ALU.add, None, ['modT'], ['a1'])
                        TT(P, 'dve', a1[:, i, :], a1[:, i, :], gmix_c[:, l, :], ALU.mult, ['a1', 'gmix'], ['a1'])
                        TS(P, 'dve', a2[:, i, :], modT[:, i, 4 * KC:5 * KC], 1.0, None, ALU.add, None, ['modT'], ['a2'])
                        TT(P, 'dve', a2[:, i, :], a2[:, i, :], gffn_c[:, l, :], ALU.mult, ['a2', 'gffn'], ['a2'])
            gather_w('in', 0)
            P.barrier()
            cut('ada')

            def mvec(l, j, which):
                return modT[:, l * 3 + j, which * KC:(which + 1) * KC]

            def bcast_cols(col_ap, dst, rd, wr, tmp, tmpname):
                for k0 in range(0, KC, 4):
                    for k in range(k0, min(KC, k0 + 4)):
                        TS(P, 'dve', tmp[:], ident[:], col_ap[:, k:k + 1], None, ALU.mult, None, ['ident'] + rd, [tmpname])
                        MM(P, ps[:, 7, (k - k0) * 128:(k - k0 + 1) * 128], ones_f[:], tmp[:], True, True,
                           ['ones_f', tmpname], [PSR(7)])
                    w = min(KC, k0 + 4) - k0
                    ACTV(P, dst[:, k0 * 128:(k0 + w) * 128], ps[:, 7, 0:w * 128], AF.Identity, [PSR(7)], wr)

            def hsrc(l, row0, n):
                if l == 0:
                    if row0 < NT:
                        return x_sh.ap()[row0:row0 + n, :]
                    return ctx_in.ap()[row0 - NT:row0 - NT + n, :]
                return h_scr.ap()[row0:row0 + n, :]

            def norm_tile(l, j, src_ap, hb, hbn, avec, shvec, small, dst_writer, extra_rd):
                DMA(P, 'sp', hb[:], src_ap, extra_rd, [hbn])
                ss, rt = small
                cut('n1')
                ACTV(P, dst_junk[0][:], hb[:], AF.Square, [hbn], [dst_junk[1], 'ss'], accum=ss[:])
                cut('n2')
                TS(P, 'dve', rt[:], ss[:], 1.0 / D, EPS, ALU.mult, ALU.add, ['ss'], ['rt'])
                ACTV(P, rt[:], rt[:], AF.Ln, ['rt'], ['rt'])
                cut('n3')
                RSQ(P, rt[:], ['rt'], ['rt'])
                cut('n4')
                TS(P, 'dve', hb[:], hb[:], rt[:, 0:1], None, ALU.mult, None, [hbn, 'rt'], [hbn])
                cut('n5')
                for k0 in range(0, KC, 4):
                    bk = 4 + (k0 // 4) % 2
                    for k in range(k0, k0 + 4):
                        TR(P, ps[:, bk, (k - k0) * 128:(k - k0 + 1) * 128], hb[:, k * 128:(k + 1) * 128], ident[:],
                           [hbn, 'ident'], [PSR(bk)])
                    cut('n6')
                    for k in range(k0, k0 + 4):
                        dst_writer(k, ps[:, bk, (k - k0) * 128:(k - k0 + 1) * 128], PSR(bk))
                        cut('n7')

            dst_junk = [None, None]

            for l in range(L):
                last = (l == L - 1)
                cur_l[0] = l
                with ExitStack() as s1:
                    hb = sb("p1_hb", [128, D], F32, s1)
                    junk = sb("p1_junk", [128, D], BF16, s1)
                    dst_junk[0], dst_junk[1] = junk, 'junk'
                    ss = sb("p1_ss", [128, 1], F32, s1)
                    rt = sb("p1_rt", [128, 1], F32, s1)
                    uT = sb("p1_uT", [128, KC, TBM], BF16, s1)
                    wr_ring = [sb("p1_w%d" % i, [128, KC, PW], BF16, s1) for i in range(3)]
                    xs = sb("p1_xs", [128, CG, TBM], F32, s1)
                    st32 = [sb("p1_st32_%d" % i, [128, 2, TBM], F32, s1) for i in range(2)]
                    st16 = [sb("p1_st16_%d" % i, [128, 2, TBM], BF16, s1) for i in range(2)]
                    stv = [sb("p1_stv_%d" % i, [128, PW], BF16, s1) for i in range(2)]
                    qs = sb("p1_qs", [128, 2, TBM], BF16, s1)
                    t1 = sb("p1_t1", [128, TBM], F32, s1)
                    t2 = sb("p1_t2", [128, TBM], F32, s1)
                    win = wview('in', l, c.PROJ_W)
                    wi = 0
                    s32i = 0
                    s16i = 0
                    svi = 0
                    for bi_, (row0, n, j, b, is_ctx, tok0) in enumerate(blocks):
                        cut('p1b%d' % bi_)
                        if is_ctx and last:
                            segs = [('k', c.OFF_K, c.QK_W), ('v', c.OFF_V, c.ATT_W)]
                        else:
                            segs = [('xa', 0, c.CONV_W), ('gc', 2 * c.CONV_W, c.CONV_W), ('gb', c.CONV_W, c.CONV_W),
                                    ('q', c.OFF_Q, c.QK_W), ('k', c.OFF_K, c.QK_W),
                                    ('ga', c.OFF_C, c.CFM_W), ('gg', c.OFF_C + c.CFM_W, c.CFM_W), ('v', c.OFF_V, c.ATT_W)]
                        av, shv = a1[:, l * 3 + j, :], mvec(l, j, 0)
                        for t in range(n // 128):
                            def wrt(k, pss, psr, t=t, av=av, shv=shv):
                                if False:
                                    ACTV(P, uT[:, k, t * 128:(t + 1) * 128], pss, AF.Identity, [psr, 'a1', 'modT'], ['uT'],
                                         bias=shv[:, k:k + 1], scale=av[:, k:k + 1])
                                else:
                                    TS(P, 'dve', uT[:, k, t * 128:(t + 1) * 128], pss, av[:, k:k + 1], shv[:, k:k + 1],
                                       ALU.mult, ALU.add, [psr, 'a1', 'modT'], ['uT'])
                            norm_tile(l, j, hsrc(l, row0 + t * 128, 128), hb, 'hb', av, shv, (ss, rt), wrt,
                                      [('h', l)])
                        cut('p1n')
                        for (kind, off, width) in segs:
                            cut('p1_' + kind)
                            for pn in range(width // PW):
                                col0 = off + pn * PW
                                w = wr_ring[wi % 3]
                                wn = ('p1w', wi % 3)
                                wi += 1
                                DMA(P, 'sp', w[:], win[:, col0:col0 + PW].rearrange("(k p) x -> p k x", p=128),
                                    [('wg', 'in', l)], [wn])
                                if kind == 'v':
                                    for t in range(n // 128):
                                        bk = t % 4
                                        for k in range(KC):
                                            MM(P, ps[:, bk, 0:PW], uT[:, k, t * 128:(t + 1) * 128], w[:, k, :], k == 0, k == KC - 1,
                                               ['uT', wn], [PSR(bk)])
                                        sv = st16[s16i % 2][:, 0, 0:PW]
                                        svn = ('st16', s16i % 2)
                                        s16i += 1
                                        if getattr(c, 'vx', '') != 'noevac':
                                            ACTV(P, sv, ps[:, bk, 0:PW], AF.Copy, [PSR(bk)], [svn])
                                        c0 = pn * PW
                                        if getattr(c, 'vx', '') in ('noevac', 'nostore'):
                                            pass
                                        elif is_ctx:
                                            dst = vc_scr.ap()[b * CTX + t * 128:b * CTX + (t + 1) * 128, c0:c0 + PW]
                                            DMA(P, 'sp', dst, sv, [svn], [('vc', l)], store=True)
                                        else:
                                            vx = getattr(c, 'vx', '')
                                            if vx == 'vc':
                                                dst = vc_scr.ap()[row0 + t * 128:row0 + (t + 1) * 128, c0:c0 + PW]
                                                DMA(P, 'sp', dst, sv, [svn], [('kvl', l)], store=True)
                                            elif vx == 'half':
                                                dst = v_loc[row0 + t * 128:row0 + (t + 1) * 128, c0:c0 + 128]
                                                DMA(P, 'sp', dst, sv[:, 0:128], [svn], [('kvl', l)], store=True)
                                            else:
                                                dst = v_loc[row0 + t * 128:row0 + (t + 1) * 128, c0:c0 + PW]
                                                DMA(P, 'sp', dst, sv, [svn], [('kvl', l)], store=True)
                                    continue
                                base_bk = 2 * (pn % 2)
                                for cc in range(2):
                                    bk = base_bk + cc
                                    for k in range(KC):
                                        MM(P, ps[:, bk, 0:n], w[:, k, cc * 128:(cc + 1) * 128], uT[:, k, 0:n], k == 0, k == KC - 1,
                                           ['uT', wn], [PSR(bk)])
                                gch = pn * 2
                                if kind == 'xa':
                                    for cc in range(2):
                                        ACTV(P, xs[:, gch + cc, 0:n], ps[:, base_bk + cc, 0:n], AF.Copy, [PSR(base_bk + cc)], ['xs'])
                                elif kind == 'ga':
                                    for cc in range(2):
                                        ACTV(P, xs[:, gch + cc, 0:n], ps[:, base_bk + cc, 0:n], AF.Copy, [PSR(base_bk + cc)], ['xs'])
                                elif kind == 'gg':
                                    st = st32[s32i % 2]
                                    stn = ('st32', s32i % 2)
                                    s32i += 1
                                    for cc in range(2):
                                        ACTV(P, st[:, cc, 0:n], ps[:, base_bk + cc, 0:n], AF.Exp, [PSR(base_bk + cc)], [stn], scale=-1.0)
                                        TS(P, 'dve', st[:, cc, 0:n], st[:, cc, 0:n], 1.0, None, ALU.add, None, [stn], [stn])
                                        RECIP(P, st[:, cc, 0:n], st[:, cc, 0:n], [stn], [stn])
                                        TT(P, 'dve', st[:, cc, 0:n], st[:, cc, 0:n], xs[:, gch + cc, 0:n], ALU.mult, [stn, 'xs'], [stn])
                                    for c2 in range(2):
                                        DMA(P, 'sp', g_scr.ap()[(gch + c2) * 128:(gch + c2 + 1) * 128, row0:row0 + n], st[:, c2, 0:n], [stn], [('ga', l)], store=True)
                                elif kind in ('gc', 'gb'):
                                    st = st32[s32i % 2]
                                    stn = ('st32', s32i % 2)
                                    s32i += 1
                                    for cc in range(2):
                                        if kind == 'gb':
                                            ACTV(P, st[:, cc, 0:n], ps[:, base_bk + cc, 0:n], AF.Copy, [PSR(base_bk + cc)], [stn])
                                        else:
                                            TT(P, 'dve', st[:, cc, 0:n], ps[:, base_bk + cc, 0:n], xs[:, gch + cc, 0:n], ALU.mult,
                                               [PSR(base_bk + cc), 'xs'], [stn])
                                    scr = {'gc': s_scr, 'gb': gb_scr}[kind]
                                    for c2 in range(2):
                                        DMA(P, 'sp', scr.ap()[(gch + c2) * 128:(gch + c2 + 1) * 128, row0:row0 + n], st[:, c2, 0:n], [stn], [(kind, l)], store=True)
                                else:
                                    st = st16[s16i % 2]
                                    stn = ('st16', s16i % 2)
                                    s16i += 1
                                    for cc in range(2):
                                        bk = base_bk + cc
                                        if is_ctx or getattr(c, 'norope', False):
                                            ACTV(P, st[:, cc, 0:n], ps[:, bk, 0:n], AF.Copy, [PSR(bk)], [stn])
                                            continue
                                        ACTV(P, qs[:, cc, 0:n], ps[:, bk, 0:n], AF.Copy, [PSR(bk)], ['qs'])
                                        rb = 6 + cc
                                        MM(P, ps[:, rb, 0:n], pm_b[:], qs[:, cc, 0:n], True, True, ['pm_b', 'qs'], [PSR(rb)])
                                        TT(P, 'dve', t2[:, 0:n], ps[:, rb, 0:n], sinT[:, tok0:tok0 + n], ALU.mult,
                                           [PSR(rb), 'sinT'], ['t2'])
                                        TT(P, 'dve', t1[:, 0:n], qs[:, cc, 0:n], cosT[:, tok0:tok0 + n], ALU.mult,
                                           ['qs', 'cosT'], ['t1'])
                                        TT(P, 'dve', st[:, cc, 0:n], t1[:, 0:n], t2[:, 0:n], ALU.add, ['t1', 't2'], [stn])
                                    if kind == 'q':
                                        for c2 in range(2):
                                            DMA(P, 'sp', q_scr.ap()[(gch + c2) * 128:(gch + c2 + 1) * 128, row0:row0 + n], st[:, c2, 0:n], [stn], [('q', l)], store=True)
                                    elif is_ctx:
                                        for c2 in range(2):
                                            DMA(P, 'sp', kc_scr.ap()[(gch + c2) * 128:(gch + c2 + 1) * 128, b * CTX:b * CTX + n], st[:, c2, 0:n], [stn], [('kc', l)], store=True)
                                    else:
                                        for c2 in range(2):
                                            DMA(P, 'sp', k_loc[(gch + c2) * 128:(gch + c2 + 1) * 128, row0:row0 + n], st[:, c2, 0:n], [stn], [('kvl', l)], store=True)
                P.barrier()
                cut('p1')

                AG(kv_loc.ap(), kvg.ap(), [('kvl', l)], [('kvg', l)])
                if l == 0:
                    MEMSET(P, 'dve', zpad[:], 0.0, ['zpad'])
                    for g in range(CG):
                        DMA(P, 'sp', halo_loc.ap()[g * 128:(g + 1) * 128, 68:128], zpad[:], ['zpad'], ['halo_loc'], store=True)
                for b in range(2):
                    hl = halo_loc.ap()
                    DMA(P, 'sp', hl[:, b * 34:b * 34 + 16], g_scr.ap()[:, b * NTh:b * NTh + 16], [('ga', l)], ['halo_loc'], store=True)
                    DMA(P, 'sp', hl[:, b * 34 + 16:b * 34 + 32], g_scr.ap()[:, (b + 1) * NTh - 16:(b + 1) * NTh], [('ga', l)],
                        ['halo_loc'], store=True)
                    DMA(P, 'sp', hl[:, b * 34 + 32:b * 34 + 33], s_scr.ap()[:, b * NTh:b * NTh + 1], [('gc', l)], ['halo_loc'], store=True)
                    DMA(P, 'sp', hl[:, b * 34 + 33:b * 34 + 34], s_scr.ap()[:, (b + 1) * NTh - 1:(b + 1) * NTh], [('gc', l)],
                        ['halo_loc'], store=True)
                AG(halo_loc.ap(), halo_g.ap(), ['halo_loc'], ['halo_g'])
                for nm in ('out', 'gate', 'up', 'down'):
                    gather_w(nm, l)
                if not last:
                    gather_w('in', l + 1)

                cut('xchg')
                with ExitStack() as s1:
                    SEGM = max(NTh, CTX)
                    CH = min(512, SEGM)
                    hal = sb("cv_hal", [128, CG, 8, 128], F32, s1)
                    hL = sb("cv_hL", [128, 2, CG, 16], F32, s1)
                    hR = sb("cv_hR", [128, 2, CG, 16], F32, s1)
                    sLR = sb("cv_sLR", [128, 2, CG, 2], F32, s1)
                    gbuf = [sb("cv_gbuf%d" % i, [128, SEGM + 32], F32, s1) for i in range(2)]
                    zall = sb("cv_z", [128, CG, SEGM], F32, s1)
                    zsq = [sb("cv_zsq%d" % i, [128, CH], F32, s1) for i in range(2)]
                    mu = sb("cv_mu", [128, CH], F32, s1)
                    msq = sb("cv_msq", [128, CH], F32, s1)
                    rs = sb("cv_rs", [128, CH], F32, s1)
                    tt = [sb("cv_tt%d" % i, [128, CH], F32, s1) for i in range(2)]
                    yst = [sb("cv_yst%d" % i, [128, CH], BF16, s1) for i in range(2)]
                    sbuf_ = [sb("cv_s%d" % i, [128, SEGM + 32], F32, s1) for i in range(2)]
                    gbt = [sb("cv_gb%d" % i, [128, SEGM], F32, s1) for i in range(2)]
                    pa = [sb("cv_pa%d" % i, [128, SEGM], F32, s1) for i in range(2)]
                    pb = sb("cv_pb", [128, SEGM], F32, s1)
                    ysh = [sb("cv_ysh%d" % i, [128, SEGM], BF16, s1) for i in range(2)]
                    for g in range(CG):
                        src = halo_g.ap().rearrange("(r x) y -> x r y", r=8)[g * 128:(g + 1) * 128]
                        DMA(P, 'sp', hal[:, g, :, :], src, ['halo_g'], ['hal'])
                    cut('cvl')
                    for b in range(2):
                        for r in range(8):
                            o = b * 34
                            if r == 0:
                                TS(P, 'dve', hL[:, b, :, 0:15], hal[:, :, r, o + 17:o + 32], sel_t[:, r:r + 1], None, ALU.mult, None,
                                   ['hal', 'sel'], ['hL'])
                                TS(P, 'dve', hR[:, b, :, 0:15], hal[:, :, r, o:o + 15], sel_t[:, 8 + r:9 + r], None, ALU.mult, None,
                                   ['hal', 'sel'], ['hR'])
                                TS(P, 'dve', sLR[:, b, :, 0:1], hal[:, :, r, o + 33:o + 34], sel_t[:, r:r + 1], None, ALU.mult, None,
                                   ['hal', 'sel'], ['sLR'])
                                TS(P, 'dve', sLR[:, b, :, 1:2], hal[:, :, r, o + 32:o + 33], sel_t[:, 8 + r:9 + r], None, ALU.mult, None,
                                   ['hal', 'sel'], ['sLR'])
                            else:
                                STT(P, hL[:, b, :, 0:15], hal[:, :, r, o + 17:o + 32], sel_t[:, r:r + 1], hL[:, b, :, 0:15],
                                    ALU.mult, ALU.add, ['hal', 'sel', 'hL'], ['hL'])
                                STT(P, hR[:, b, :, 0:15], hal[:, :, r, o:o + 15], sel_t[:, 8 + r:9 + r], hR[:, b, :, 0:15],
                                    ALU.mult, ALU.add, ['hal', 'sel', 'hR'], ['hR'])
                                STT(P, sLR[:, b, :, 0:1], hal[:, :, r, o + 33:o + 34], sel_t[:, r:r + 1], sLR[:, b, :, 0:1],
                                    ALU.mult, ALU.add, ['hal', 'sel', 'sLR'], ['sLR'])
                                STT(P, sLR[:, b, :, 1:2], hal[:, :, r, o + 32:o + 33], sel_t[:, 8 + r:9 + r], sLR[:, b, :, 1:2],
                                    ALU.mult, ALU.add, ['hal', 'sel', 'sLR'], ['sLR'])
                    cut('cvh')
                    segs = [(b * NTh, NTh, b, False) for b in range(2)]
                    if not last:
                        segs += [(NT + b * CTX, CTX, b, True) for b in range(2)]
                    gi = 0
                    for (row0, n, b, is_ctx) in segs:
                        for g in range(CG):
                            gb_ = gbuf[gi % 2]
                            gbn = ('gbuf', gi % 2)
                            DMA(P, 'sp', gb_[:, 16:16 + n], g_scr.ap()[g * 128:(g + 1) * 128, row0:row0 + n], [('ga', l)], [gbn])
                            if is_ctx:
                                MEMSET(P, 'pool', gb_[:, 1:16], 0.0, [gbn])
                                MEMSET(P, 'pool', gb_[:, 16 + n:31 + n], 0.0, [gbn])
                            else:
                                COPY(P, 'pool', gb_[:, 1:16], hL[:, b, g, 0:15], ['hL'], [gbn])
                                COPY(P, 'pool', gb_[:, 16 + n:31 + n], hR[:, b, g, 0:15], ['hR'], [gbn])
                            zr = ('z', g)
                            TS(P, 'dve', zall[:, g, 0:n], gb_[:, 1:1 + n], wcf_c[:, l, g, 0:1], bcf_c[:, l, g:g + 1], ALU.mult, ALU.add,
                               [gbn, 'wcf', 'bcf'], [zr])
                            for k in range(1, 31):
                                STT(P, zall[:, g, 0:n], gb_[:, k + 1:k + 1 + n], wcf_c[:, l, g, k:k + 1], zall[:, g, 0:n], ALU.mult, ALU.add,
                                    [gbn, 'wcf', zr], [zr])
                            cut('cv1')
                            s_ = sbuf_[gi % 2]
                            sn = ('sbuf', gi % 2)
                            g2 = gbt[gi % 2]
                            g2n = ('gbt', gi % 2)
                            p_a = pa[gi % 2]
                            pan = ('pa', gi % 2)
                            y_ = ysh[gi % 2]
                            yn = ('ysh', gi % 2)
                            DMA(P, 'sp', s_[:, 16:16 + n], s_scr.ap()[g * 128:(g + 1) * 128, row0:row0 + n], [('gc', l)], [sn])
                            DMA(P, 'sp', g2[:, 0:n], gb_scr.ap()[g * 128:(g + 1) * 128, row0:row0 + n], [('gb', l)], [g2n])
                            if is_ctx:
                                MEMSET(P, 'pool', s_[:, 15:16], 0.0, [sn])
                                MEMSET(P, 'pool', s_[:, 16 + n:17 + n], 0.0, [sn])
                            else:
                                COPY(P, 'pool', s_[:, 15:16], sLR[:, b, g, 0:1], ['sLR'], [sn])
                                COPY(P, 'pool', s_[:, 16 + n:17 + n], sLR[:, b, g, 1:2], ['sLR'], [sn])
                            TS(P, 'pool', p_a[:, 0:n], s_[:, 15:15 + n], wsc_c[:, l, g, 0:1], None, ALU.mult, None, [sn, 'wsc'], [pan])
                            TS(P, 'pool', pb[:, 0:n], s_[:, 16:16 + n], wsc_c[:, l, g, 1:2], None, ALU.mult, None, [sn, 'wsc'], ['pb'])
                            TT(P, 'pool', p_a[:, 0:n], p_a[:, 0:n], pb[:, 0:n], ALU.add, [pan, 'pb'], [pan])
                            TS(P, 'pool', pb[:, 0:n], s_[:, 17:17 + n], wsc_c[:, l, g, 2:3], None, ALU.mult, None, [sn, 'wsc'], ['pb'])
                            TT(P, 'pool', p_a[:, 0:n], p_a[:, 0:n], pb[:, 0:n], ALU.add, [pan, 'pb'], [pan])
                            TT(P, 'pool', y_[:, 0:n], p_a[:, 0:n], g2[:, 0:n], ALU.mult, [pan, g2n], [yn])
                            DMA(P, 'sp', mix_scr.ap()[g * 128:(g + 1) * 128, row0:row0 + n], y_[:, 0:n], [yn], [('mix', l)], store=True)
                            gi += 1
                        cut('cv2')
                        zi = 0
                        for c0 in range(0, n, CH):
                            m = min(CH, n - c0)
                            for g in range(CG):
                                MM(P, ps[:, 0, 0:m], ones_f[:], zall[:, g, c0:c0 + m], g == 0, g == CG - 1, ['ones_f', ('z', g)], [PSR(0)])
                            for g in range(CG):
                                zq = zsq[zi % 2]
                                zqn = ('zsq', zi % 2)
                                zi += 1
                                TT(P, 'pool', zq[:, 0:m], zall[:, g, c0:c0 + m], zall[:, g, c0:c0 + m], ALU.mult, [('z', g)], [zqn])
                                MM(P, ps[:, 1, 0:m], ones_f[:], zq[:, 0:m], g == 0, g == CG - 1, ['ones_f', zqn], [PSR(1)])
                            ACTV(P, mu[:, 0:m], ps[:, 0, 0:m], AF.Copy, [PSR(0)], ['mu'], scale=1.0 / c.CFM_W)
                            TT(P, 'pool', msq[:, 0:m], mu[:, 0:m], mu[:, 0:m], ALU.mult, ['mu'], ['msq'])
                            STT(P, rs[:, 0:m], ps[:, 1, 0:m], 1.0 / c.CFM_W, msq[:, 0:m], ALU.mult, ALU.subtract, [PSR(1), 'msq'], ['rs'])
                            TS(P, 'dve', rs[:, 0:m], rs[:, 0:m], 1.0, EPS, ALU.mult, ALU.add, ['rs'], ['rs'])
                            ACTV(P, rs[:, 0:m], rs[:, 0:m], AF.Ln, ['rs'], ['rs'])
                            RSQ(P, rs[:, 0:m], ['rs'], ['rs'])
                            for g in range(CG):
                                t_ = tt[g % 2]
                                tn = ('tt', g % 2)
                                y_ = yst[g % 2]
                                yn = ('yst', g % 2)
                                TT(P, 'dve', t_[:, 0:m], zall[:, g, c0:c0 + m], mu[:, 0:m], ALU.subtract, [('z', g), 'mu'], [tn])
                                TT(P, 'pool', t_[:, 0:m], t_[:, 0:m], rs[:, 0:m], ALU.mult, [tn, 'rs'], [tn])
                                TS(P, 'dve', t_[:, 0:m], t_[:, 0:m], lng_c[:, l, g:g + 1], lnb_c[:, l, g:g + 1], ALU.mult, ALU.add, [tn, 'lng', 'lnb'], [tn])
                                ACTV(P, y_[:, 0:m], t_[:, 0:m], AF.Silu, [tn], [yn])

                                ch = CG + H2 + g
                                DMA(P, 'sp', mix_scr.ap()[ch * 128:(ch + 1) * 128, row0 + c0:row0 + c0 + m], y_[:, 0:m], [yn],
                                    [('mix', l)], store=True)
                P.barrier()
                cut('conv')

                with ExitStack() as s1:
                    NKEY = c.SEQ + CTX
                    NKT = NKEY // 128
                    LKT = c.SEQ // 128
                    kT = [sb("at_kT%d" % i, [128, 2, NKEY], BF16, s1) for i in range(2)]
                    vt = sb("at_vt", [128, NKT, 256], BF16, s1)
                    qT = [sb("at_qT%d" % i, [128, 2, NTh + CTX], BF16, s1) for i in range(2)]
                    pT = [sb("at_pT%d" % i, [128, 512], BF16, s1) for i in range(4)]
                    om = [sb("at_om%d" % i, [128, 2, 512], F32, s1) for i in range(2)]
                    rl = sb("at_rl", [128, 512], F32, s1)
                    att = sb("at_att", [128, 2, 512], F32, s1)
                    sq = sb("at_sq", [128, 2, 512], F32, s1)
                    rsd = sb("at_rsd", [128, 512], F32, s1)
                    ybf = [sb("at_y%d" % i, [128, 2, 512], BF16, s1) for i in range(2)]
                    scale = 128.0 ** -0.5
                    hi = 0
                    pi = 0
                    yi = 0
                    for b in range(2):
                        for h in range(H):
                            kt_ = kT[hi % 2]
                            ktn = ('kT', hi % 2)
                            qt_ = qT[hi % 2]
                            qtn = ('qT', hi % 2)
                            hi += 1
                            for r in range(8):
                                src = kg_rank(r)[2 * h * 128:(2 * h + 2) * 128, b * NTh:(b + 1) * NTh].rearrange("(m p) t -> p m t", p=128)
                                DMA(P, 'sp', kt_[:, :, r * NTh:(r + 1) * NTh], src, [('kvg', l)], [ktn])
                                src = vg_rank(r)[b * NTh:(b + 1) * NTh, h * 256:(h + 1) * 256].rearrange("(k p) x -> p k x", p=128)
                                DMA(P, 'sp', vt[:, r * (NTh // 128):(r + 1) * (NTh // 128), :], src, [('kvg', l)], ['vt'])
                            src = kc_scr.ap()[2 * h * 128:(2 * h + 2) * 128, b * CTX:(b + 1) * CTX].rearrange("(m p) t -> p m t", p=128)
                            DMA(P, 'sp', kt_[:, :, c.SEQ:NKEY], src, [('kc', l)], [ktn])
                            src = vc_scr.ap()[b * CTX:(b + 1) * CTX, h * 256:(h + 1) * 256].rearrange("(k p) x -> p k x", p=128)
                            DMA(P, 'sp', vt[:, LKT:NKT, :], src, [('vc', l)], ['vt'])
                            src = q_scr.ap()[2 * h * 128:(2 * h + 2) * 128, b * NTh:(b + 1) * NTh].rearrange("(m p) t -> p m t", p=128)
                            DMA(P, 'sp', qt_[:, :, 0:NTh], src, [('q', l)], [qtn])
                            if not last:
                                src = q_scr.ap()[2 * h * 128:(2 * h + 2) * 128, NT + b * CTX:NT + (b + 1) * CTX].rearrange(
                                    "(m p) t -> p m t", p=128)
                                DMA(P, 'sp', qt_[:, :, NTh:NTh + CTX], src, [('q', l)], [qtn])
                            qblocks = [(k * TB, TB, 0, NKT, b * NTh + k * TB) for k in range(NTh // TB)]
                            if not last:
                                qblocks.append((NTh, CTX, LKT, NKT, NT + b * CTX))
                            for (q0, nq, kt0, kt1, orow) in qblocks:
                                for m in range(2):
                                    kts = list(range(kt0, kt1))

                                    def S(i):
                                        kt = kts[i]
                                        bk = i % 3
                                        MM(P, ps[:, bk, 0:nq], kt_[:, m, kt * 128:(kt + 1) * 128], qt_[:, m, q0:q0 + nq], True, True,
                                           [ktn, qtn], [PSR(bk)])

                                    def AV(i, pi):
                                        kt = kts[i]
                                        bk = i % 3
                                        p_ = pT[pi % 4]
                                        pn = ('pT', pi % 4)
                                        ACTV(P, p_[:, 0:nq], ps[:, bk, 0:nq], AF.Exp, [PSR(bk)], [pn], scale=scale)
                                        first, lastk = (i == 0), (i == len(kts) - 1)
                                        MM(P, ps[:, 3, 0:nq], vt[:, kt, 0:128], p_[:, 0:nq], first, lastk, ['vt', pn], [PSR(3)])
                                        MM(P, ps[:, 4, 0:nq], vt[:, kt, 128:256], p_[:, 0:nq], first, lastk, ['vt', pn], [PSR(4)])
                                        MM(P, ps[:, 5, 0:nq], ones_b[:], p_[:, 0:nq], first, lastk, ['ones_b', pn], [PSR(5)])

                                    S(0)
                                    for i in range(len(kts)):
                                        if i + 1 < len(kts):
                                            S(i + 1)
                                        AV(i, pi)
                                        pi += 1
                                    RECIP(P, rl[:, 0:nq], ps[:, 5, 0:nq], [PSR(5)], ['rl'])
                                    omr = ('om', m)
                                    TT(P, 'dve', om[m][:, 0, 0:nq], ps[:, 3, 0:nq], rl[:, 0:nq], ALU.mult, [PSR(3), 'rl'], [omr])
                                    TT(P, 'dve', om[m][:, 1, 0:nq], ps[:, 4, 0:nq], rl[:, 0:nq], ALU.mult, [PSR(4), 'rl'], [omr])
                                STT(P, att[:, :, 0:nq], om[1][:, :, 0:nq], neglam[:, l:l + 1], om[0][:, :, 0:nq], ALU.mult, ALU.add,
                                    [('om', 0), ('om', 1), 'neglam'], ['att'])
                                TT(P, 'pool', sq[:, :, 0:nq], att[:, :, 0:nq], att[:, :, 0:nq], ALU.mult, ['att'], ['sq'])
                                MM(P, ps[:, 6, 0:nq], ones_f[:], sq[:, 0, 0:nq], True, False, ['ones_f', 'sq'], [PSR(6)])
                                MM(P, ps[:, 6, 0:nq], ones_f[:], sq[:, 1, 0:nq], False, True, ['ones_f', 'sq'], [PSR(6)])
                                TS(P, 'dve', rsd[:, 0:nq], ps[:, 6, 0:nq], 1.0 / 256, EPS, ALU.mult, ALU.add, [PSR(6)], ['rsd'])
                                ACTV(P, rsd[:, 0:nq], rsd[:, 0:nq], AF.Ln, ['rsd'], ['rsd'])
                                RSQ(P, rsd[:, 0:nq], ['rsd'], ['rsd'])
                                y_ = ybf[yi % 2]
                                yn = ('ybf', yi % 2)
                                yi += 1
                                for cc in range(2):
                                    TT(P, 'pool', sq[:, cc, 0:nq], att[:, cc, 0:nq], rsd[:, 0:nq], ALU.mult, ['att', 'rsd', 'sq'], ['sq'])
                                    TS(P, 'dve', y_[:, cc, 0:nq], sq[:, cc, 0:nq], subg_c[:, l, cc:cc + 1], None, ALU.mult, None, ['sq', 'subg'], [yn])
                                ch = CG + 2 * h
                                for c2 in range(2):
                                    DMA(P, 'sp', mix_scr.ap()[(ch + c2) * 128:(ch + c2 + 1) * 128, orow:orow + nq], y_[:, c2, 0:nq], [yn], [('mix', l)], store=True)
                P.barrier()
                cut('attn')

                wout = wview('out', l, D)
                wgt = wview('gate', l, c.FF)
                wup = wview('up', l, c.FF)
                wdn = wview('down', l, D)
                for (row0, n, j, b, is_ctx, tok0) in blocks:
                    if is_ctx and last:
                        continue
                    NTL = n // 128
                    with ExitStack() as s1:
                        mixT = sb("o_mixT", [128, KC, TBM], BF16, s1)
                        wo = [sb("o_w%d" % i, [128, KC, 256], BF16, s1) for i in range(2)]
                        hbk = sb("o_hb", [128, TBM // 128, D], F32, s1)
                        gbc = sb("o_gbc", [128, D], F32, s1)
                        dtmp = sb("o_dtmp", [128, 128], F32, s1)
                        tmp = [sb("o_tmp%d" % i, [128, 256], F32, s1) for i in range(2)]
                        bcast_cols(mvec(l, j, 2), gbc, ['modT'], ['gbc'], dtmp, 'dtmp')
                        DMA(P, 'sp', mixT[:, :, 0:n], mix_scr.ap()[:, row0:row0 + n].rearrange("(k p) t -> p k t", p=128), [('mix', l)], ['mixT'])
                        for t in range(NTL):
                            DMA(P, 'sp', hbk[:, t, :], hsrc(l, row0 + t * 128, 128), [('h', l)], [('hbk', t)])
                        ti = 0
                        for pn in range(D // 256):
                            w = wo[pn % 2]
                            wn = ('wo', pn % 2)
                            DMA(P, 'sp', w[:], wout[:, pn * 256:(pn + 1) * 256].rearrange("(k p) x -> p k x", p=128), [('wg', 'out', l)], [wn])
                            for t in range(NTL):
                                bk = t % 4
                                for k in range(KC):
                                    MM(P, ps[:, bk, 0:256], mixT[:, k, t * 128:(t + 1) * 128], w[:, k, :], k == 0, k == KC - 1,
                                       ['mixT', wn], [PSR(bk)])
                                tm = tmp[ti % 2]
                                tmn = ('otmp', ti % 2)
                                ti += 1
                                TT(P, 'dve', tm[:], ps[:, bk, 0:256], gbc[:, pn * 256:(pn + 1) * 256], ALU.mult, [PSR(bk), 'gbc'], [tmn])
                                TT(P, 'pool', hbk[:, t, pn * 256:(pn + 1) * 256], hbk[:, t, pn * 256:(pn + 1) * 256], tm[:], ALU.add,
                                   [('hbk', t), tmn], [('hbk', t)])
                        for t in range(NTL):
                            DMA(P, 'sp', h1_scr.ap()[row0 + t * 128:row0 + (t + 1) * 128, :], hbk[:, t, :], [('hbk', t)], [('h1', l)], store=True)
                    P.barrier()
                    cut('p5')

                    with ExitStack() as sB:
                        fT = sb("m_fT", [128, KC, TBM], BF16, sB)
                        gTb = sb("m_gT", [32, TBM], F32, sB)
                        acc = sb("m_acc", [128, TBM // 128, D], F32, sB)
                        with ExitStack() as s1:
                            hb = sb("r_hb", [128, D], F32, s1)
                            junk = sb("r_junk", [128, D], BF16, s1)
                            dst_junk[0], dst_junk[1] = junk, 'junk'
                            ss = sb("r_ss", [128, 1], F32, s1)
                            rt = sb("r_rt", [128, 1], F32, s1)
                            f32T = sb("r_f32T", [128, KC, 128], F32, s1)
                            wr_t = sb("r_wr", [128, KC, 36], F32, s1)
                            lg = sb("r_lg", [128, 36], F32, s1)
                            sm = sb("r_sm", [128, 64], F32, s1)
                            oh = sb("r_oh", [128, 4], F32, s1)
                            lsel = sb("r_lsel", [128, 8], F32, s1)
                            l2 = sb("r_l2", [128, 8], F32, s1)
                            mk = sb("r_mk", [128, 16], F32, s1)
                            wi_ = sb("r_wi", [128, 8], F32, s1)
                            gts = sb("r_gts", [128, 32], F32, s1)
                            DMA(P, 'sp', wr_t[:, :, 0:4], w_rg.ap()[l].rearrange("(k p) x -> p k x", p=128), (), ['wr_t'])
                            DMA(P, 'sp', wr_t[:, :, 4:36], w_re.ap()[l].rearrange("(k p) x -> p k x", p=128), (), ['wr_t'])
                            av, shv = a2[:, l * 3 + j, :], mvec(l, j, 3)
                            for t in range(NTL):
                                def wrt(k, pss, psr, t=t, av=av, shv=shv):
                                    if False:
                                        ACTV(P, f32T[:, k, :], pss, AF.Identity, [psr, 'a2', 'modT'], [('f32T', k)],
                                             bias=shv[:, k:k + 1], scale=av[:, k:k + 1])
                                    else:
                                        TS(P, 'dve', f32T[:, k, :], pss, av[:, k:k + 1], shv[:, k:k + 1], ALU.mult, ALU.add,
                                           [psr, 'a2', 'modT'], [('f32T', k)])
                                    COPY(P, 'pool', fT[:, k, t * 128:(t + 1) * 128], f32T[:, k, :], [('f32T', k)], ['fT'])
                                norm_tile(l, j, h1_scr.ap()[row0 + t * 128:row0 + (t + 1) * 128, :], hb, 'hb', av, shv, (ss, rt), wrt,
                                          [('h1', l)])
                                for k in range(KC):
                                    MM(P, ps[:, 6, 0:36], f32T[:, k, :], wr_t[:, k, :], k == 0, False, [('f32T', k), 'wr_t'], [PSR(6)])
                                MM(P, ps[:, 6, 0:36], ones_f[0:1, :], brow[0:1, l, :], False, True, ['ones_f', 'brow'], [PSR(6)])
                                COPY(P, 'dve', lg[:], ps[:, 6, 0:36], [PSR(6)], ['lg'])
                                R = ['lg', 'sm', 'oh', 'lsel', 'l2', 'mk', 'wi', 'gts']
                                RMAX(P, sm[:, 0:1], lg[:, 0:4], R, R)
                                TS(P, 'dve', oh[:], lg[:, 0:4], sm[:, 0:1], None, ALU.is_equal, None, R, R)
                                TS(P, 'dve', sm[:, 1:2], sm[:, 0:1], -1.0, None, ALU.mult, None, R, R)
                                TS(P, 'dve', sm[:, 4:8], lg[:, 0:4], sm[:, 0:1], None, ALU.subtract, None, R, R)
                                ACTV(P, sm[:, 4:8], sm[:, 4:8], AF.Exp, R, R, accum=sm[:, 2:3])
                                RECIP(P, sm[:, 3:4], sm[:, 2:3], R, R)
                                TS(P, 'dve', lsel[:], lg[:, 4:12], oh[:, 0:1], None, ALU.mult, None, R, R)
                                for g in range(1, 4):
                                    STT(P, lsel[:], lg[:, 4 + 8 * g:12 + 8 * g], oh[:, g:g + 1], lsel[:], ALU.mult, ALU.add, R, R)
                                RMAX(P, sm[:, 8:9], lsel[:], R, R)
                                TS(P, 'dve', mk[:, 0:8], lsel[:], sm[:, 8:9], None, ALU.is_equal, None, R, R)
                                STT(P, l2[:], mk[:, 0:8], -1e30, lsel[:], ALU.mult, ALU.add, R, R)
                                RMAX(P, sm[:, 9:10], l2[:], R, R)
                                TS(P, 'dve', mk[:, 8:16], l2[:], sm[:, 9:10], None, ALU.is_equal, None, R, R)
                                TT(P, 'dve', sm[:, 10:11], sm[:, 9:10], sm[:, 8:9], ALU.subtract, R, R)
                                ACTV(P, sm[:, 11:12], sm[:, 10:11], AF.Exp, R, R)
                                TS(P, 'dve', sm[:, 12:13], sm[:, 11:12], 1.0, None, ALU.add, None, R, R)
                                RECIP(P, sm[:, 12:13], sm[:, 12:13], R, R)
                                TT(P, 'dve', sm[:, 13:14], sm[:, 12:13], sm[:, 3:4], ALU.mult, R, R)
                                TT(P, 'dve', sm[:, 14:15], sm[:, 13:14], sm[:, 11:12], ALU.mult, R, R)
                                TS(P, 'dve', wi_[:], mk[:, 0:8], sm[:, 13:14], None, ALU.mult, None, R, R)
                                STT(P, wi_[:], mk[:, 8:16], sm[:, 14:15], wi_[:], ALU.mult, ALU.add, R, R)
                                for g in range(4):
                                    TS(P, 'dve', gts[:, 8 * g:8 * g + 8], wi_[:], oh[:, g:g + 1], None, ALU.mult, None, R, R)
                                TR(P, ps[0:32, 7, 0:128], gts[:], ident[:], R + ['ident'], [PSR(7)])
                                ACTV(P, gTb[0:32, t * 128:(t + 1) * 128], ps[0:32, 7, 0:128], AF.Copy, [PSR(7)], ['gTb'])
                        P.barrier()
                        cut('p6')

                        with ExitStack() as s1:
                            NHALF = max(1, FC // 2)
                            FH = FC // NHALF
                            WH = FH * 128
                            ring = [sb("e_ring%d" % i, [128, 2048 * 4], BF16, s1) for i in range(3)]
                            hid = sb("e_hid", [128, FC, TBM], BF16, s1)
                            gbcs = [sb("e_gbc%d" % i, [128, TBM], F32, s1) for i in range(2)]
                            sg = [sb("e_sg%d" % i, [128, TBM], F32, s1) for i in range(2)]
                            tu = [sb("e_tu%d" % i, [128, TBM], F32, s1) for i in range(2)]
                            sel_e = [sb("e_sel%d" % i, [32, 128], F32, s1) for i in range(2)]
                            ri = 0
                            ei = 0
                            for e_ in range(NE):
                                se = sel_e[e_ % 2]
                                sen = ('sel_e', e_ % 2)
                                gb_ = gbcs[e_ % 2]
                                gbn = ('gbcs', e_ % 2)
                                TS(P, 'dve', se[:], ones_f[0:32, :], ident[0:32, e_:e_ + 1], None, ALU.mult, None, ['ones_f', 'ident'], [sen])
                                MM(P, ps[:, 6, 0:n], se[:], gTb[0:32, 0:n], True, True, [sen, 'gTb'], [PSR(6)])
                                ACTV(P, gb_[:, 0:n], ps[:, 6, 0:n], AF.Copy, [PSR(6)], [gbn])
                                for hf in range(NHALF):
                                    wts = []
                                    for (wv, nm) in ((wgt, 'gate'), (wup, 'up')):
                                        rg_ = ring[ri % 3]
                                        rn = ('ring', ri % 3)
                                        ri += 1
                                        wt = rg_[:, 0:KC * WH].rearrange("p (k x) -> p k x", k=KC)
                                        DMA(P, 'sp', wt, wv[e_ * D:(e_ + 1) * D, hf * WH:(hf + 1) * WH].rearrange("(k p) x -> p k x", p=128),
                                            [('wg', nm, l)], [rn])
                                        wts.append((wt, rn))
                                    for fi in range(FH):
                                        for k in range(KC):
                                            MM(P, ps[:, fi, 0:n], wts[0][0][:, k, fi * 128:(fi + 1) * 128], fT[:, k, 0:n], k == 0, k == KC - 1,
                                               [wts[0][1], 'fT'], [PSR(fi)])
                                    for fi in range(FH):
                                        for k in range(KC):
                                            MM(P, ps[:, 2 + fi, 0:n], wts[1][0][:, k, fi * 128:(fi + 1) * 128], fT[:, k, 0:n], k == 0, k == KC - 1,
                                               [wts[1][1], 'fT'], [PSR(2 + fi)])
                                    for fi in range(FH):
                                        s_ = sg[ei % 2]
                                        sn = ('sg', ei % 2)
                                        u_ = tu[ei % 2]
                                        un = ('tu', ei % 2)
                                        ei += 1
                                        ACTV(P, s_[:, 0:n], ps[:, fi, 0:n], AF.Silu, [PSR(fi)], [sn])
                                        TT(P, 'dve', u_[:, 0:n], ps[:, 2 + fi, 0:n], gb_[:, 0:n], ALU.mult, [PSR(2 + fi), gbn], [un])
                                        TT(P, 'pool', hid[:, hf * FH + fi, 0:n], s_[:, 0:n], u_[:, 0:n], ALU.mult, [sn, un], ['hid'])
                                DCH = 512
                                for d0 in range(0, D, 2048):
                                    dw = min(2048, D - d0)
                                    rg_ = ring[ri % 3]
                                    rn = ('ring', ri % 3)
                                    ri += 1
                                    wd = rg_[:, 0:FC * dw].rearrange("p (f x) -> p f x", f=FC)
                                    DMA(P, 'sp', wd, wdn[e_ * c.FF:(e_ + 1) * c.FF, d0:d0 + dw].rearrange("(f p) x -> p f x", p=128),
                                        [('wg', 'down', l)], [rn])
                                    oi = 0
                                    for t in range(NTL):
                                        for dc in range(0, dw, DCH):
                                            bk = 4 + oi % 2
                                            oi += 1
                                            for f in range(FC):
                                                MM(P, ps[:, bk, 0:DCH], hid[:, f, t * 128:(t + 1) * 128], wd[:, f, dc:dc + DCH], f == 0, f == FC - 1,
                                                   ['hid', rn], [PSR(bk)])
                                            a_ = acc[:, t, d0 + dc:d0 + dc + DCH]
                                            if e_ == 0:
                                                COPY(P, 'dve', a_, ps[:, bk, 0:DCH], [PSR(bk)], [('acc', t)])
                                            else:
                                                TT(P, 'dve', a_, ps[:, bk, 0:DCH], a_, ALU.add, [PSR(bk), ('acc', t)], [('acc', t)])
                        P.barrier()
                        cut('moe')

                        with ExitStack() as s1:
                            hb2 = [sb("f_hb%d" % i, [128, D], F32, s1) for i in range(2)]
                            gbc = sb("f_gbc", [128, D], F32, s1)
                            gfb = sb("f_gfb", [128, D], F32, s1)
                            dtmp = sb("f_dtmp", [128, 128], F32, s1)
                            junk = sb("f_junk", [128, D], BF16, s1)
                            ss = sb("f_ss", [128, 1], F32, s1)
                            rt = sb("f_rt", [128, 1], F32, s1)
                            bcast_cols(mvec(l, j, 5), gbc, ['modT'], ['gbc'], dtmp, 'dtmp')
                            if last:
                                bcast_cols(gfin_c, gfb, ['gfin'], ['gfb'], dtmp, 'dtmp')
                            for t in range(NTL):
                                hb = hb2[t % 2]
                                hbn = ('hb2', t % 2)
                                DMA(P, 'sp', hb[:], h1_scr.ap()[row0 + t * 128:row0 + (t + 1) * 128, :], [('h1', l)], [hbn])
                                TT(P, 'pool', acc[:, t, :], acc[:, t, :], gbc[:], ALU.mult, [('acc', t), 'gbc'], [('acc', t)])
                                TT(P, 'dve', hb[:], hb[:], acc[:, t, :], ALU.add, [hbn, ('acc', t)], [hbn])
                                if not last:
                                    DMA(P, 'sp', h_scr.ap()[row0 + t * 128:row0 + (t + 1) * 128, :], hb[:], [hbn], [('h', l + 1)], store=True)
                                else:
                                    ACTV(P, junk[:], hb[:], AF.Square, [hbn], ['junk', 'ss'], accum=ss[:])
                                    TS(P, 'dve', rt[:], ss[:], 1.0 / D, EPS, ALU.mult, ALU.add, ['ss'], ['rt'])
                                    ACTV(P, rt[:], rt[:], AF.Ln, ['rt'], ['rt'])
                                    RSQ(P, rt[:], ['rt'], ['rt'])
                                    STT(P, hb[:], hb[:], rt[:, 0:1], gfb[:], ALU.mult, ALU.mult, [hbn, 'rt', 'gfb'], [hbn])
                                    DMA(P, 'sp', out_sh.ap()[row0 + t * 128:row0 + (t + 1) * 128, :], hb[:], [hbn], ['out'], store=True)
                        P.barrier()
                        cut('p7')
                        if is_ctx and b == 1:
                            cut('l%d' % l)

        except StopBuild:
            pass
        P.dead = False
        P.barrier(full=True)
        P.emit(nc, es)
        nc_ctx.__exit__(None, None, None)
    return nc, P


def host_inputs(c, inp):
    L, D, NTh, CTX = c.L, c.D, c.NTh, c.CTX
    f = lambda a: np.ascontiguousarray(np.asarray(a, dtype=np.float32))
    x = f(inp['x'])
    ctx = f(inp['ctx']).reshape(2 * CTX, D)
    cvec = np.concatenate([f(inp['c']), f(inp['c_ctx'])[None, :]], axis=0)
    ident = np.eye(128, dtype=np.float32)
    pm = np.zeros((128, 128), np.float32)
    for i in range(32):
        pm[32 + i, i] = -1.0
        pm[i, 32 + i] = 1.0
        pm[96 + i, 64 + i] = -1.0
        pm[64 + i, 96 + i] = 1.0
    w_gate = f(inp['w_gate']).reshape(L, 32, D, c.FF)
    w_up = f(inp['w_up']).reshape(L, 32, D, c.FF)
    w_down = f(inp['w_down']).reshape(L, 32, c.FF, D)
    w_in, w_out, w_ada, b_ada = f(inp['w_in']), f(inp['w_out']), f(inp['w_ada']), f(inp['b_ada'])
    axis_dim = 64
    inv = (10000.0 ** (-np.arange(0, axis_dim, 2, dtype=np.float32) / axis_dim)).astype(np.float32)
    maps = []
    for i in range(8):
        pos = np.arange(i * NTh, (i + 1) * NTh)
        row = (pos // c.GRID_W).astype(np.float32)
        col = (pos % c.GRID_W).astype(np.float32)
        ang_r = row[:, None] * inv[None, :]
        ang_c = col[:, None] * inv[None, :]
        ang = np.concatenate([ang_r, ang_r, ang_c, ang_c], axis=-1).astype(np.float32)
        sel = np.zeros((128, 16), np.float32)
        if i > 0:
            sel[:, i - 1] = 1.0
        if i < 7:
            sel[:, 8 + i + 1] = 1.0
        m = {
            'x_sh': np.ascontiguousarray(x[:, i * NTh:(i + 1) * NTh, :].reshape(2 * NTh, D)),
            'ctx_in': ctx, 'cvec': cvec, 'selLR': sel,
            'w_ada_sh': np.ascontiguousarray(w_ada[:, :, i * c.NS:(i + 1) * c.NS]),
            'b_ada_sh': np.ascontiguousarray(b_ada[:, i * c.NS:(i + 1) * c.NS]),
            'g_mix': f(inp['g_mix']), 'g_ffn': f(inp['g_ffn']), 'g_final': f(inp['g_final']),
            'w_in_sh': np.ascontiguousarray(w_in[:, i * (D // 8):(i + 1) * (D // 8), :]),
            'w_out_sh': np.ascontiguousarray(w_out[:, i * (D // 8):(i + 1) * (D // 8), :]),
            'w_gate_sh': np.ascontiguousarray(w_gate[:, 4 * i:4 * i + 4].reshape(L, 4 * D, c.FF)),
            'w_up_sh': np.ascontiguousarray(w_up[:, 4 * i:4 * i + 4].reshape(L, 4 * D, c.FF)),
            'w_down_sh': np.ascontiguousarray(w_down[:, 4 * i:4 * i + 4].reshape(L, 4 * c.FF, D)),
            'sc_w': f(inp['short_conv_w']), 'cf_w': f(inp['cfm_conv_w']), 'cf_b': f(inp['cfm_conv_b']),
            'ln_g': f(inp['cfm_ln_g']), 'ln_b': f(inp['cfm_ln_b']), 'lam_qk': f(inp['lam_qk']),
            'subln_g': f(inp['subln_g']), 'w_rg': f(inp['w_route_group']), 'b_rg': f(inp['b_route_group']),
            'w_re': f(inp['w_route_expert']), 'b_re': f(inp['b_route_expert']),
            'cosT': np.ascontiguousarray(np.cos(ang).T.astype(np.float32)),
            'sinT': np.ascontiguousarray(np.sin(ang).T.astype(np.float32)),
            'ident': ident, 'pm': pm,
        }
        maps.append(m)
    return maps


def run_cfg(c, inp, trace=False):
    nc, P = build(c)
    maps = host_inputs(c, inp)
    res = run_bass_kernel_spmd(nc, maps, core_ids=list(range(8)), trace=trace)
    out = np.empty((2, c.SEQ, c.D), np.float32)
    for i in range(8):
        o = res.results[i]['out_sh'].reshape(2, c.NTh, c.D)
        out[:, i * c.NTh:(i + 1) * c.NTh, :] = o
    return out, res


def kernel(**inputs):
    c = Cfg(4096, 8192)
    out, _ = run_cfg(c, inputs)
    return out
```

```python
import math
from contextlib import ExitStack
import numpy as np
import concourse.bass as bass
import concourse.mybir as mybir
from concourse.bass_utils import run_bass_kernel_spmd

F32 = mybir.dt.float32
BF16 = mybir.dt.bfloat16
ALU = mybir.AluOpType
AF = mybir.ActivationFunctionType
EPS = 1e-6
ENGS = ('pe', 'act', 'dve', 'pool', 'sp')


class Cfg:
    def __init__(self, D, SEQ, CTX=256, L=2):
        self.D, self.SEQ, self.CTX, self.L = D, SEQ, CTX, L
        self.NC = 8
        self.KC = D // 128
        self.NTh = SEQ // 8
        self.NT = 2 * self.NTh
        self.NTOT = self.NT + 2 * CTX
        self.CONV_W = D // 4
        self.CG = self.CONV_W // 128
        self.ATT_W = D // 2
        self.H = self.ATT_W // 256
        self.H2 = 2 * self.H
        self.CFM_W = D - self.CONV_W - self.ATT_W
        self.QK_W = self.H * 256
        self.OFF_Q = 3 * self.CONV_W
        self.OFF_K = self.OFF_Q + self.QK_W
        self.OFF_V = self.OFF_K + self.QK_W
        self.OFF_C = self.OFF_V + self.ATT_W
        self.PROJ_W = self.OFF_C + 2 * self.CFM_W
        self.FF = D // 8
        self.FC = self.FF // 128
        self.NE = 32
        self.NS = 6 * D // 8
        self.NSC = self.NS // 128
        self.TB = min(512, self.NTh)
        self.PW = 256
        self.GRID_W = 64
        self.KVN = self.H2 * 128 * self.NT + self.NT * self.ATT_W


class StopBuild(Exception):
    pass


class Prog:
    CH = 16000
    K = 8
    PU = 1000

    def __init__(self):
        self.ops = []
        self.lw = {}
        self.rd = {}
        self.last_on = {}
        self.pending_st = []
        self.dead = False

    def op(self, eng, fn, reads=(), writes=(), kind='c', extra=()):
        if self.dead:
            return None
        i = len(self.ops)
        deps = set(extra)
        for r in reads:
            w = self.lw.get(r)
            if w:
                deps.update(w)
        for r in writes:
            rr = self.rd.get(r)
            if rr and (rr[0] or rr[1]):
                deps.update(rr[0].values())
                deps.update(rr[1])
        for r in reads:
            rr = self.rd.get(r)
            if rr is None:
                rr = self.rd[r] = ({}, [])
            if kind == 'c':
                rr[0][eng] = i
            else:
                rr[1].append(i)
        for r in writes:
            rr = self.rd.get(r)
            if rr and (rr[0] or rr[1]):
                self.lw[r] = [i]
                self.rd[r] = ({}, [])
            else:
                self.lw.setdefault(r, []).append(i)
                if r in reads:
                    pass
        deps.discard(i)
        self.ops.append([eng, fn, kind, deps])
        self.last_on[(eng, kind)] = i
        return i

    def barrier(self, full=False):
        if self.dead:
            return
        if full:
            hasdep = set()
            for o in self.ops:
                hasdep.update(o[3])
            extra = {i for i, o in enumerate(self.ops) if o[2] in ('cc', 'd') and i not in hasdep}
            self.pending_st = list(set(self.pending_st) | extra)
        deps = {v for (eng, kind), v in self.last_on.items() if kind == 'c'} | set(self.pending_st)
        b = self.op('sp', None, extra=deps)
        for e in ('pe', 'act', 'dve', 'pool'):
            self.op(e, None, extra={b})
        self.pending_st = []

    def emit(self, nc, es):
        ops = self.ops
        n = len(ops)
        need = [False] * n
        for (eng, fn, kind, deps) in ops:
            for d in deps:
                de, _, dk, _ = ops[d]
                if dk == 'c' and kind == 'c' and de == 'pe' and eng == 'pe':
                    continue
                need[d] = True
        sig = [None] * n
        ccount = {e: 0 for e in ENGS}
        csems = {e: [] for e in ENGS}
        dcount = {'sp': 0, 'pool': 0}
        dsems = {'sp': [], 'pool': []}
        slot_prev = {}
        ncc = 0
        for i, (eng, fn, kind, deps) in enumerate(ops):
            if kind == 'c':
                if need[i]:
                    k = ccount[eng]
                    ccount[eng] += 1
                    ch = k // self.CH
                    while len(csems[eng]) <= ch:
                        csems[eng].append(es.enter_context(nc.semaphore("c_%s_%d" % (eng, len(csems[eng])))))
                    sig[i] = (csems[eng][ch], k % self.CH + 1, 1, ('c', eng, ch))
            elif kind == 'd':
                j = dcount[eng]
                dcount[eng] += 1
                s = j % self.K
                m = j // self.K
                pool = m // self.PU
                val = 16 * ((m % self.PU) + 1)
                while len(dsems[eng]) <= pool:
                    pi = len(dsems[eng])
                    dsems[eng].append([es.enter_context(nc.semaphore("d_%s_%d_%d" % (eng, pi, t)))
                                       for t in range(self.K)])
                sig[i] = (dsems[eng][pool][s], val, 16, ('d', eng, pool, s))
                prev = slot_prev.get((eng, pool, s))
                if prev is not None:
                    deps.add(prev)
                slot_prev[(eng, pool, s)] = i
            else:
                sem = es.enter_context(nc.semaphore("cc_%d" % ncc))
                ncc += 1
                sig[i] = (sem, 1, 1, ('cc', ncc))
        self.nsig = dict(ccount)
        self.ndma = dict(dcount)

        def run(engname, e):
            waited = {}
            cwait = {}
            for i, (eng, fn, kind, deps) in enumerate(ops):
                if eng != engname:
                    continue
                for d in sorted(deps):
                    sg = sig[d]
                    if sg is None:
                        continue
                    de, _, dk, _ = ops[d]
                    if dk == 'c' and kind == 'c' and de == 'pe' and eng == 'pe':
                        continue
                    sem, val, _, key = sg
                    if key[0] == 'c':
                        cw = cwait.get(key[1])
                        if cw is not None and (cw[0] > key[2] or (cw[0] == key[2] and cw[1] >= val)):
                            continue
                        cwait[key[1]] = (key[2], val)
                    else:
                        if waited.get(key, 0) >= val:
                            continue
                        waited[key] = val
                    e.wait_ge(sem, val)
                if fn is None:
                    if sig[i] is not None:
                        e.nop().then_inc(sig[i][0], sig[i][2])
                else:
                    ins = fn(e)
                    if sig[i] is not None:
                        if kind == 'cc':
                            ins.then_inc(sig[i][0])
                        else:
                            ins.then_inc(sig[i][0], sig[i][2])

        with nc.Block() as block:
            @block.tensor
            def _(e):
                run('pe', e)

            @block.vector
            def _(e):
                run('dve', e)

            @block.scalar
            def _(e):
                run('act', e)

            @block.gpsimd
            def _(e):
                run('pool', e)

            @block.sync
            def _(e):
                run('sp', e)


def MM(P, out, lhsT, rhs, start, stop, rd, wr):
    P.op('pe', lambda e: e.matmul(out, lhsT, rhs, start=start, stop=stop), rd, wr)


def TR(P, out, in_, ident, rd, wr):
    P.op('pe', lambda e: e.transpose(out, in_, ident), rd, wr)


def ACTV(P, out, in_, func, rd, wr, bias=None, scale=None, accum=None):
    kw = {}
    if bias is not None:
        kw['bias'] = bias
    if scale is not None:
        kw['scale'] = scale
    if accum is not None:
        kw['accum_out'] = accum
    P.op('act', lambda e: e.activation(out, in_, func, **kw), rd, wr)


def TS(P, eng, out, in0, s1, s2, op0, op1, rd, wr):
    if op1 is None:
        P.op(eng, lambda e: e.tensor_scalar(out, in0, s1, None, op0), rd, wr)
    else:
        P.op(eng, lambda e: e.tensor_scalar(out, in0, s1, s2, op0, op1), rd, wr)


def TT(P, eng, out, in0, in1, op, rd, wr):
    P.op(eng, lambda e: e.tensor_tensor(out, in0, in1, op), rd, wr)


def STT(P, out, in0, scalar, in1, op0, op1, rd, wr):
    P.op('dve', lambda e: e.scalar_tensor_tensor(out, in0, scalar, in1, op0, op1), rd, wr)


def RECIP(P, out, in_, rd, wr):
    ACTV(P, out, in_, AF.Ln, rd, wr)
    ACTV(P, out, out, AF.Exp, list(wr), wr, scale=-1.0)


def RSQ(P, out, rd, wr):
    ACTV(P, out, out, AF.Exp, rd, wr, scale=-0.5)


def RMAX(P, out, in_, rd, wr):
    P.op('dve', lambda e: e.reduce_max(out, in_, mybir.AxisListType.X), rd, wr)


def COPY(P, eng, out, in_, rd, wr):
    P.op(eng, lambda e: e.tensor_copy(out, in_), rd, wr)


def MEMSET(P, eng, ap, val, wr):
    P.op(eng, lambda e: e.memset(ap, val), (), wr)


def DMA(P, q, out, in_, rd, wr, store=False, **kw):
    i = P.op(q, lambda e: e.dma_start(out=out, in_=in_, **kw), rd, wr, kind='d')
    if store and i is not None:
        P.pending_st.append(i)
    return i


def flat(ap):
    nd = len(ap.shape)
    names = " ".join("a%d" % i for i in range(nd))
    return ap.rearrange("%s -> (%s)" % (names, names))


def build(c):
    nc = bass.Bass("TRN2", target_bir_lowering=False)
    P = Prog()
    D, KC, L, NT, NTh, NTOT, CTX = c.D, c.KC, c.L, c.NT, c.NTh, c.NTOT, c.CTX
    CG, H, H2, TB, PW, FC, NE = c.CG, c.H, c.H2, c.TB, c.PW, c.FC, c.NE
    NSC = c.NSC
    TBM = max(TB, CTX)

    def din(name, shape, dt=F32):
        return nc.dram_tensor(name, list(shape), dt, kind="ExternalInput")

    def dscr(name, shape, dt):
        return nc.dram_tensor(name, list(shape), dt, kind="Internal")

    x_sh = din("x_sh", [NT, D])
    ctx_in = din("ctx_in", [2 * CTX, D])
    cvec = din("cvec", [3, D])
    selLR = din("selLR", [128, 16])
    w_ada_sh = din("w_ada_sh", [L, D, c.NS])
    b_ada_sh = din("b_ada_sh", [L, c.NS])
    g_mix = din("g_mix", [L, D])
    g_ffn = din("g_ffn", [L, D])
    g_final = din("g_final", [D])
    w_in_sh = din("w_in_sh", [L, D // 8, c.PROJ_W])
    w_out_sh = din("w_out_sh", [L, D // 8, D])
    w_gate_sh = din("w_gate_sh", [L, 4 * D, c.FF])
    w_up_sh = din("w_up_sh", [L, 4 * D, c.FF])
    w_down_sh = din("w_down_sh", [L, 4 * c.FF, D])
    sc_w = din("sc_w", [L, 3, c.CONV_W])
    cf_w = din("cf_w", [L, 31, c.CFM_W])
    cf_b = din("cf_b", [L, c.CFM_W])
    ln_g = din("ln_g", [L, c.CFM_W])
    ln_b = din("ln_b", [L, c.CFM_W])
    lam_qk = din("lam_qk", [L, 4, 128])
    subln_g = din("subln_g", [L, 256])
    w_rg = din("w_rg", [L, D, 4])
    b_rg = din("b_rg", [L, 4])
    w_re = din("w_re", [L, D, 32])
    b_re = din("b_re", [L, 32])
    cosT_d = din("cosT", [128, NTh])
    sinT_d = din("sinT", [128, NTh])
    ident_d = din("ident", [128, 128])
    pm_d = din("pm", [128, 128])
    out_sh = nc.dram_tensor("out_sh", [NT, D], F32, kind="ExternalOutput")

    wspec = {'in': (w_in_sh, (D // 8) * c.PROJ_W), 'out': (w_out_sh, (D // 8) * D),
             'gate': (w_gate_sh, 4 * D * c.FF), 'up': (w_up_sh, 4 * D * c.FF), 'down': (w_down_sh, 4 * c.FF * D)}
    wl = {}
    wg = {}
    for nm, (src, nel) in wspec.items():
        for l in range(L):
            wl[(nm, l)] = dscr("wl_%s_%d" % (nm, l), [nel // 2048, 2048], BF16)
            wg[(nm, l)] = dscr("wg_%s_%d" % (nm, l), [8 * nel // 2048, 2048], BF16)
    h_scr = dscr("h_scr", [NTOT, D], F32)
    h1_scr = dscr("h1_scr", [NTOT, D], F32)
    s_scr = dscr("s_scr", [c.CONV_W, NTOT], F32)
    gb_scr = dscr("gb_scr", [c.CONV_W, NTOT], F32)
    g_scr = dscr("g_scr", [c.CFM_W, NTOT], F32)
    q_scr = dscr("q_scr", [H2 * 128, NTOT], BF16)
    kv_loc = dscr("kv_loc", [c.KVN // 2048, 2048], BF16)
    kvg = dscr("kvg", [8 * c.KVN // 2048, 2048], BF16)
    kc_scr = dscr("kc_scr", [H2 * 128, 2 * CTX], BF16)
    vc_scr = dscr("vc_scr", [2 * CTX, c.ATT_W], BF16)
    halo_loc = dscr("halo_loc", [c.CFM_W, 128], F32)
    halo_g = dscr("halo_g", [8 * c.CFM_W, 128], F32)
    mix_scr = dscr("mix_scr", [D, NTOT], BF16)
    MODN = L * 3 * 128 * NSC
    mod_loc = dscr("mod_loc", [1, MODN], F32)
    mod_all = dscr("mod_all", [8, MODN], F32)

    NK = H2 * 128 * NT
    kvl_flat = flat(kv_loc.ap())
    k_loc = kvl_flat[0:NK].rearrange("(r t) -> r t", t=NT)
    v_loc = kvl_flat[NK:c.KVN].rearrange("(t a) -> t a", a=c.ATT_W)
    kvg_flat = flat(kvg.ap())

    def kg_rank(r):
        return kvg_flat[r * c.KVN:r * c.KVN + NK].rearrange("(r t) -> r t", t=NT)

    def vg_rank(r):
        return kvg_flat[r * c.KVN + NK:(r + 1) * c.KVN].rearrange("(t a) -> t a", a=c.ATT_W)

    def wview(nm, l, cols):
        return flat(wg[(nm, l)].ap()).rearrange("(r x) -> r x", x=cols)

    RG = [list(range(8))]

    def AG(src_ap, dst_ap, rd, wr):
        P.op('pool', lambda e: e.collective_compute("AllGather", ALU.bypass, replica_groups=RG,
                                                    ins=[src_ap], outs=[dst_ap]), rd, wr, kind='cc')

    blocks = []
    for b in range(2):
        for k in range(NTh // TB):
            blocks.append((b * NTh + k * TB, TB, b, b, False, k * TB))
    for b in range(2):
        blocks.append((NT + b * CTX, CTX, 2, b, True, 0))

    cur_l = [0]
    hits = [0]

    def cut(name):
        st = getattr(c, 'stop', None)
        if st is None or P.dead:
            return
        sl = 0
        skip = 0
        if '#' in st:
            st, skip = st.split('#')
            skip = int(skip)
        if '@' in st:
            st, sl = st.split('@')
            sl = int(sl)
        if st == name and cur_l[0] == sl:
            hits[0] += 1
        if st == name and cur_l[0] == sl and hits[0] > skip:
            P.barrier()
            P.dead = True

    es = ExitStack()
    with es:
        uid = [0]

        def sb(name, shape, dt, stack=None):
            uid[0] += 1
            return (stack or es).enter_context(nc.sbuf_tensor("s%d_%s" % (uid[0], name), list(shape), dt))

        ps = es.enter_context(nc.psum_tensor("ps", [128, 8, 512], F32))

        def PSR(b):
            return ('ps', b)

        ident = sb("ident", [128, 128], F32)
        pm_f = sb("pm_f", [128, 128], F32)
        pm_b = sb("pm_b", [128, 128], BF16)
        ones_f = sb("ones_f", [128, 128], F32)
        ones_b = sb("ones_b", [128, 128], BF16)
        eps_t = sb("eps_t", [128, 1], F32)
        cosT = sb("cosT_s", [128, NTh], F32)
        sinT = sb("sinT_s", [128, NTh], F32)
        sel_t = sb("sel_t", [128, 16], F32)
        gmix_c = sb("gmix_c", [128, L, KC], F32)
        gffn_c = sb("gffn_c", [128, L, KC], F32)
        gfin_c = sb("gfin_c", [128, KC], F32)
        wsc_c = sb("wsc_c", [128, L, CG, 3], F32)
        wcf_c = sb("wcf_c", [128, L, CG, 31], F32)
        bcf_c = sb("bcf_c", [128, L, CG], F32)
        lng_c = sb("lng_c", [128, L, CG], F32)
        lnb_c = sb("lnb_c", [128, L, CG], F32)
        subg_c = sb("subg_c", [128, L, 2], F32)
        lam_c = sb("lam_c", [128, L, 4], F32)
        lamp = sb("lamp", [128, L, 2], F32)
        neglam = sb("neglam", [128, L], F32)
        modT = sb("modT", [128, L * 3, 6 * KC], F32)
        a1 = sb("a1", [128, L * 3, KC], F32)
        a2 = sb("a2", [128, L * 3, KC], F32)
        brow = sb("brow", [1, L, 36], F32)
        zpad = sb("zpad", [128, 60], F32)

        nc_ctx = nc.allow_non_contiguous_dma(reason="small column-layout parameter loads")
        nc_ctx.__enter__()
        try:

            DMA(P, 'sp', ident[:], ident_d.ap(), (), ['ident'])
            DMA(P, 'sp', pm_f[:], pm_d.ap(), (), ['pm_f'])
            COPY(P, 'dve', pm_b[:], pm_f[:], ['pm_f'], ['pm_b'])
            MEMSET(P, 'dve', ones_f[:], 1.0, ['ones_f'])
            MEMSET(P, 'dve', ones_b[:], 1.0, ['ones_b'])
            MEMSET(P, 'dve', eps_t[:], EPS, ['eps_t'])
            DMA(P, 'sp', cosT[:], cosT_d.ap(), (), ['cosT'])
            DMA(P, 'sp', sinT[:], sinT_d.ap(), (), ['sinT'])
            DMA(P, 'sp', sel_t[:], selLR.ap(), (), ['sel'])
            for l in range(L):
                DMA(P, 'sp', gmix_c[:, l, :], g_mix.ap()[l].rearrange("(k p) -> p k", p=128), (), ['gmix'])
                DMA(P, 'sp', gffn_c[:, l, :], g_ffn.ap()[l].rearrange("(k p) -> p k", p=128), (), ['gffn'])
                for g in range(CG):
                    DMA(P, 'sp', wsc_c[:, l, g, :], sc_w.ap()[l][:, g * 128:(g + 1) * 128].rearrange("k p -> p k"), (), ['wsc'])
                    DMA(P, 'sp', wcf_c[:, l, g, :], cf_w.ap()[l][:, g * 128:(g + 1) * 128].rearrange("k p -> p k"), (), ['wcf'])
                DMA(P, 'sp', bcf_c[:, l, :], cf_b.ap()[l].rearrange("(g p) -> p g", p=128), (), ['bcf'])
                DMA(P, 'sp', lng_c[:, l, :], ln_g.ap()[l].rearrange("(g p) -> p g", p=128), (), ['lng'])
                DMA(P, 'sp', lnb_c[:, l, :], ln_b.ap()[l].rearrange("(g p) -> p g", p=128), (), ['lnb'])
                DMA(P, 'sp', subg_c[:, l, :], subln_g.ap()[l].rearrange("(g p) -> p g", p=128), (), ['subg'])
                DMA(P, 'sp', lam_c[:, l, :], lam_qk.ap()[l].rearrange("k p -> p k"), (), ['lamc'])
                DMA(P, 'sp', brow[0:1, l, 0:4], b_rg.ap()[l:l + 1, :], (), ['brow'])
                DMA(P, 'sp', brow[0:1, l, 4:36], b_re.ap()[l:l + 1, :], (), ['brow'])
            DMA(P, 'sp', gfin_c[:], g_final.ap().rearrange("(k p) -> p k", p=128), (), ['gfin'])

            lam_init = [0.8 - 0.6 * math.exp(-0.3 * l) for l in range(L)]
            for l in range(L):
                TT(P, 'dve', lamp[:, l, 0:1], lam_c[:, l, 0:1], lam_c[:, l, 1:2], ALU.mult, ['lamc'], ['lamp'])
                TT(P, 'dve', lamp[:, l, 1:2], lam_c[:, l, 2:3], lam_c[:, l, 3:4], ALU.mult, ['lamc', 'lamp'], ['lamp'])
            lamflat = lamp[:].rearrange("p l k -> p (l k)")
            MM(P, ps[:, 7, 0:2 * L], ones_f[:], lamflat, True, True, ['ones_f', 'lamp'], [PSR(7)])
            ACTV(P, lamp[:].rearrange("p l k -> p (l k)"), ps[:, 7, 0:2 * L], AF.Exp, [PSR(7)], ['lamp'])
            for l in range(L):
                STT(P, neglam[:, l:l + 1], lamp[:, l, 1:2], -lam_init[l], lamp[:, l, 0:1], ALU.add, ALU.subtract,
                    ['lamp'], ['neglam'])
                TS(P, 'dve', subg_c[:, l, :], subg_c[:, l, :], 1.0 - lam_init[l], None, ALU.mult, None, ['subg'], ['subg'])

            for l in range(L):
                for nm in ('in', 'out', 'gate', 'up', 'down'):
                    src, nel = wspec[nm]
                    sv = flat(src.ap()[l]).rearrange("(a x) -> a x", x=2048)
                    DMA(P, 'pool', wl[(nm, l)].ap(), sv, (), [('wl', nm, l)])

            def gather_w(nm, l):
                AG(wl[(nm, l)].ap(), wg[(nm, l)].ap(), [('wl', nm, l)], [('wg', nm, l)])

            with ExitStack() as s1:
                crow = sb("crow", [3, D], F32, s1)
                cT = sb("cT", [128, KC, 4], F32, s1)
                modl = sb("modl", [128, L * 3, NSC], F32, s1)
                bcol = sb("bcol", [128, L, NSC], F32, s1)
                wad = [sb("wad%d" % i, [128, KC, 256], F32, s1) for i in range(2)]
                DMA(P, 'sp', crow[:], cvec.ap(), (), ['crow'])
                ACTV(P, crow[:], crow[:], AF.Silu, ['crow'], ['crow'])
                for l in range(L):
                    DMA(P, 'sp', bcol[:, l, :], b_ada_sh.ap()[l].rearrange("(n p) -> p n", p=128), (), ['bcol'])
                for k in range(KC):
                    TR(P, ps[:, 7, 0:3], crow[0:3, k * 128:(k + 1) * 128], ident[0:3, 0:3], ['crow', 'ident'], [PSR(7)])
                    COPY(P, 'dve', cT[:, k, 0:3], ps[:, 7, 0:3], [PSR(7)], ['cT'])
                npan = c.NS // 256
                pi = 0
                for l in range(L):
                    for pn in range(npan):
                        wt = wad[pi % 2]
                        wr_ = ('wad', pi % 2)
                        pi += 1
                        DMA(P, 'sp', wt[:], w_ada_sh.ap()[l][:, pn * 256:(pn + 1) * 256].rearrange("(k p) x -> p k x", p=128),
                            (), [wr_])
                        for j2 in range(2):
                            n = pn * 2 + j2
                            bk = 4 + (n % 2)
                            for k in range(KC):
                                MM(P, ps[:, bk, 0:3], wt[:, k, j2 * 128:(j2 + 1) * 128], cT[:, k, 0:3], k == 0, k == KC - 1,
                                   [wr_, 'cT'], [PSR(bk)])
                            TS(P, 'dve', modl[:, l * 3:(l + 1) * 3, n], ps[:, bk, 0:3], bcol[:, l, n:n + 1], None, ALU.add, None,
                               [PSR(bk), 'bcol'], ['modl'])
                for l in range(L):
                    dst = mod_loc.ap()[0, l * 3 * 128 * NSC:(l + 1) * 3 * 128 * NSC].rearrange("(j p n) -> p j n", j=3, p=128)
                    DMA(P, 'sp', dst, modl[:, l * 3:(l + 1) * 3, :], ['modl'], ['mod_loc'], store=True)
                AG(mod_loc.ap(), mod_all.ap(), ['mod_loc'], ['mod_all'])
                for l in range(L):
                    for j in range(3):
                        o0 = (l * 3 + j) * 128 * NSC
                        src = mod_all.ap()[:, o0:o0 + 128 * NSC].rearrange("r (p n) -> p r n", p=128)
                        DMA(P, 'sp', modT[:, l * 3 + j, :].rearrange("p (r n) -> p r n", r=8), src, ['mod_all'], ['modT'])
                for l in range(L):
                    for j in range(3):
                        i = l * 3 + j
                        TS(P, 'dve', a1[:, i, :], modT[:, i, KC:2 * KC], 1.0, None, ALU.add, None, ['modT'], ['a1'])
                        TT(P, 'dve', a1[:, i, :], a1[:, i, :], gmix_c[:, l, :], ALU.mult, ['a1', 'gmix'], ['a1'])
                        TS(P, 'dve', a2[:, i, :], modT[:, i, 4 * KC:5 * KC], 1.0, None, ALU.add, None, ['modT'], ['a2'])
                        TT(P, 'dve', a2[:, i, :], a2[:, i, :], gffn_c[:, l, :], ALU.mult, ['a2', 'gffn'], ['a2'])
            gather_w('in', 0)
            P.barrier()
            cut('ada')

            def mvec(l, j, which):
                return modT[:, l * 3 + j, which * KC:(which + 1) * KC]

            def bcast_cols(col_ap, dst, rd, wr, tmp, tmpname):
                for k0 in range(0, KC, 4):
                    for k in range(k0, min(KC, k0 + 4)):
                        TS(P, 'dve', tmp[:], ident[:], col_ap[:, k:k + 1], None, ALU.mult, None, ['ident'] + rd, [tmpname])
                        MM(P, ps[:, 7, (k - k0) * 128:(k - k0 + 1) * 128], ones_f[:], tmp[:], True, True,
                           ['ones_f', tmpname], [PSR(7)])
                    w = min(KC, k0 + 4) - k0
                    ACTV(P, dst[:, k0 * 128:(k0 + w) * 128], ps[:, 7, 0:w * 128], AF.Copy, [PSR(7)], wr)

            def hsrc(l, row0, n):
                if l == 0:
                    if row0 < NT:
                        return x_sh.ap()[row0:row0 + n, :]
                    return ctx_in.ap()[row0 - NT:row0 - NT + n, :]
                return h_scr.ap()[row0:row0 + n, :]

            def norm_tile(l, j, src_ap, hb, hbn, avec, shvec, small, dst_writer, extra_rd):
                DMA(P, 'sp', hb[:], src_ap, extra_rd, [hbn])
                ss, rt = small
                cut('n1')
                ACTV(P, dst_junk[0][:], hb[:], AF.Square, [hbn], [dst_junk[1], 'ss'], accum=ss[:])
                cut('n2')
                TS(P, 'dve', rt[:], ss[:], 1.0 / D, EPS, ALU.mult, ALU.add, ['ss'], ['rt'])
                ACTV(P, rt[:], rt[:], AF.Ln, ['rt'], ['rt'])
                cut('n3')
                RSQ(P, rt[:], ['rt'], ['rt'])
                cut('n4')
                TS(P, 'dve', hb[:], hb[:], rt[:, 0:1], None, ALU.mult, None, [hbn, 'rt'], [hbn])
                cut('n5')
                for k0 in range(0, KC, 4):
                    bk = 4 + (k0 // 4) % 2
                    for k in range(k0, k0 + 4):
                        TR(P, ps[:, bk, (k - k0) * 128:(k - k0 + 1) * 128], hb[:, k * 128:(k + 1) * 128], ident[:],
                           [hbn, 'ident'], [PSR(bk)])
                    cut('n6')
                    for k in range(k0, k0 + 4):
                        dst_writer(k, ps[:, bk, (k - k0) * 128:(k - k0 + 1) * 128], PSR(bk))
                        cut('n7')

            dst_junk = [None, None]

            for l in range(L):
                last = (l == L - 1)
                cur_l[0] = l
                with ExitStack() as s1:
                    hb = sb("p1_hb", [128, D], F32, s1)
                    junk = sb("p1_junk", [128, D], BF16, s1)
                    dst_junk[0], dst_junk[1] = junk, 'junk'
                    ss = sb("p1_ss", [128, 1], F32, s1)
                    rt = sb("p1_rt", [128, 1], F32, s1)
                    uT = sb("p1_uT", [128, KC, TBM], BF16, s1)
                    wr_ring = [sb("p1_w%d" % i, [128, KC, PW], BF16, s1) for i in range(3)]
                    xs = sb("p1_xs", [128, CG, TBM], F32, s1)
                    st32 = [sb("p1_st32_%d" % i, [128, 2, TBM], F32, s1) for i in range(2)]
                    st16 = [sb("p1_st16_%d" % i, [128, 2, TBM], BF16, s1) for i in range(2)]
                    stv = [sb("p1_stv_%d" % i, [128, PW], BF16, s1) for i in range(2)]
                    qs = sb("p1_qs", [128, 2, TBM], BF16, s1)
                    t1 = sb("p1_t1", [128, TBM], F32, s1)
                    t2 = sb("p1_t2", [128, TBM], F32, s1)
                    win = wview('in', l, c.PROJ_W)
                    wi = 0
                    s32i = 0
                    s16i = 0
                    svi = 0
                    for bi_, (row0, n, j, b, is_ctx, tok0) in enumerate(blocks):
                        cut('p1b%d' % bi_)
                        if is_ctx and last:
                            segs = [('k', c.OFF_K, c.QK_W), ('v', c.OFF_V, c.ATT_W)]
                        else:
                            segs = [('xa', 0, c.CONV_W), ('gc', 2 * c.CONV_W, c.CONV_W), ('gb', c.CONV_W, c.CONV_W),
                                    ('q', c.OFF_Q, c.QK_W), ('k', c.OFF_K, c.QK_W),
                                    ('ga', c.OFF_C, c.CFM_W), ('gg', c.OFF_C + c.CFM_W, c.CFM_W), ('v', c.OFF_V, c.ATT_W)]
                        av, shv = a1[:, l * 3 + j, :], mvec(l, j, 0)
                        for t in range(n // 128):
                            def wrt(k, pss, psr, t=t, av=av, shv=shv):
                                if False:
                                    ACTV(P, uT[:, k, t * 128:(t + 1) * 128], pss, AF.Identity, [psr, 'a1', 'modT'], ['uT'],
                                         bias=shv[:, k:k + 1], scale=av[:, k:k + 1])
                                else:
                                    TS(P, 'dve', uT[:, k, t * 128:(t + 1) * 128], pss, av[:, k:k + 1], shv[:, k:k + 1],
                                       ALU.mult, ALU.add, [psr, 'a1', 'modT'], ['uT'])
                            norm_tile(l, j, hsrc(l, row0 + t * 128, 128), hb, 'hb', av, shv, (ss, rt), wrt,
                                      [('h', l)])
                        cut('p1n')
                        for (kind, off, width) in segs:
                            cut('p1_' + kind)
                            for pn in range(width // PW):
                                col0 = off + pn * PW
                                w = wr_ring[wi % 3]
                                wn = ('p1w', wi % 3)
                                wi += 1
                                DMA(P, 'sp', w[:], win[:, col0:col0 + PW].rearrange("(k p) x -> p k x", p=128),
                                    [('wg', 'in', l)], [wn])
                                if kind == 'v':
                                    for t in range(n // 128):
                                        bk = t % 4
                                        for k in range(KC):
                                            MM(P, ps[:, bk, 0:PW], uT[:, k, t * 128:(t + 1) * 128], w[:, k, :], k == 0, k == KC - 1,
                                               ['uT', wn], [PSR(bk)])
                                        sv = st16[s16i % 2][:, 0, 0:PW]
                                        svn = ('st16', s16i % 2)
                                        s16i += 1
                                        if getattr(c, 'vx', '') != 'noevac':
                                            ACTV(P, sv, ps[:, bk, 0:PW], AF.Copy, [PSR(bk)], [svn])
                                        c0 = pn * PW
                                        if getattr(c, 'vx', '') in ('noevac', 'nostore'):
                                            pass
                                        elif is_ctx:
                                            dst = vc_scr.ap()[b * CTX + t * 128:b * CTX + (t + 1) * 128, c0:c0 + PW]
                                            DMA(P, 'sp', dst, sv, [svn], [('vc', l)], store=True)
                                        else:
                                            vx = getattr(c, 'vx', '')
                                            if vx == 'vc':
                                                dst = vc_scr.ap()[row0 + t * 128:row0 + (t + 1) * 128, c0:c0 + PW]
                                                DMA(P, 'sp', dst, sv, [svn], [('kvl', l)], store=True)
                                            elif vx == 'half':
                                                dst = v_loc[row0 + t * 128:row0 + (t + 1) * 128, c0:c0 + 128]
                                                DMA(P, 'sp', dst, sv[:, 0:128], [svn], [('kvl', l)], store=True)
                                            else:
                                                dst = v_loc[row0 + t * 128:row0 + (t + 1) * 128, c0:c0 + PW]
                                                DMA(P, 'sp', dst, sv, [svn], [('kvl', l)], store=True)
                                    continue
                                base_bk = 2 * (pn % 2)
                                for cc in range(2):
                                    bk = base_bk + cc
                                    for k in range(KC):
                                        MM(P, ps[:, bk, 0:n], w[:, k, cc * 128:(cc + 1) * 128], uT[:, k, 0:n], k == 0, k == KC - 1,
                                           ['uT', wn], [PSR(bk)])
                                gch = pn * 2
                                if kind == 'xa':
                                    for cc in range(2):
                                        ACTV(P, xs[:, gch + cc, 0:n], ps[:, base_bk + cc, 0:n], AF.Copy, [PSR(base_bk + cc)], ['xs'])
                                elif kind == 'ga':
                                    for cc in range(2):
                                        ACTV(P, xs[:, gch + cc, 0:n], ps[:, base_bk + cc, 0:n], AF.Copy, [PSR(base_bk + cc)], ['xs'])
                                elif kind == 'gg':
                                    st = st32[s32i % 2]
                                    stn = ('st32', s32i % 2)
                                    s32i += 1
                                    for cc in range(2):
                                        ACTV(P, st[:, cc, 0:n], ps[:, base_bk + cc, 0:n], AF.Exp, [PSR(base_bk + cc)], [stn], scale=-1.0)
                                        TS(P, 'dve', st[:, cc, 0:n], st[:, cc, 0:n], 1.0, None, ALU.add, None, [stn], [stn])
                                        RECIP(P, st[:, cc, 0:n], st[:, cc, 0:n], [stn], [stn])
                                        TT(P, 'dve', st[:, cc, 0:n], st[:, cc, 0:n], xs[:, gch + cc, 0:n], ALU.mult, [stn, 'xs'], [stn])
                                    for c2 in range(2):
                                        DMA(P, 'sp', g_scr.ap()[(gch + c2) * 128:(gch + c2 + 1) * 128, row0:row0 + n], st[:, c2, 0:n], [stn], [('ga', l)], store=True)
                                elif kind in ('gc', 'gb'):
                                    st = st32[s32i % 2]
                                    stn = ('st32', s32i % 2)
                                    s32i += 1
                                    for cc in range(2):
                                        if kind == 'gb':
                                            ACTV(P, st[:, cc, 0:n], ps[:, base_bk + cc, 0:n], AF.Copy, [PSR(base_bk + cc)], [stn])
                                        else:
                                            TT(P, 'dve', st[:, cc, 0:n], ps[:, base_bk + cc, 0:n], xs[:, gch + cc, 0:n], ALU.mult,
                                               [PSR(base_bk + cc), 'xs'], [stn])
                                    scr = {'gc': s_scr, 'gb': gb_scr}[kind]
                                    for c2 in range(2):
                                        DMA(P, 'sp', scr.ap()[(gch + c2) * 128:(gch + c2 + 1) * 128, row0:row0 + n], st[:, c2, 0:n], [stn], [(kind, l)], store=True)
                                else:
                                    st = st16[s16i % 2]
                                    stn = ('st16', s16i % 2)
                                    s16i += 1
                                    for cc in range(2):
                                        bk = base_bk + cc
                                        if is_ctx or getattr(c, 'norope', False):
                                            ACTV(P, st[:, cc, 0:n], ps[:, bk, 0:n], AF.Copy, [PSR(bk)], [stn])
                                            continue
                                        ACTV(P, qs[:, cc, 0:n], ps[:, bk, 0:n], AF.Copy, [PSR(bk)], ['qs'])
                                        rb = 6 + cc
                                        MM(P, ps[:, rb, 0:n], pm_b[:], qs[:, cc, 0:n], True, True, ['pm_b', 'qs'], [PSR(rb)])
                                        TT(P, 'dve', t2[:, 0:n], ps[:, rb, 0:n], sinT[:, tok0:tok0 + n], ALU.mult,
                                           [PSR(rb), 'sinT'], ['t2'])
                                        TT(P, 'dve', t1[:, 0:n], qs[:, cc, 0:n], cosT[:, tok0:tok0 + n], ALU.mult,
                                           ['qs', 'cosT'], ['t1'])
                                        TT(P, 'dve', st[:, cc, 0:n], t1[:, 0:n], t2[:, 0:n], ALU.add, ['t1', 't2'], [stn])
                                    if kind == 'q':
                                        for c2 in range(2):
                                            DMA(P, 'sp', q_scr.ap()[(gch + c2) * 128:(gch + c2 + 1) * 128, row0:row0 + n], st[:, c2, 0:n], [stn], [('q', l)], store=True)
                                    elif is_ctx:
                                        for c2 in range(2):
                                            DMA(P, 'sp', kc_scr.ap()[(gch + c2) * 128:(gch + c2 + 1) * 128, b * CTX:b * CTX + n], st[:, c2, 0:n], [stn], [('kc', l)], store=True)
                                    else:
                                        for c2 in range(2):
                                            DMA(P, 'sp', k_loc[(gch + c2) * 128:(gch + c2 + 1) * 128, row0:row0 + n], st[:, c2, 0:n], [stn], [('kvl', l)], store=True)
                P.barrier()
                cut('p1')

                AG(kv_loc.ap(), kvg.ap(), [('kvl', l)], [('kvg', l)])
                if l == 0:
                    MEMSET(P, 'dve', zpad[:], 0.0, ['zpad'])
                    for g in range(CG):
                        DMA(P, 'sp', halo_loc.ap()[g * 128:(g + 1) * 128, 68:128], zpad[:], ['zpad'], ['halo_loc'], store=True)
                for b in range(2):
                    hl = halo_loc.ap()
                    DMA(P, 'sp', hl[:, b * 34:b * 34 + 16], g_scr.ap()[:, b * NTh:b * NTh + 16], [('ga', l)], ['halo_loc'], store=True)
                    DMA(P, 'sp', hl[:, b * 34 + 16:b * 34 + 32], g_scr.ap()[:, (b + 1) * NTh - 16:(b + 1) * NTh], [('ga', l)],
                        ['halo_loc'], store=True)
                    DMA(P, 'sp', hl[:, b * 34 + 32:b * 34 + 33], s_scr.ap()[:, b * NTh:b * NTh + 1], [('gc', l)], ['halo_loc'], store=True)
                    DMA(P, 'sp', hl[:, b * 34 + 33:b * 34 + 34], s_scr.ap()[:, (b + 1) * NTh - 1:(b + 1) * NTh], [('gc', l)],
                        ['halo_loc'], store=True)
                AG(halo_loc.ap(), halo_g.ap(), ['halo_loc'], ['halo_g'])
                for nm in ('out', 'gate', 'up', 'down'):
                    gather_w(nm, l)
                if not last:
                    gather_w('in', l + 1)

                cut('xchg')
                with ExitStack() as s1:
                    SEGM = max(NTh, CTX)
                    CH = min(512, SEGM)
                    hal = sb("cv_hal", [128, CG, 8, 128], F32, s1)
                    hL = sb("cv_hL", [128, 2, CG, 16], F32, s1)
                    hR = sb("cv_hR", [128, 2, CG, 16], F32, s1)
                    sLR = sb("cv_sLR", [128, 2, CG, 2], F32, s1)
                    gbuf = [sb("cv_gbuf%d" % i, [128, SEGM + 32], F32, s1) for i in range(2)]
                    zall = sb("cv_z", [128, CG, SEGM], F32, s1)
                    zsq = [sb("cv_zsq%d" % i, [128, CH], F32, s1) for i in range(2)]
                    mu = sb("cv_mu", [128, CH], F32, s1)
                    msq = sb("cv_msq", [128, CH], F32, s1)
                    rs = sb("cv_rs", [128, CH], F32, s1)
                    tt = [sb("cv_tt%d" % i, [128, CH], F32, s1) for i in range(2)]
                    yst = [sb("cv_yst%d" % i, [128, 2, CH], BF16, s1) for i in range(2)]
                    sbuf_ = [sb("cv_s%d" % i, [128, SEGM + 32], F32, s1) for i in range(2)]
                    gbt = [sb("cv_gb%d" % i, [128, SEGM], F32, s1) for i in range(2)]
                    pa = [sb("cv_pa%d" % i, [128, SEGM], F32, s1) for i in range(2)]
                    pb = sb("cv_pb", [128, SEGM], F32, s1)
                    ysh = [sb("cv_ysh%d" % i, [128, 2, SEGM], BF16, s1) for i in range(2)]
                    for g in range(CG):
                        src = halo_g.ap().rearrange("(r x) y -> x r y", r=8)[g * 128:(g + 1) * 128]
                        DMA(P, 'sp', hal[:, g, :, :], src, ['halo_g'], ['hal'])
                    cut('cvl')
                    for b in range(2):
                        for r in range(8):
                            o = b * 34
                            if r == 0:
                                TS(P, 'dve', hL[:, b, :, 0:15], hal[:, :, r, o + 17:o + 32], sel_t[:, r:r + 1], None, ALU.mult, None,
                                   ['hal', 'sel'], ['hL'])
                                TS(P, 'dve', hR[:, b, :, 0:15], hal[:, :, r, o:o + 15], sel_t[:, 8 + r:9 + r], None, ALU.mult, None,
                                   ['hal', 'sel'], ['hR'])
                                TS(P, 'dve', sLR[:, b, :, 0:1], hal[:, :, r, o + 33:o + 34], sel_t[:, r:r + 1], None, ALU.mult, None,
                                   ['hal', 'sel'], ['sLR'])
                                TS(P, 'dve', sLR[:, b, :, 1:2], hal[:, :, r, o + 32:o + 33], sel_t[:, 8 + r:9 + r], None, ALU.mult, None,
                                   ['hal', 'sel'], ['sLR'])
                            else:
                                STT(P, hL[:, b, :, 0:15], hal[:, :, r, o + 17:o + 32], sel_t[:, r:r + 1], hL[:, b, :, 0:15],
                                    ALU.mult, ALU.add, ['hal', 'sel', 'hL'], ['hL'])
                                STT(P, hR[:, b, :, 0:15], hal[:, :, r, o:o + 15], sel_t[:, 8 + r:9 + r], hR[:, b, :, 0:15],
                                    ALU.mult, ALU.add, ['hal', 'sel', 'hR'], ['hR'])
                                STT(P, sLR[:, b, :, 0:1], hal[:, :, r, o + 33:o + 34], sel_t[:, r:r + 1], sLR[:, b, :, 0:1],
                                    ALU.mult, ALU.add, ['hal', 'sel', 'sLR'], ['sLR'])
                                STT(P, sLR[:, b, :, 1:2], hal[:, :, r, o + 32:o + 33], sel_t[:, 8 + r:9 + r], sLR[:, b, :, 1:2],
                                    ALU.mult, ALU.add, ['hal', 'sel', 'sLR'], ['sLR'])
                    cut('cvh')
                    segs = [(b * NTh, NTh, b, False) for b in range(2)]
                    if not last:
                        segs += [(NT + b * CTX, CTX, b, True) for b in range(2)]
                    gi = 0
                    for (row0, n, b, is_ctx) in segs:
                        for g in range(CG):
                            gb_ = gbuf[gi % 2]
                            gbn = ('gbuf', gi % 2)
                            DMA(P, 'sp', gb_[:, 16:16 + n], g_scr.ap()[g * 128:(g + 1) * 128, row0:row0 + n], [('ga', l)], [gbn])
                            if is_ctx:
                                MEMSET(P, 'dve', gb_[:, 1:16], 0.0, [gbn])
                                MEMSET(P, 'dve', gb_[:, 16 + n:31 + n], 0.0, [gbn])
                            else:
                                COPY(P, 'dve', gb_[:, 1:16], hL[:, b, g, 0:15], ['hL'], [gbn])
                                COPY(P, 'dve', gb_[:, 16 + n:31 + n], hR[:, b, g, 0:15], ['hR'], [gbn])
                            zr = ('z', g)
                            TS(P, 'dve', zall[:, g, 0:n], gb_[:, 1:1 + n], wcf_c[:, l, g, 0:1], bcf_c[:, l, g:g + 1], ALU.mult, ALU.add,
                               [gbn, 'wcf', 'bcf'], [zr])
                            for k in range(1, 31):
                                STT(P, zall[:, g, 0:n], gb_[:, k + 1:k + 1 + n], wcf_c[:, l, g, k:k + 1], zall[:, g, 0:n], ALU.mult, ALU.add,
                                    [gbn, 'wcf', zr], [zr])
                            cut('cv1')
                            s_ = sbuf_[gi % 2]
                            sn = ('sbuf', gi % 2)
                            g2 = gbt[gi % 2]
                            g2n = ('gbt', gi % 2)
                            p_a = pa[gi % 2]
                            pan = ('pa', gi % 2)
                            y_ = ysh[gi % 2]
                            yn = ('ysh', gi % 2)
                            DMA(P, 'sp', s_[:, 16:16 + n], s_scr.ap()[g * 128:(g + 1) * 128, row0:row0 + n], [('gc', l)], [sn])
                            DMA(P, 'sp', g2[:, 0:n], gb_scr.ap()[g * 128:(g + 1) * 128, row0:row0 + n], [('gb', l)], [g2n])
                            if is_ctx:
                                MEMSET(P, 'dve', s_[:, 15:16], 0.0, [sn])
                                MEMSET(P, 'dve', s_[:, 16 + n:17 + n], 0.0, [sn])
                            else:
                                COPY(P, 'dve', s_[:, 15:16], sLR[:, b, g, 0:1], ['sLR'], [sn])
                                COPY(P, 'dve', s_[:, 16 + n:17 + n], sLR[:, b, g, 1:2], ['sLR'], [sn])
                            TS(P, 'dve', p_a[:, 0:n], s_[:, 15:15 + n], wsc_c[:, l, g, 0:1], None, ALU.mult, None, [sn, 'wsc'], [pan])
                            TS(P, 'dve', pb[:, 0:n], s_[:, 16:16 + n], wsc_c[:, l, g, 1:2], None, ALU.mult, None, [sn, 'wsc'], ['pb'])
                            TT(P, 'dve', p_a[:, 0:n], p_a[:, 0:n], pb[:, 0:n], ALU.add, [pan, 'pb'], [pan])
                            TS(P, 'dve', pb[:, 0:n], s_[:, 17:17 + n], wsc_c[:, l, g, 2:3], None, ALU.mult, None, [sn, 'wsc'], ['pb'])
                            TT(P, 'dve', p_a[:, 0:n], p_a[:, 0:n], pb[:, 0:n], ALU.add, [pan, 'pb'], [pan])
                            TT(P, 'dve', y_[:, 0, 0:n], p_a[:, 0:n], g2[:, 0:n], ALU.mult, [pan, g2n], [yn])
                            DMA(P, 'sp', mix_scr.ap()[g * 128:(g + 1) * 128, row0:row0 + n], y_[:, 0, 0:n], [yn], [('mix', l)], store=True)
                            gi += 1
                        cut('cv2')
                        zi = 0
                        for c0 in range(0, n, CH):
                            m = min(CH, n - c0)
                            for g in range(CG):
                                MM(P, ps[:, 0, 0:m], ones_f[:], zall[:, g, c0:c0 + m], g == 0, g == CG - 1, ['ones_f', ('z', g)], [PSR(0)])
                            for g in range(CG):
                                zq = zsq[zi % 2]
                                zqn = ('zsq', zi % 2)
                                zi += 1
                                TT(P, 'dve', zq[:, 0:m], zall[:, g, c0:c0 + m], zall[:, g, c0:c0 + m], ALU.mult, [('z', g)], [zqn])
                                MM(P, ps[:, 1, 0:m], ones_f[:], zq[:, 0:m], g == 0, g == CG - 1, ['ones_f', zqn], [PSR(1)])
                            ACTV(P, mu[:, 0:m], ps[:, 0, 0:m], AF.Copy, [PSR(0)], ['mu'], scale=1.0 / c.CFM_W)
                            TT(P, 'dve', msq[:, 0:m], mu[:, 0:m], mu[:, 0:m], ALU.mult, ['mu'], ['msq'])
                            STT(P, rs[:, 0:m], ps[:, 1, 0:m], 1.0 / c.CFM_W, msq[:, 0:m], ALU.mult, ALU.subtract, [PSR(1), 'msq'], ['rs'])
                            TS(P, 'dve', rs[:, 0:m], rs[:, 0:m], 1.0, EPS, ALU.mult, ALU.add, ['rs'], ['rs'])
                            ACTV(P, rs[:, 0:m], rs[:, 0:m], AF.Ln, ['rs'], ['rs'])
                            RSQ(P, rs[:, 0:m], ['rs'], ['rs'])
                            for g in range(CG):
                                t_ = tt[g % 2]
                                tn = ('tt', g % 2)
                                y_ = yst[g % 2]
                                yn = ('yst', g % 2)
                                TT(P, 'dve', t_[:, 0:m], zall[:, g, c0:c0 + m], mu[:, 0:m], ALU.subtract, [('z', g), 'mu'], [tn])
                                TT(P, 'dve', t_[:, 0:m], t_[:, 0:m], rs[:, 0:m], ALU.mult, [tn, 'rs'], [tn])
                                TS(P, 'dve', t_[:, 0:m], t_[:, 0:m], lng_c[:, l, g:g + 1], lnb_c[:, l, g:g + 1], ALU.mult, ALU.add, [tn, 'lng', 'lnb'], [tn])
                                ACTV(P, y_[:, 0, 0:m], t_[:, 0:m], AF.Silu, [tn], [yn])

                                ch = CG + H2 + g
                                DMA(P, 'sp', mix_scr.ap()[ch * 128:(ch + 1) * 128, row0 + c0:row0 + c0 + m], y_[:, 0, 0:m], [yn],
                                    [('mix', l)], store=True)
                P.barrier()
                cut('conv')

                with ExitStack() as s1:
                    NKEY = c.SEQ + CTX
                    NKT = NKEY // 128
                    LKT = c.SEQ // 128
                    kT = [sb("at_kT%d" % i, [128, 2, NKEY], BF16, s1) for i in range(2)]
                    vt = sb("at_vt", [128, NKT, 256], BF16, s1)
                    qT = [sb("at_qT%d" % i, [128, 2, NTh + CTX], BF16, s1) for i in range(2)]
                    pT = [sb("at_pT%d" % i, [128, 512], BF16, s1) for i in range(4)]
                    om = [sb("at_om%d" % i, [128, 2, 512], F32, s1) for i in range(2)]
                    rl = sb("at_rl", [128, 512], F32, s1)
                    att = sb("at_att", [128, 2, 512], F32, s1)
                    sq = sb("at_sq", [128, 2, 512], F32, s1)
                    rsd = sb("at_rsd", [128, 512], F32, s1)
                    ybf = [sb("at_y%d" % i, [128, 2, 512], BF16, s1) for i in range(2)]
                    scale = 128.0 ** -0.5
                    hi = 0
                    pi = 0
                    yi = 0
                    for b in range(2):
                        for h in range(H):
                            kt_ = kT[hi % 2]
                            ktn = ('kT', hi % 2)
                            qt_ = qT[hi % 2]
                            qtn = ('qT', hi % 2)
                            hi += 1
                            for r in range(8):
                                src = kg_rank(r)[2 * h * 128:(2 * h + 2) * 128, b * NTh:(b + 1) * NTh].rearrange("(m p) t -> p m t", p=128)
                                DMA(P, 'sp', kt_[:, :, r * NTh:(r + 1) * NTh], src, [('kvg', l)], [ktn])
                                src = vg_rank(r)[b * NTh:(b + 1) * NTh, h * 256:(h + 1) * 256].rearrange("(k p) x -> p k x", p=128)
                                DMA(P, 'sp', vt[:, r * (NTh // 128):(r + 1) * (NTh // 128), :], src, [('kvg', l)], ['vt'])
                            src = kc_scr.ap()[2 * h * 128:(2 * h + 2) * 128, b * CTX:(b + 1) * CTX].rearrange("(m p) t -> p m t", p=128)
                            DMA(P, 'sp', kt_[:, :, c.SEQ:NKEY], src, [('kc', l)], [ktn])
                            src = vc_scr.ap()[b * CTX:(b + 1) * CTX, h * 256:(h + 1) * 256].rearrange("(k p) x -> p k x", p=128)
                            DMA(P, 'sp', vt[:, LKT:NKT, :], src, [('vc', l)], ['vt'])
                            src = q_scr.ap()[2 * h * 128:(2 * h + 2) * 128, b * NTh:(b + 1) * NTh].rearrange("(m p) t -> p m t", p=128)
                            DMA(P, 'sp', qt_[:, :, 0:NTh], src, [('q', l)], [qtn])
                            if not last:
                                src = q_scr.ap()[2 * h * 128:(2 * h + 2) * 128, NT + b * CTX:NT + (b + 1) * CTX].rearrange(
                                    "(m p) t -> p m t", p=128)
                                DMA(P, 'sp', qt_[:, :, NTh:NTh + CTX], src, [('q', l)], [qtn])
                            qblocks = [(k * TB, TB, 0, NKT, b * NTh + k * TB) for k in range(NTh // TB)]
                            if not last:
                                qblocks.append((NTh, CTX, LKT, NKT, NT + b * CTX))
                            for (q0, nq, kt0, kt1, orow) in qblocks:
                                for m in range(2):
                                    kts = list(range(kt0, kt1))

                                    def S(i):
                                        kt = kts[i]
                                        bk = i % 3
                                        MM(P, ps[:, bk, 0:nq], kt_[:, m, kt * 128:(kt + 1) * 128], qt_[:, m, q0:q0 + nq], True, True,
                                           [ktn, qtn], [PSR(bk)])

                                    def AV(i, pi):
                                        kt = kts[i]
                                        bk = i % 3
                                        p_ = pT[pi % 4]
                                        pn = ('pT', pi % 4)
                                        ACTV(P, p_[:, 0:nq], ps[:, bk, 0:nq], AF.Exp, [PSR(bk)], [pn], scale=scale)
                                        first, lastk = (i == 0), (i == len(kts) - 1)
                                        MM(P, ps[:, 3, 0:nq], vt[:, kt, 0:128], p_[:, 0:nq], first, lastk, ['vt', pn], [PSR(3)])
                                        MM(P, ps[:, 4, 0:nq], vt[:, kt, 128:256], p_[:, 0:nq], first, lastk, ['vt', pn], [PSR(4)])
                                        MM(P, ps[:, 5, 0:nq], ones_b[:], p_[:, 0:nq], first, lastk, ['ones_b', pn], [PSR(5)])

                                    S(0)
                                    for i in range(len(kts)):
                                        if i + 1 < len(kts):
                                            S(i + 1)
                                        AV(i, pi)
                                        pi += 1
                                    RECIP(P, rl[:, 0:nq], ps[:, 5, 0:nq], [PSR(5)], ['rl'])
                                    omr = ('om', m)
                                    TT(P, 'dve', om[m][:, 0, 0:nq], ps[:, 3, 0:nq], rl[:, 0:nq], ALU.mult, [PSR(3), 'rl'], [omr])
                                    TT(P, 'dve', om[m][:, 1, 0:nq], ps[:, 4, 0:nq], rl[:, 0:nq], ALU.mult, [PSR(4), 'rl'], [omr])
                                STT(P, att[:, :, 0:nq], om[1][:, :, 0:nq], neglam[:, l:l + 1], om[0][:, :, 0:nq], ALU.mult, ALU.add,
                                    [('om', 0), ('om', 1), 'neglam'], ['att'])
                                TT(P, 'dve', sq[:, :, 0:nq], att[:, :, 0:nq], att[:, :, 0:nq], ALU.mult, ['att'], ['sq'])
                                MM(P, ps[:, 6, 0:nq], ones_f[:], sq[:, 0, 0:nq], True, False, ['ones_f', 'sq'], [PSR(6)])
                                MM(P, ps[:, 6, 0:nq], ones_f[:], sq[:, 1, 0:nq], False, True, ['ones_f', 'sq'], [PSR(6)])
                                TS(P, 'dve', rsd[:, 0:nq], ps[:, 6, 0:nq], 1.0 / 256, EPS, ALU.mult, ALU.add, [PSR(6)], ['rsd'])
                                ACTV(P, rsd[:, 0:nq], rsd[:, 0:nq], AF.Ln, ['rsd'], ['rsd'])
                                RSQ(P, rsd[:, 0:nq], ['rsd'], ['rsd'])
                                y_ = ybf[yi % 2]
                                yn = ('ybf', yi % 2)
                                yi += 1
                                for cc in range(2):
                                    TT(P, 'dve', sq[:, cc, 0:nq], att[:, cc, 0:nq], rsd[:, 0:nq], ALU.mult, ['att', 'rsd', 'sq'], ['sq'])
                                    TS(P, 'dve', y_[:, cc, 0:nq], sq[:, cc, 0:nq], subg_c[:, l, cc:cc + 1], None, ALU.mult, None, ['sq', 'subg'], [yn])
                                ch = CG + 2 * h
                                for c2 in range(2):
                                    DMA(P, 'sp', mix_scr.ap()[(ch + c2) * 128:(ch + c2 + 1) * 128, orow:orow + nq], y_[:, c2, 0:nq], [yn], [('mix', l)], store=True)
                P.barrier()
                cut('attn')

                wout = wview('out', l, D)
                wgt = wview('gate', l, c.FF)
                wup = wview('up', l, c.FF)
                wdn = wview('down', l, D)
                for (row0, n, j, b, is_ctx, tok0) in blocks:
                    if is_ctx and last:
                        continue
                    NTL = n // 128
                    with ExitStack() as s1:
                        mixT = sb("o_mixT", [128, KC, TBM], BF16, s1)
                        wo = [sb("o_w%d" % i, [128, KC, 256], BF16, s1) for i in range(2)]
                        hbk = sb("o_hb", [128, TBM // 128, D], F32, s1)
                        gbc = sb("o_gbc", [128, D], F32, s1)
                        dtmp = sb("o_dtmp", [128, 128], F32, s1)
                        tmp = [sb("o_tmp%d" % i, [128, 256], F32, s1) for i in range(2)]
                        bcast_cols(mvec(l, j, 2), gbc, ['modT'], ['gbc'], dtmp, 'dtmp')
                        DMA(P, 'sp', mixT[:, :, 0:n], mix_scr.ap()[:, row0:row0 + n].rearrange("(k p) t -> p k t", p=128), [('mix', l)], ['mixT'])
                        for t in range(NTL):
                            DMA(P, 'sp', hbk[:, t, :], hsrc(l, row0 + t * 128, 128), [('h', l)], [('hbk', t)])
                        ti = 0
                        for pn in range(D // 256):
                            w = wo[pn % 2]
                            wn = ('wo', pn % 2)
                            DMA(P, 'sp', w[:], wout[:, pn * 256:(pn + 1) * 256].rearrange("(k p) x -> p k x", p=128), [('wg', 'out', l)], [wn])
                            for t in range(NTL):
                                bk = t % 4
                                for k in range(KC):
                                    MM(P, ps[:, bk, 0:256], mixT[:, k, t * 128:(t + 1) * 128], w[:, k, :], k == 0, k == KC - 1,
                                       ['mixT', wn], [PSR(bk)])
                                tm = tmp[ti % 2]
                                tmn = ('otmp', ti % 2)
                                ti += 1
                                TT(P, 'dve', tm[:], ps[:, bk, 0:256], gbc[:, pn * 256:(pn + 1) * 256], ALU.mult, [PSR(bk), 'gbc'], [tmn])
                                TT(P, 'dve', hbk[:, t, pn * 256:(pn + 1) * 256], hbk[:, t, pn * 256:(pn + 1) * 256], tm[:], ALU.add,
                                   [('hbk', t), tmn], [('hbk', t)])
                        for t in range(NTL):
                            DMA(P, 'sp', h1_scr.ap()[row0 + t * 128:row0 + (t + 1) * 128, :], hbk[:, t, :], [('hbk', t)], [('h1', l)], store=True)
                    P.barrier()
                    cut('p5')

                    with ExitStack() as sB:
                        fT = sb("m_fT", [128, KC, TBM], BF16, sB)
                        gTb = sb("m_gT", [32, TBM], F32, sB)
                        acc = sb("m_acc", [128, TBM // 128, D], F32, sB)
                        with ExitStack() as s1:
                            hb = sb("r_hb", [128, D], F32, s1)
                            junk = sb("r_junk", [128, D], BF16, s1)
                            dst_junk[0], dst_junk[1] = junk, 'junk'
                            ss = sb("r_ss", [128, 1], F32, s1)
                            rt = sb("r_rt", [128, 1], F32, s1)
                            f32T = sb("r_f32T", [128, KC, 128], F32, s1)
                            wr_t = sb("r_wr", [128, KC, 36], F32, s1)
                            lg = sb("r_lg", [128, 36], F32, s1)
                            sm = sb("r_sm", [128, 64], F32, s1)
                            oh = sb("r_oh", [128, 4], F32, s1)
                            lsel = sb("r_lsel", [128, 8], F32, s1)
                            l2 = sb("r_l2", [128, 8], F32, s1)
                            mk = sb("r_mk", [128, 16], F32, s1)
                            wi_ = sb("r_wi", [128, 8], F32, s1)
                            gts = sb("r_gts", [128, 32], F32, s1)
                            DMA(P, 'sp', wr_t[:, :, 0:4], w_rg.ap()[l].rearrange("(k p) x -> p k x", p=128), (), ['wr_t'])
                            DMA(P, 'sp', wr_t[:, :, 4:36], w_re.ap()[l].rearrange("(k p) x -> p k x", p=128), (), ['wr_t'])
                            av, shv = a2[:, l * 3 + j, :], mvec(l, j, 3)
                            for t in range(NTL):
                                def wrt(k, pss, psr, t=t, av=av, shv=shv):
                                    if False:
                                        ACTV(P, f32T[:, k, :], pss, AF.Identity, [psr, 'a2', 'modT'], [('f32T', k)],
                                             bias=shv[:, k:k + 1], scale=av[:, k:k + 1])
                                    else:
                                        TS(P, 'dve', f32T[:, k, :], pss, av[:, k:k + 1], shv[:, k:k + 1], ALU.mult, ALU.add,
                                           [psr, 'a2', 'modT'], [('f32T', k)])
                                    COPY(P, 'dve', fT[:, k, t * 128:(t + 1) * 128], f32T[:, k, :], [('f32T', k)], ['fT'])
                                norm_tile(l, j, h1_scr.ap()[row0 + t * 128:row0 + (t + 1) * 128, :], hb, 'hb', av, shv, (ss, rt), wrt,
                                          [('h1', l)])
                                for k in range(KC):
                                    MM(P, ps[:, 6, 0:36], f32T[:, k, :], wr_t[:, k, :], k == 0, False, [('f32T', k), 'wr_t'], [PSR(6)])
                                MM(P, ps[:, 6, 0:36], ones_f[0:1, :], brow[0:1, l, :], False, True, ['ones_f', 'brow'], [PSR(6)])
                                COPY(P, 'dve', lg[:], ps[:, 6, 0:36], [PSR(6)], ['lg'])
                                R = ['lg', 'sm', 'oh', 'lsel', 'l2', 'mk', 'wi', 'gts']
                                RMAX(P, sm[:, 0:1], lg[:, 0:4], R, R)
                                TS(P, 'dve', oh[:], lg[:, 0:4], sm[:, 0:1], None, ALU.is_equal, None, R, R)
                                TS(P, 'dve', sm[:, 1:2], sm[:, 0:1], -1.0, None, ALU.mult, None, R, R)
                                TS(P, 'dve', sm[:, 4:8], lg[:, 0:4], sm[:, 0:1], None, ALU.subtract, None, R, R)
                                ACTV(P, sm[:, 4:8], sm[:, 4:8], AF.Exp, R, R, accum=sm[:, 2:3])
                                RECIP(P, sm[:, 3:4], sm[:, 2:3], R, R)
                                TS(P, 'dve', lsel[:], lg[:, 4:12], oh[:, 0:1], None, ALU.mult, None, R, R)
                                for g in range(1, 4):
                                    STT(P, lsel[:], lg[:, 4 + 8 * g:12 + 8 * g], oh[:, g:g + 1], lsel[:], ALU.mult, ALU.add, R, R)
                                RMAX(P, sm[:, 8:9], lsel[:], R, R)
                                TS(P, 'dve', mk[:, 0:8], lsel[:], sm[:, 8:9], None, ALU.is_equal, None, R, R)
                                STT(P, l2[:], mk[:, 0:8], -1e30, lsel[:], ALU.mult, ALU.add, R, R)
                                RMAX(P, sm[:, 9:10], l2[:], R, R)
                                TS(P, 'dve', mk[:, 8:16], l2[:], sm[:, 9:10], None, ALU.is_equal, None, R, R)
                                TT(P, 'dve', sm[:, 10:11], sm[:, 9:10], sm[:, 8:9], ALU.subtract, R, R)
                                ACTV(P, sm[:, 11:12], sm[:, 10:11], AF.Exp, R, R)
                                TS(P, 'dve', sm[:, 12:13], sm[:, 11:12], 1.0, None, ALU.add, None, R, R)
                                RECIP(P, sm[:, 12:13], sm[:, 12:13], R, R)
                                TT(P, 'dve', sm[:, 13:14], sm[:, 12:13], sm[:, 3:4], ALU.mult, R, R)
                                TT(P, 'dve', sm[:, 14:15], sm[:, 13:14], sm[:, 11:12], ALU.mult, R, R)
                                TS(P, 'dve', wi_[:], mk[:, 0:8], sm[:, 13:14], None, ALU.mult, None, R, R)
                                STT(P, wi_[:], mk[:, 8:16], sm[:, 14:15], wi_[:], ALU.mult, ALU.add, R, R)
                                for g in range(4):
                                    TS(P, 'dve', gts[:, 8 * g:8 * g + 8], wi_[:], oh[:, g:g + 1], None, ALU.mult, None, R, R)
                                TR(P, ps[0:32, 7, 0:128], gts[:], ident[:], R + ['ident'], [PSR(7)])
                                ACTV(P, gTb[0:32, t * 128:(t + 1) * 128], ps[0:32, 7, 0:128], AF.Copy, [PSR(7)], ['gTb'])
                        P.barrier()
                        cut('p6')

                        with ExitStack() as s1:
                            NHALF = max(1, FC // 2)
                            FH = FC // NHALF
                            WH = FH * 128
                            ring = [sb("e_ring%d" % i, [128, 2048 * 4], BF16, s1) for i in range(3)]
                            hid = sb("e_hid", [128, FC, TBM], BF16, s1)
                            gbcs = [sb("e_gbc%d" % i, [128, TBM], F32, s1) for i in range(2)]
                            sg = [sb("e_sg%d" % i, [128, TBM], F32, s1) for i in range(2)]
                            tu = [sb("e_tu%d" % i, [128, TBM], F32, s1) for i in range(2)]
                            sel_e = [sb("e_sel%d" % i, [32, 128], F32, s1) for i in range(2)]
                            ri = 0
                            ei = 0
                            for e_ in range(NE):
                                se = sel_e[e_ % 2]
                                sen = ('sel_e', e_ % 2)
                                gb_ = gbcs[e_ % 2]
                                gbn = ('gbcs', e_ % 2)
                                TS(P, 'dve', se[:], ones_f[0:32, :], ident[0:32, e_:e_ + 1], None, ALU.mult, None, ['ones_f', 'ident'], [sen])
                                MM(P, ps[:, 6, 0:n], se[:], gTb[0:32, 0:n], True, True, [sen, 'gTb'], [PSR(6)])
                                ACTV(P, gb_[:, 0:n], ps[:, 6, 0:n], AF.Copy, [PSR(6)], [gbn])
                                for hf in range(NHALF):
                                    wts = []
                                    for (wv, nm) in ((wgt, 'gate'), (wup, 'up')):
                                        rg_ = ring[ri % 3]
                                        rn = ('ring', ri % 3)
                                        ri += 1
                                        wt = rg_[:, 0:KC * WH].rearrange("p (k x) -> p k x", k=KC)
                                        DMA(P, 'sp', wt, wv[e_ * D:(e_ + 1) * D, hf * WH:(hf + 1) * WH].rearrange("(k p) x -> p k x", p=128),
                                            [('wg', nm, l)], [rn])
                                        wts.append((wt, rn))
                                    for fi in range(FH):
                                        for k in range(KC):
                                            MM(P, ps[:, fi, 0:n], wts[0][0][:, k, fi * 128:(fi + 1) * 128], fT[:, k, 0:n], k == 0, k == KC - 1,
                                               [wts[0][1], 'fT'], [PSR(fi)])
                                    for fi in range(FH):
                                        for k in range(KC):
                                            MM(P, ps[:, 2 + fi, 0:n], wts[1][0][:, k, fi * 128:(fi + 1) * 128], fT[:, k, 0:n], k == 0, k == KC - 1,
                                               [wts[1][1], 'fT'], [PSR(2 + fi)])
                                    for fi in range(FH):
                                        s_ = sg[ei % 2]
                                        sn = ('sg', ei % 2)
                                        u_ = tu[ei % 2]
                                        un = ('tu', ei % 2)
                                        ei += 1
                                        ACTV(P, s_[:, 0:n], ps[:, fi, 0:n], AF.Silu, [PSR(fi)], [sn])
                                        TT(P, 'dve', u_[:, 0:n], ps[:, 2 + fi, 0:n], gb_[:, 0:n], ALU.mult, [PSR(2 + fi), gbn], [un])
                                        TT(P, 'dve', hid[:, hf * FH + fi, 0:n], s_[:, 0:n], u_[:, 0:n], ALU.mult, [sn, un], ['hid'])
                                DCH = 512
                                for d0 in range(0, D, 2048):
                                    dw = min(2048, D - d0)
                                    rg_ = ring[ri % 3]
                                    rn = ('ring', ri % 3)
                                    ri += 1
                                    wd = rg_[:, 0:FC * dw].rearrange("p (f x) -> p f x", f=FC)
                                    DMA(P, 'sp', wd, wdn[e_ * c.FF:(e_ + 1) * c.FF, d0:d0 + dw].rearrange("(f p) x -> p f x", p=128),
                                        [('wg', 'down', l)], [rn])
                                    oi = 0
                                    for t in range(NTL):
                                        for dc in range(0, dw, DCH):
                                            bk = 4 + oi % 2
                                            oi += 1
                                            for f in range(FC):
                                                MM(P, ps[:, bk, 0:DCH], hid[:, f, t * 128:(t + 1) * 128], wd[:, f, dc:dc + DCH], f == 0, f == FC - 1,
                                                   ['hid', rn], [PSR(bk)])
                                            a_ = acc[:, t, d0 + dc:d0 + dc + DCH]
                                            if e_ == 0:
                                                COPY(P, 'dve', a_, ps[:, bk, 0:DCH], [PSR(bk)], [('acc', t)])
                                            else:
                                                TT(P, 'dve', a_, ps[:, bk, 0:DCH], a_, ALU.add, [PSR(bk), ('acc', t)], [('acc', t)])
                        P.barrier()
                        cut('moe')

                        with ExitStack() as s1:
                            hb2 = [sb("f_hb%d" % i, [128, D], F32, s1) for i in range(2)]
                            gbc = sb("f_gbc", [128, D], F32, s1)
                            gfb = sb("f_gfb", [128, D], F32, s1)
                            dtmp = sb("f_dtmp", [128, 128], F32, s1)
                            junk = sb("f_junk", [128, D], BF16, s1)
                            ss = sb("f_ss", [128, 1], F32, s1)
                            rt = sb("f_rt", [128, 1], F32, s1)
                            bcast_cols(mvec(l, j, 5), gbc, ['modT'], ['gbc'], dtmp, 'dtmp')
                            if last:
                                bcast_cols(gfin_c, gfb, ['gfin'], ['gfb'], dtmp, 'dtmp')
                            for t in range(NTL):
                                hb = hb2[t % 2]
                                hbn = ('hb2', t % 2)
                                DMA(P, 'sp', hb[:], h1_scr.ap()[row0 + t * 128:row0 + (t + 1) * 128, :], [('h1', l)], [hbn])
                                TT(P, 'dve', acc[:, t, :], acc[:, t, :], gbc[:], ALU.mult, [('acc', t), 'gbc'], [('acc', t)])
                                TT(P, 'dve', hb[:], hb[:], acc[:, t, :], ALU.add, [hbn, ('acc', t)], [hbn])
                                if not last:
                                    DMA(P, 'sp', h_scr.ap()[row0 + t * 128:row0 + (t + 1) * 128, :], hb[:], [hbn], [('h', l + 1)], store=True)
                                else:
                                    ACTV(P, junk[:], hb[:], AF.Square, [hbn], ['junk', 'ss'], accum=ss[:])
                                    TS(P, 'dve', rt[:], ss[:], 1.0 / D, EPS, ALU.mult, ALU.add, ['ss'], ['rt'])
                                    ACTV(P, rt[:], rt[:], AF.Ln, ['rt'], ['rt'])
                                    RSQ(P, rt[:], ['rt'], ['rt'])
                                    STT(P, hb[:], hb[:], rt[:, 0:1], gfb[:], ALU.mult, ALU.mult, [hbn, 'rt', 'gfb'], [hbn])
                                    DMA(P, 'sp', out_sh.ap()[row0 + t * 128:row0 + (t + 1) * 128, :], hb[:], [hbn], ['out'], store=True)
                        P.barrier()
                        cut('p7')
                        cut('blk%d' % blocks.index((row0, n, j, b, is_ctx, tok0)))
                        if is_ctx and b == 1:
                            cut('l%d' % l)

        except StopBuild:
            pass
        P.dead = False
        P.barrier(full=True)
        P.emit(nc, es)
        nc_ctx.__exit__(None, None, None)
    return nc, P


def host_inputs(c, inp):
    L, D, NTh, CTX = c.L, c.D, c.NTh, c.CTX
    f = lambda a: np.ascontiguousarray(np.asarray(a, dtype=np.float32))
    x = f(inp['x'])
    ctx = f(inp['ctx']).reshape(2 * CTX, D)
    cvec = np.concatenate([f(inp['c']), f(inp['c_ctx'])[None, :]], axis=0)
    ident = np.eye(128, dtype=np.float32)
    pm = np.zeros((128, 128), np.float32)
    for i in range(32):
        pm[32 + i, i] = -1.0
        pm[i, 32 + i] = 1.0
        pm[96 + i, 64 + i] = -1.0
        pm[64 + i, 96 + i] = 1.0
    w_gate = f(inp['w_gate']).reshape(L, 32, D, c.FF)
    w_up = f(inp['w_up']).reshape(L, 32, D, c.FF)
    w_down = f(inp['w_down']).reshape(L, 32, c.FF, D)
    w_in, w_out, w_ada, b_ada = f(inp['w_in']), f(inp['w_out']), f(inp['w_ada']), f(inp['b_ada'])
    axis_dim = 64
    inv = (10000.0 ** (-np.arange(0, axis_dim, 2, dtype=np.float32) / axis_dim)).astype(np.float32)
    maps = []
    for i in range(8):
        pos = np.arange(i * NTh, (i + 1) * NTh)
        row = (pos // c.GRID_W).astype(np.float32)
        col = (pos % c.GRID_W).astype(np.float32)
        ang_r = row[:, None] * inv[None, :]
        ang_c = col[:, None] * inv[None, :]
        ang = np.concatenate([ang_r, ang_r, ang_c, ang_c], axis=-1).astype(np.float32)
        sel = np.zeros((128, 16), np.float32)
        if i > 0:
            sel[:, i - 1] = 1.0
        if i < 7:
            sel[:, 8 + i + 1] = 1.0
        m = {
            'x_sh': np.ascontiguousarray(x[:, i * NTh:(i + 1) * NTh, :].reshape(2 * NTh, D)),
            'ctx_in': ctx, 'cvec': cvec, 'selLR': sel,
            'w_ada_sh': np.ascontiguousarray(w_ada[:, :, i * c.NS:(i + 1) * c.NS]),
            'b_ada_sh': np.ascontiguousarray(b_ada[:, i * c.NS:(i + 1) * c.NS]),
            'g_mix': f(inp['g_mix']), 'g_ffn': f(inp['g_ffn']), 'g_final': f(inp['g_final']),
            'w_in_sh': np.ascontiguousarray(w_in[:, i * (D // 8):(i + 1) * (D // 8), :]),
            'w_out_sh': np.ascontiguousarray(w_out[:, i * (D // 8):(i + 1) * (D // 8), :]),
            'w_gate_sh': np.ascontiguousarray(w_gate[:, 4 * i:4 * i + 4].reshape(L, 4 * D, c.FF)),
            'w_up_sh': np.ascontiguousarray(w_up[:, 4 * i:4 * i + 4].reshape(L, 4 * D, c.FF)),
            'w_down_sh': np.ascontiguousarray(w_down[:, 4 * i:4 * i + 4].reshape(L, 4 * c.FF, D)),
            'sc_w': f(inp['short_conv_w']), 'cf_w': f(inp['cfm_conv_w']), 'cf_b': f(inp['cfm_conv_b']),
            'ln_g': f(inp['cfm_ln_g']), 'ln_b': f(inp['cfm_ln_b']), 'lam_qk': f(inp['lam_qk']),
            'subln_g': f(inp['subln_g']), 'w_rg': f(inp['w_route_group']), 'b_rg': f(inp['b_route_group']),
            'w_re': f(inp['w_route_expert']), 'b_re': f(inp['b_route_expert']),
            'cosT': np.ascontiguousarray(np.cos(ang).T.astype(np.float32)),
            'sinT': np.ascontiguousarray(np.sin(ang).T.astype(np.float32)),
            'ident': ident, 'pm': pm,
        }
        maps.append(m)
    return maps


def run_cfg(c, inp, trace=False):
    nc, P = build(c)
    maps = host_inputs(c, inp)
    res = run_bass_kernel_spmd(nc, maps, core_ids=list(range(8)), trace=trace)
    out = np.empty((2, c.SEQ, c.D), np.float32)
    for i in range(8):
        o = res.results[i]['out_sh'].reshape(2, c.NTh, c.D)
        out[:, i * c.NTh:(i + 1) * c.NTh, :] = o
    return out, res


def kernel(**inputs):
    c = Cfg(4096, 8192)
    out, _ = run_cfg(c, inputs)
    return out
```

```python
import math
from contextlib import ExitStack
import numpy as np
import concourse.bass as bass
import concourse.mybir as mybir
from concourse.bass_utils import run_bass_kernel_spmd

F32 = mybir.dt.float32
BF16 = mybir.dt.bfloat16
ALU = mybir.AluOpType
AF = mybir.ActivationFunctionType
EPS = 1e-6
ENGS = ('pe', 'act', 'dve', 'pool', 'sp')


class Cfg:
    def __init__(self, D, SEQ, CTX=256, L=2):
        self.D, self.SEQ, self.CTX, self.L = D, SEQ, CTX, L
        self.NC = 8
        self.KC = D // 128
        self.NTh = SEQ // 8
        self.NT = 2 * self.NTh
        self.NTOT = self.NT + 2 * CTX
        self.CONV_W = D // 4
        self.CG = self.CONV_W // 128
        self.ATT_W = D // 2
        self.H = self.ATT_W // 256
        self.H2 = 2 * self.H
        self.CFM_W = D - self.CONV_W - self.ATT_W
        self.QK_W = self.H * 256
        self.OFF_Q = 3 * self.CONV_W
        self.OFF_K = self.OFF_Q + self.QK_W
        self.OFF_V = self.OFF_K + self.QK_W
        self.OFF_C = self.OFF_V + self.ATT_W
        self.PROJ_W = self.OFF_C + 2 * self.CFM_W
        self.FF = D // 8
        self.FC = self.FF // 128
        self.NE = 32
        self.NS = 6 * D // 8
        self.NSC = self.NS // 128
        self.TB = min(512, self.NTh)
        self.PW = 256
        self.GRID_W = 64
        self.KVN = self.H2 * 128 * self.NT + self.NT * self.ATT_W


class StopBuild(Exception):
    pass


class Prog:
    CH = 4000
    K = 8
    PU = 250

    def __init__(self):
        self.ops = []
        self.lw = {}
        self.rd = {}
        self.last_on = {}
        self.pending_st = []
        self.dead = False

    def op(self, eng, fn, reads=(), writes=(), kind='c', extra=()):
        if self.dead:
            return None
        i = len(self.ops)
        deps = set(extra)
        for r in reads:
            w = self.lw.get(r)
            if w:
                deps.update(w)
        for r in writes:
            rr = self.rd.get(r)
            if rr and (rr[0] or rr[1]):
                deps.update(rr[0].values())
                deps.update(rr[1])
        for r in reads:
            rr = self.rd.get(r)
            if rr is None:
                rr = self.rd[r] = ({}, [])
            if kind == 'c':
                rr[0][eng] = i
            else:
                rr[1].append(i)
        for r in writes:
            rr = self.rd.get(r)
            if rr and (rr[0] or rr[1]):
                self.lw[r] = [i]
                self.rd[r] = ({}, [])
            else:
                self.lw.setdefault(r, []).append(i)
                if r in reads:
                    pass
        deps.discard(i)
        self.ops.append([eng, fn, kind, deps])
        self.last_on[(eng, kind)] = i
        return i

    def barrier(self, full=False):
        if self.dead:
            return
        if full:
            hasdep = set()
            for o in self.ops:
                hasdep.update(o[3])
            extra = {i for i, o in enumerate(self.ops) if o[2] in ('cc', 'd') and i not in hasdep}
            self.pending_st = list(set(self.pending_st) | extra)
        deps = {v for (eng, kind), v in self.last_on.items() if kind == 'c'} | set(self.pending_st)
        b = self.op('sp', None, extra=deps)
        for e in ('pe', 'act', 'dve', 'pool'):
            self.op(e, None, extra={b})
        self.pending_st = []

    def emit(self, nc, es):
        ops = self.ops
        n = len(ops)
        for o in ops:
            red = {}
            keep = set()
            for d in o[3]:
                de, _, dk, _ = ops[d]
                if dk == 'c':
                    if red.get(de, -1) < d:
                        red[de] = d
                else:
                    keep.add(d)
            o[3] = keep | set(red.values())
        need = [False] * n
        for (eng, fn, kind, deps) in ops:
            for d in deps:
                de, _, dk, _ = ops[d]
                if dk == 'c' and kind == 'c' and de == 'pe' and eng == 'pe':
                    continue
                need[d] = True
        sig = [None] * n
        ccount = {e: 0 for e in ENGS}
        csems = {e: [] for e in ENGS}
        dcount = {'sp': 0, 'pool': 0}
        dsems = {'sp': [], 'pool': []}
        slot_prev = {}
        ncc = 0
        for i, (eng, fn, kind, deps) in enumerate(ops):
            if kind == 'c':
                if need[i]:
                    k = ccount[eng]
                    ccount[eng] += 1
                    ch = k // self.CH
                    while len(csems[eng]) <= ch:
                        csems[eng].append(es.enter_context(nc.semaphore("c_%s_%d" % (eng, len(csems[eng])))))
                    sig[i] = (csems[eng][ch], k % self.CH + 1, 1, ('c', eng, ch))
            elif kind == 'd':
                j = dcount[eng]
                dcount[eng] += 1
                s = j % self.K
                m = j // self.K
                pool = m // self.PU
                val = 16 * ((m % self.PU) + 1)
                while len(dsems[eng]) <= pool:
                    pi = len(dsems[eng])
                    dsems[eng].append([es.enter_context(nc.semaphore("d_%s_%d_%d" % (eng, pi, t)))
                                       for t in range(self.K)])
                sig[i] = (dsems[eng][pool][s], val, 16, ('d', eng, pool, s))
                prev = slot_prev.get((eng, pool, s))
                if prev is not None:
                    deps.add(prev)
                slot_prev[(eng, pool, s)] = i
            else:
                sem = es.enter_context(nc.semaphore("cc_%d" % ncc))
                ncc += 1
                sig[i] = (sem, 1, 1, ('cc', ncc))
        self.nsig = dict(ccount)
        self.ndma = dict(dcount)

        def run(engname, e):
            waited = {}
            cwait = {}
            for i, (eng, fn, kind, deps) in enumerate(ops):
                if eng != engname:
                    continue
                for d in sorted(deps):
                    sg = sig[d]
                    if sg is None:
                        continue
                    de, _, dk, _ = ops[d]
                    if dk == 'c' and kind == 'c' and de == 'pe' and eng == 'pe':
                        continue
                    sem, val, _, key = sg
                    if key[0] == 'c':
                        cw = cwait.get(key[1])
                        if cw is not None and (cw[0] > key[2] or (cw[0] == key[2] and cw[1] >= val)):
                            continue
                        cwait[key[1]] = (key[2], val)
                    else:
                        if waited.get(key, 0) >= val:
                            continue
                        waited[key] = val
                    e.wait_ge(sem, val)
                if fn is None:
                    if sig[i] is not None:
                        e.nop().then_inc(sig[i][0], sig[i][2])
                else:
                    ins = fn(e)
                    if sig[i] is not None:
                        if kind == 'cc':
                            ins.then_inc(sig[i][0])
                        else:
                            ins.then_inc(sig[i][0], sig[i][2])

        with nc.Block() as block:
            @block.tensor
            def _(e):
                run('pe', e)

            @block.vector
            def _(e):
                run('dve', e)

            @block.scalar
            def _(e):
                run('act', e)

            @block.gpsimd
            def _(e):
                run('pool', e)

            @block.sync
            def _(e):
                run('sp', e)


def MM(P, out, lhsT, rhs, start, stop, rd, wr):
    P.op('pe', lambda e: e.matmul(out, lhsT, rhs, start=start, stop=stop), rd, wr)


def TR(P, out, in_, ident, rd, wr):
    P.op('pe', lambda e: e.transpose(out, in_, ident), rd, wr)


def ACTV(P, out, in_, func, rd, wr, bias=None, scale=None, accum=None):
    kw = {}
    if bias is not None:
        kw['bias'] = bias
    if scale is not None:
        kw['scale'] = scale
    if accum is not None:
        kw['accum_out'] = accum
    P.op('act', lambda e: e.activation(out, in_, func, **kw), rd, wr)


def TS(P, eng, out, in0, s1, s2, op0, op1, rd, wr):
    if op1 is None:
        P.op(eng, lambda e: e.tensor_scalar(out, in0, s1, None, op0), rd, wr)
    else:
        P.op(eng, lambda e: e.tensor_scalar(out, in0, s1, s2, op0, op1), rd, wr)


def TT(P, eng, out, in0, in1, op, rd, wr):
    P.op(eng, lambda e: e.tensor_tensor(out, in0, in1, op), rd, wr)


def STT(P, out, in0, scalar, in1, op0, op1, rd, wr):
    P.op('dve', lambda e: e.scalar_tensor_tensor(out, in0, scalar, in1, op0, op1), rd, wr)


def RECIP(P, out, in_, rd, wr):
    ACTV(P, out, in_, AF.Ln, rd, wr)
    ACTV(P, out, out, AF.Exp, list(wr), wr, scale=-1.0)


def RSQ(P, out, rd, wr):
    ACTV(P, out, out, AF.Exp, rd, wr, scale=-0.5)


def RMAX(P, out, in_, rd, wr):
    P.op('dve', lambda e: e.reduce_max(out, in_, mybir.AxisListType.X), rd, wr)


def COPY(P, eng, out, in_, rd, wr):
    P.op(eng, lambda e: e.tensor_copy(out, in_), rd, wr)


def MEMSET(P, eng, ap, val, wr):
    P.op(eng, lambda e: e.memset(ap, val), (), wr)


def DMA(P, q, out, in_, rd, wr, store=False, **kw):
    i = P.op(q, lambda e: e.dma_start(out=out, in_=in_, **kw), rd, wr, kind='d')
    if store and i is not None:
        P.pending_st.append(i)
    return i


def flat(ap):
    nd = len(ap.shape)
    names = " ".join("a%d" % i for i in range(nd))
    return ap.rearrange("%s -> (%s)" % (names, names))


def build(c):
    nc = bass.Bass("TRN2", target_bir_lowering=False)
    P = Prog()
    D, KC, L, NT, NTh, NTOT, CTX = c.D, c.KC, c.L, c.NT, c.NTh, c.NTOT, c.CTX
    CG, H, H2, TB, PW, FC, NE = c.CG, c.H, c.H2, c.TB, c.PW, c.FC, c.NE
    NSC = c.NSC
    TBM = max(TB, CTX)

    def din(name, shape, dt=F32):
        return nc.dram_tensor(name, list(shape), dt, kind="ExternalInput")

    def dscr(name, shape, dt):
        return nc.dram_tensor(name, list(shape), dt, kind="Internal")

    x_sh = din("x_sh", [NT, D])
    ctx_in = din("ctx_in", [2 * CTX, D])
    cvec = din("cvec", [3, D])
    selLR = din("selLR", [128, 16])
    w_ada_sh = din("w_ada_sh", [L, D, c.NS])
    b_ada_sh = din("b_ada_sh", [L, c.NS])
    g_mix = din("g_mix", [L, D])
    g_ffn = din("g_ffn", [L, D])
    g_final = din("g_final", [D])
    w_in_sh = din("w_in_sh", [L, D // 8, c.PROJ_W])
    w_out_sh = din("w_out_sh", [L, D // 8, D])
    w_gate_sh = din("w_gate_sh", [L, 4 * D, c.FF])
    w_up_sh = din("w_up_sh", [L, 4 * D, c.FF])
    w_down_sh = din("w_down_sh", [L, 4 * c.FF, D])
    sc_w = din("sc_w", [L, 3, c.CONV_W])
    cf_w = din("cf_w", [L, 31, c.CFM_W])
    cf_b = din("cf_b", [L, c.CFM_W])
    ln_g = din("ln_g", [L, c.CFM_W])
    ln_b = din("ln_b", [L, c.CFM_W])
    lam_qk = din("lam_qk", [L, 4, 128])
    subln_g = din("subln_g", [L, 256])
    w_rg = din("w_rg", [L, D, 4])
    b_rg = din("b_rg", [L, 4])
    w_re = din("w_re", [L, D, 32])
    b_re = din("b_re", [L, 32])
    cosT_d = din("cosT", [128, NTh])
    sinT_d = din("sinT", [128, NTh])
    ident_d = din("ident", [128, 128])
    pm_d = din("pm", [128, 128])
    out_sh = nc.dram_tensor("out_sh", [NT, D], F32, kind="ExternalOutput")

    wspec = {'in': (w_in_sh, (D // 8) * c.PROJ_W), 'out': (w_out_sh, (D // 8) * D),
             'gate': (w_gate_sh, 4 * D * c.FF), 'up': (w_up_sh, 4 * D * c.FF), 'down': (w_down_sh, 4 * c.FF * D)}
    wl = {}
    wg = {}
    for nm, (src, nel) in wspec.items():
        for l in range(L):
            wl[(nm, l)] = dscr("wl_%s_%d" % (nm, l), [nel // 2048, 2048], BF16)
            wg[(nm, l)] = dscr("wg_%s_%d" % (nm, l), [8 * nel // 2048, 2048], BF16)
    h_scr = dscr("h_scr", [NTOT, D], F32)
    h1_scr = dscr("h1_scr", [NTOT, D], F32)
    s_scr = dscr("s_scr", [c.CONV_W, NTOT], F32)
    gb_scr = dscr("gb_scr", [c.CONV_W, NTOT], F32)
    g_scr = dscr("g_scr", [c.CFM_W, NTOT], F32)
    q_scr = dscr("q_scr", [H2 * 128, NTOT], BF16)
    kv_loc = dscr("kv_loc", [c.KVN // 2048, 2048], BF16)
    kvg = dscr("kvg", [8 * c.KVN // 2048, 2048], BF16)
    kc_scr = dscr("kc_scr", [H2 * 128, 2 * CTX], BF16)
    vc_scr = dscr("vc_scr", [2 * CTX, c.ATT_W], BF16)
    halo_loc = dscr("halo_loc", [c.CFM_W, 128], F32)
    halo_g = dscr("halo_g", [8 * c.CFM_W, 128], F32)
    mix_scr = dscr("mix_scr", [D, NTOT], BF16)
    MODN = L * 3 * 128 * NSC
    mod_loc = dscr("mod_loc", [1, MODN], F32)
    mod_all = dscr("mod_all", [8, MODN], F32)

    NK = H2 * 128 * NT
    kvl_flat = flat(kv_loc.ap())
    k_loc = kvl_flat[0:NK].rearrange("(r t) -> r t", t=NT)
    v_loc = kvl_flat[NK:c.KVN].rearrange("(t a) -> t a", a=c.ATT_W)
    kvg_flat = flat(kvg.ap())

    def kg_rank(r):
        return kvg_flat[r * c.KVN:r * c.KVN + NK].rearrange("(r t) -> r t", t=NT)

    def vg_rank(r):
        return kvg_flat[r * c.KVN + NK:(r + 1) * c.KVN].rearrange("(t a) -> t a", a=c.ATT_W)

    def wview(nm, l, cols):
        return flat(wg[(nm, l)].ap()).rearrange("(r x) -> r x", x=cols)

    RG = [list(range(8))]

    def AG(src_ap, dst_ap, rd, wr):
        P.op('pool', lambda e: e.collective_compute("AllGather", ALU.bypass, replica_groups=RG,
                                                    ins=[src_ap], outs=[dst_ap]), rd, wr, kind='cc')

    blocks = []
    for b in range(2):
        for k in range(NTh // TB):
            blocks.append((b * NTh + k * TB, TB, b, b, False, k * TB))
    for b in range(2):
        blocks.append((NT + b * CTX, CTX, 2, b, True, 0))

    cur_l = [0]
    hits = [0]

    def cut(name):
        st = getattr(c, 'stop', None)
        if st is None or P.dead:
            return
        sl = 0
        skip = 0
        if '#' in st:
            st, skip = st.split('#')
            skip = int(skip)
        if '@' in st:
            st, sl = st.split('@')
            sl = int(sl)
        if st == name and cur_l[0] == sl:
            hits[0] += 1
        if st == name and cur_l[0] == sl and hits[0] > skip:
            P.barrier()
            P.dead = True

    es = ExitStack()
    with es:
        uid = [0]

        def sb(name, shape, dt, stack=None):
            uid[0] += 1
            return (stack or es).enter_context(nc.sbuf_tensor("s%d_%s" % (uid[0], name), list(shape), dt))

        ps = es.enter_context(nc.psum_tensor("ps", [128, 8, 512], F32))

        def PSR(b):
            return ('ps', b)

        ident = sb("ident", [128, 128], F32)
        pm_f = sb("pm_f", [128, 128], F32)
        pm_b = sb("pm_b", [128, 128], BF16)
        ones_f = sb("ones_f", [128, 128], F32)
        ones_b = sb("ones_b", [128, 128], BF16)
        eps_t = sb("eps_t", [128, 1], F32)
        cosT = sb("cosT_s", [128, NTh], F32)
        sinT = sb("sinT_s", [128, NTh], F32)
        sel_t = sb("sel_t", [128, 16], F32)
        gmix_c = sb("gmix_c", [128, L, KC], F32)
        gffn_c = sb("gffn_c", [128, L, KC], F32)
        gfin_c = sb("gfin_c", [128, KC], F32)
        wsc_c = sb("wsc_c", [128, L, CG, 3], F32)
        wcf_c = sb("wcf_c", [128, L, CG, 31], F32)
        bcf_c = sb("bcf_c", [128, L, CG], F32)
        lng_c = sb("lng_c", [128, L, CG], F32)
        lnb_c = sb("lnb_c", [128, L, CG], F32)
        subg_c = sb("subg_c", [128, L, 2], F32)
        lam_c = sb("lam_c", [128, L, 4], F32)
        lamp = sb("lamp", [128, L, 2], F32)
        neglam = sb("neglam", [128, L], F32)
        modT = sb("modT", [128, L * 3, 6 * KC], F32)
        a1 = sb("a1", [128, L * 3, KC], F32)
        a2 = sb("a2", [128, L * 3, KC], F32)
        brow = sb("brow", [1, L, 36], F32)
        zpad = sb("zpad", [128, 60], F32)

        nc_ctx = nc.allow_non_contiguous_dma(reason="small column-layout parameter loads")
        nc_ctx.__enter__()
        try:

            DMA(P, 'sp', ident[:], ident_d.ap(), (), ['ident'])
            DMA(P, 'sp', pm_f[:], pm_d.ap(), (), ['pm_f'])
            COPY(P, 'dve', pm_b[:], pm_f[:], ['pm_f'], ['pm_b'])
            MEMSET(P, 'dve', ones_f[:], 1.0, ['ones_f'])
            MEMSET(P, 'dve', ones_b[:], 1.0, ['ones_b'])
            MEMSET(P, 'dve', eps_t[:], EPS, ['eps_t'])
            DMA(P, 'sp', cosT[:], cosT_d.ap(), (), ['cosT'])
            DMA(P, 'sp', sinT[:], sinT_d.ap(), (), ['sinT'])
            DMA(P, 'sp', sel_t[:], selLR.ap(), (), ['sel'])
            for l in range(L):
                DMA(P, 'sp', gmix_c[:, l, :], g_mix.ap()[l].rearrange("(k p) -> p k", p=128), (), ['gmix'])
                DMA(P, 'sp', gffn_c[:, l, :], g_ffn.ap()[l].rearrange("(k p) -> p k", p=128), (), ['gffn'])
                for g in range(CG):
                    DMA(P, 'sp', wsc_c[:, l, g, :], sc_w.ap()[l][:, g * 128:(g + 1) * 128].rearrange("k p -> p k"), (), ['wsc'])
                    DMA(P, 'sp', wcf_c[:, l, g, :], cf_w.ap()[l][:, g * 128:(g + 1) * 128].rearrange("k p -> p k"), (), ['wcf'])
                DMA(P, 'sp', bcf_c[:, l, :], cf_b.ap()[l].rearrange("(g p) -> p g", p=128), (), ['bcf'])
                DMA(P, 'sp', lng_c[:, l, :], ln_g.ap()[l].rearrange("(g p) -> p g", p=128), (), ['lng'])
                DMA(P, 'sp', lnb_c[:, l, :], ln_b.ap()[l].rearrange("(g p) -> p g", p=128), (), ['lnb'])
                DMA(P, 'sp', subg_c[:, l, :], subln_g.ap()[l].rearrange("(g p) -> p g", p=128), (), ['subg'])
                DMA(P, 'sp', lam_c[:, l, :], lam_qk.ap()[l].rearrange("k p -> p k"), (), ['lamc'])
                DMA(P, 'sp', brow[0:1, l, 0:4], b_rg.ap()[l:l + 1, :], (), ['brow'])
                DMA(P, 'sp', brow[0:1, l, 4:36], b_re.ap()[l:l + 1, :], (), ['brow'])
            DMA(P, 'sp', gfin_c[:], g_final.ap().rearrange("(k p) -> p k", p=128), (), ['gfin'])

            lam_init = [0.8 - 0.6 * math.exp(-0.3 * l) for l in range(L)]
            for l in range(L):
                TT(P, 'dve', lamp[:, l, 0:1], lam_c[:, l, 0:1], lam_c[:, l, 1:2], ALU.mult, ['lamc'], ['lamp'])
                TT(P, 'dve', lamp[:, l, 1:2], lam_c[:, l, 2:3], lam_c[:, l, 3:4], ALU.mult, ['lamc', 'lamp'], ['lamp'])
            lamflat = lamp[:].rearrange("p l k -> p (l k)")
            MM(P, ps[:, 7, 0:2 * L], ones_f[:], lamflat, True, True, ['ones_f', 'lamp'], [PSR(7)])
            ACTV(P, lamp[:].rearrange("p l k -> p (l k)"), ps[:, 7, 0:2 * L], AF.Exp, [PSR(7)], ['lamp'])
            for l in range(L):
                STT(P, neglam[:, l:l + 1], lamp[:, l, 1:2], -lam_init[l], lamp[:, l, 0:1], ALU.add, ALU.subtract,
                    ['lamp'], ['neglam'])
                TS(P, 'dve', subg_c[:, l, :], subg_c[:, l, :], 1.0 - lam_init[l], None, ALU.mult, None, ['subg'], ['subg'])

            cast_ids = []
            for l in range(L):
                for nm in ('in', 'out', 'gate', 'up', 'down'):
                    src, nel = wspec[nm]
                    sv = flat(src.ap()[l]).rearrange("(a x) -> a x", x=2048)
                    prevs = set(cast_ids[-2:-1])
                    cid = P.op('pool', (lambda o, i_: (lambda e: e.dma_start(out=o, in_=i_)))(wl[(nm, l)].ap(), sv), (), [('wl', nm, l)], kind='d', extra=prevs)
                    cast_ids.append(cid)

            def gather_w(nm, l):
                AG(wl[(nm, l)].ap(), wg[(nm, l)].ap(), [('wl', nm, l)], [('wg', nm, l)])

            with ExitStack() as s1:
                crow = sb("crow", [3, D], F32, s1)
                cT = sb("cT", [128, KC, 4], F32, s1)
                modl = sb("modl", [128, L * 3, NSC], F32, s1)
                bcol = sb("bcol", [128, L, NSC], F32, s1)
                wad = [sb("wad%d" % i, [128, KC, 256], F32, s1) for i in range(2)]
                DMA(P, 'sp', crow[:], cvec.ap(), (), ['crow'])
                ACTV(P, crow[:], crow[:], AF.Silu, ['crow'], ['crow'])
                for l in range(L):
                    DMA(P, 'sp', bcol[:, l, :], b_ada_sh.ap()[l].rearrange("(n p) -> p n", p=128), (), ['bcol'])
                for k in range(KC):
                    TR(P, ps[:, 7, 0:3], crow[0:3, k * 128:(k + 1) * 128], ident[0:3, 0:3], ['crow', 'ident'], [PSR(7)])
                    COPY(P, 'dve', cT[:, k, 0:3], ps[:, 7, 0:3], [PSR(7)], ['cT'])
                npan = c.NS // 256
                pi = 0
                for l in range(L):
                    for pn in range(npan):
                        wt = wad[pi % 2]
                        wr_ = ('wad', pi % 2)
                        pi += 1
                        DMA(P, 'sp', wt[:], w_ada_sh.ap()[l][:, pn * 256:(pn + 1) * 256].rearrange("(k p) x -> p k x", p=128),
                            (), [wr_])
                        for j2 in range(2):
                            n = pn * 2 + j2
                            bk = 4 + (n % 2)
                            for k in range(KC):
                                MM(P, ps[:, bk, 0:3], wt[:, k, j2 * 128:(j2 + 1) * 128], cT[:, k, 0:3], k == 0, k == KC - 1,
                                   [wr_, 'cT'], [PSR(bk)])
                            TS(P, 'dve', modl[:, l * 3:(l + 1) * 3, n], ps[:, bk, 0:3], bcol[:, l, n:n + 1], None, ALU.add, None,
                               [PSR(bk), 'bcol'], ['modl'])
                for l in range(L):
                    dst = mod_loc.ap()[0, l * 3 * 128 * NSC:(l + 1) * 3 * 128 * NSC].rearrange("(j p n) -> p j n", j=3, p=128)
                    DMA(P, 'sp', dst, modl[:, l * 3:(l + 1) * 3, :], ['modl'], ['mod_loc'], store=True)
                AG(mod_loc.ap(), mod_all.ap(), ['mod_loc'], ['mod_all'])
                for l in range(L):
                    for j in range(3):
                        o0 = (l * 3 + j) * 128 * NSC
                        src = mod_all.ap()[:, o0:o0 + 128 * NSC].rearrange("r (p n) -> p r n", p=128)
                        DMA(P, 'sp', modT[:, l * 3 + j, :].rearrange("p (r n) -> p r n", r=8), src, ['mod_all'], ['modT'])
                for l in range(L):
                    for j in range(3):
                        i = l * 3 + j
                        TS(P, 'dve', a1[:, i, :], modT[:, i, KC:2 * KC], 1.0, None, ALU.add, None, ['modT'], ['a1'])
                        TT(P, 'dve', a1[:, i, :], a1[:, i, :], gmix_c[:, l, :], ALU.mult, ['a1', 'gmix'], ['a1'])
                        TS(P, 'dve', a2[:, i, :], modT[:, i, 4 * KC:5 * KC], 1.0, None, ALU.add, None, ['modT'], ['a2'])
                        TT(P, 'dve', a2[:, i, :], a2[:, i, :], gffn_c[:, l, :], ALU.mult, ['a2', 'gffn'], ['a2'])
            gather_w('in', 0)
            P.barrier()
            cut('ada')

            def mvec(l, j, which):
                return modT[:, l * 3 + j, which * KC:(which + 1) * KC]

            def bcast_cols(col_ap, dst, rd, wr, tmp, tmpname):
                for k0 in range(0, KC, 4):
                    for k in range(k0, min(KC, k0 + 4)):
                        TS(P, 'dve', tmp[:], ident[:], col_ap[:, k:k + 1], None, ALU.mult, None, ['ident'] + rd, [tmpname])
                        MM(P, ps[:, 7, (k - k0) * 128:(k - k0 + 1) * 128], ones_f[:], tmp[:], True, True,
                           ['ones_f', tmpname], [PSR(7)])
                    w = min(KC, k0 + 4) - k0
                    ACTV(P, dst[:, k0 * 128:(k0 + w) * 128], ps[:, 7, 0:w * 128], AF.Copy, [PSR(7)], wr)

            def hsrc(l, row0, n):
                if l == 0:
                    if row0 < NT:
                        return x_sh.ap()[row0:row0 + n, :]
                    return ctx_in.ap()[row0 - NT:row0 - NT + n, :]
                return h_scr.ap()[row0:row0 + n, :]

            def norm_tile(l, j, src_ap, hb, hbn, avec, shvec, small, dst_writer, extra_rd):
                DMA(P, 'sp', hb[:], src_ap, extra_rd, [hbn])
                ss, rt = small
                cut('n1')
                ACTV(P, dst_junk[0][:], hb[:], AF.Square, [hbn], [dst_junk[1], 'ss'], accum=ss[:])
                cut('n2')
                TS(P, 'dve', rt[:], ss[:], 1.0 / D, EPS, ALU.mult, ALU.add, ['ss'], ['rt'])
                ACTV(P, rt[:], rt[:], AF.Ln, ['rt'], ['rt'])
                cut('n3')
                RSQ(P, rt[:], ['rt'], ['rt'])
                cut('n4')
                TS(P, 'dve', hb[:], hb[:], rt[:, 0:1], None, ALU.mult, None, [hbn, 'rt'], [hbn])
                cut('n5')
                for k0 in range(0, KC, 4):
                    bk = 4 + (k0 // 4) % 2
                    for k in range(k0, k0 + 4):
                        TR(P, ps[:, bk, (k - k0) * 128:(k - k0 + 1) * 128], hb[:, k * 128:(k + 1) * 128], ident[:],
                           [hbn, 'ident'], [PSR(bk)])
                    cut('n6')
                    for k in range(k0, k0 + 4):
                        dst_writer(k, ps[:, bk, (k - k0) * 128:(k - k0 + 1) * 128], PSR(bk))
                        cut('n7')

            dst_junk = [None, None]

            for l in range(L):
                last = (l == L - 1)
                cur_l[0] = l
                with ExitStack() as s1:
                    hb = sb("p1_hb", [128, D], F32, s1)
                    junk = sb("p1_junk", [128, D], BF16, s1)
                    dst_junk[0], dst_junk[1] = junk, 'junk'
                    ss = sb("p1_ss", [128, 1], F32, s1)
                    rt = sb("p1_rt", [128, 1], F32, s1)
                    uT = sb("p1_uT", [128, KC, TBM], BF16, s1)
                    wr_ring = [sb("p1_w%d" % i, [128, KC, PW], BF16, s1) for i in range(3)]
                    xs = sb("p1_xs", [128, CG, TBM], F32, s1)
                    st32 = [sb("p1_st32_%d" % i, [128, 2, TBM], F32, s1) for i in range(2)]
                    st16 = [sb("p1_st16_%d" % i, [128, 2, TBM], BF16, s1) for i in range(2)]
                    stv = [sb("p1_stv_%d" % i, [128, PW], BF16, s1) for i in range(2)]
                    qs = sb("p1_qs", [128, 2, TBM], BF16, s1)
                    t1 = sb("p1_t1", [128, TBM], F32, s1)
                    t2 = sb("p1_t2", [128, TBM], F32, s1)
                    win = wview('in', l, c.PROJ_W)
                    wi = 0
                    s32i = 0
                    s16i = 0
                    svi = 0
                    for bi_, (row0, n, j, b, is_ctx, tok0) in enumerate(blocks):
                        cut('p1b%d' % bi_)
                        if is_ctx and last:
                            segs = [('k', c.OFF_K, c.QK_W), ('v', c.OFF_V, c.ATT_W)]
                        else:
                            segs = [('xa', 0, c.CONV_W), ('gc', 2 * c.CONV_W, c.CONV_W), ('gb', c.CONV_W, c.CONV_W),
                                    ('q', c.OFF_Q, c.QK_W), ('k', c.OFF_K, c.QK_W),
                                    ('ga', c.OFF_C, c.CFM_W), ('gg', c.OFF_C + c.CFM_W, c.CFM_W), ('v', c.OFF_V, c.ATT_W)]
                        av, shv = a1[:, l * 3 + j, :], mvec(l, j, 0)
                        for t in range(n // 128):
                            def wrt(k, pss, psr, t=t, av=av, shv=shv):
                                if False:
                                    ACTV(P, uT[:, k, t * 128:(t + 1) * 128], pss, AF.Identity, [psr, 'a1', 'modT'], ['uT'],
                                         bias=shv[:, k:k + 1], scale=av[:, k:k + 1])
                                else:
                                    TS(P, 'dve', uT[:, k, t * 128:(t + 1) * 128], pss, av[:, k:k + 1], shv[:, k:k + 1],
                                       ALU.mult, ALU.add, [psr, 'a1', 'modT'], ['uT'])
                            norm_tile(l, j, hsrc(l, row0 + t * 128, 128), hb, 'hb', av, shv, (ss, rt), wrt,
                                      [('h', l)])
                        cut('p1n')
                        for (kind, off, width) in segs:
                            cut('p1_' + kind)
                            for pn in range(width // PW):
                                col0 = off + pn * PW
                                w = wr_ring[wi % 3]
                                wn = ('p1w', wi % 3)
                                wi += 1
                                DMA(P, 'sp', w[:], win[:, col0:col0 + PW].rearrange("(k p) x -> p k x", p=128),
                                    [('wg', 'in', l)], [wn])
                                if kind == 'v':
                                    for t in range(n // 128):
                                        bk = t % 4
                                        for k in range(KC):
                                            MM(P, ps[:, bk, 0:PW], uT[:, k, t * 128:(t + 1) * 128], w[:, k, :], k == 0, k == KC - 1,
                                               ['uT', wn], [PSR(bk)])
                                        sv = st16[s16i % 2][:, 0, 0:PW]
                                        svn = ('st16', s16i % 2)
                                        s16i += 1
                                        if getattr(c, 'vx', '') != 'noevac':
                                            ACTV(P, sv, ps[:, bk, 0:PW], AF.Copy, [PSR(bk)], [svn])
                                        c0 = pn * PW
                                        if getattr(c, 'vx', '') in ('noevac', 'nostore'):
                                            pass
                                        elif is_ctx:
                                            dst = vc_scr.ap()[b * CTX + t * 128:b * CTX + (t + 1) * 128, c0:c0 + PW]
                                            DMA(P, 'sp', dst, sv, [svn], [('vc', l)], store=True)
                                        else:
                                            vx = getattr(c, 'vx', '')
                                            if vx == 'vc':
                                                dst = vc_scr.ap()[row0 + t * 128:row0 + (t + 1) * 128, c0:c0 + PW]
                                                DMA(P, 'sp', dst, sv, [svn], [('kvl', l)], store=True)
                                            elif vx == 'half':
                                                dst = v_loc[row0 + t * 128:row0 + (t + 1) * 128, c0:c0 + 128]
                                                DMA(P, 'sp', dst, sv[:, 0:128], [svn], [('kvl', l)], store=True)
                                            else:
                                                dst = v_loc[row0 + t * 128:row0 + (t + 1) * 128, c0:c0 + PW]
                                                DMA(P, 'sp', dst, sv, [svn], [('kvl', l)], store=True)
                                    continue
                                base_bk = 2 * (pn % 2)
                                for cc in range(2):
                                    bk = base_bk + cc
                                    for k in range(KC):
                                        MM(P, ps[:, bk, 0:n], w[:, k, cc * 128:(cc + 1) * 128], uT[:, k, 0:n], k == 0, k == KC - 1,
                                           ['uT', wn], [PSR(bk)])
                                gch = pn * 2
                                if kind == 'xa':
                                    for cc in range(2):
                                        ACTV(P, xs[:, gch + cc, 0:n], ps[:, base_bk + cc, 0:n], AF.Copy, [PSR(base_bk + cc)], ['xs'])
                                elif kind == 'ga':
                                    for cc in range(2):
                                        ACTV(P, xs[:, gch + cc, 0:n], ps[:, base_bk + cc, 0:n], AF.Copy, [PSR(base_bk + cc)], ['xs'])
                                elif kind == 'gg':
                                    st = st32[s32i % 2]
                                    stn = ('st32', s32i % 2)
                                    s32i += 1
                                    for cc in range(2):
                                        ACTV(P, st[:, cc, 0:n], ps[:, base_bk + cc, 0:n], AF.Exp, [PSR(base_bk + cc)], [stn], scale=-1.0)
                                        TS(P, 'dve', st[:, cc, 0:n], st[:, cc, 0:n], 1.0, None, ALU.add, None, [stn], [stn])
                                        RECIP(P, st[:, cc, 0:n], st[:, cc, 0:n], [stn], [stn])
                                        TT(P, 'dve', st[:, cc, 0:n], st[:, cc, 0:n], xs[:, gch + cc, 0:n], ALU.mult, [stn, 'xs'], [stn])
                                    for c2 in range(2):
                                        DMA(P, 'sp', g_scr.ap()[(gch + c2) * 128:(gch + c2 + 1) * 128, row0:row0 + n], st[:, c2, 0:n], [stn], [('ga', l)], store=True)
                                elif kind in ('gc', 'gb'):
                                    st = st32[s32i % 2]
                                    stn = ('st32', s32i % 2)
                                    s32i += 1
                                    for cc in range(2):
                                        if kind == 'gb':
                                            ACTV(P, st[:, cc, 0:n], ps[:, base_bk + cc, 0:n], AF.Copy, [PSR(base_bk + cc)], [stn])
                                        else:
                                            TT(P, 'dve', st[:, cc, 0:n], ps[:, base_bk + cc, 0:n], xs[:, gch + cc, 0:n], ALU.mult,
                                               [PSR(base_bk + cc), 'xs'], [stn])
                                    scr = {'gc': s_scr, 'gb': gb_scr}[kind]
                                    for c2 in range(2):
                                        DMA(P, 'sp', scr.ap()[(gch + c2) * 128:(gch + c2 + 1) * 128, row0:row0 + n], st[:, c2, 0:n], [stn], [(kind, l)], store=True)
                                else:
                                    st = st16[s16i % 2]
                                    stn = ('st16', s16i % 2)
                                    s16i += 1
                                    for cc in range(2):
                                        bk = base_bk + cc
                                        if is_ctx or getattr(c, 'norope', False):
                                            ACTV(P, st[:, cc, 0:n], ps[:, bk, 0:n], AF.Copy, [PSR(bk)], [stn])
                                            continue
                                        ACTV(P, qs[:, cc, 0:n], ps[:, bk, 0:n], AF.Copy, [PSR(bk)], ['qs'])
                                        rb = 6 + cc
                                        MM(P, ps[:, rb, 0:n], pm_b[:], qs[:, cc, 0:n], True, True, ['pm_b', 'qs'], [PSR(rb)])
                                        TT(P, 'dve', t2[:, 0:n], ps[:, rb, 0:n], sinT[:, tok0:tok0 + n], ALU.mult,
                                           [PSR(rb), 'sinT'], ['t2'])
                                        TT(P, 'dve', t1[:, 0:n], qs[:, cc, 0:n], cosT[:, tok0:tok0 + n], ALU.mult,
                                           ['qs', 'cosT'], ['t1'])
                                        TT(P, 'dve', st[:, cc, 0:n], t1[:, 0:n], t2[:, 0:n], ALU.add, ['t1', 't2'], [stn])
                                    if kind == 'q':
                                        for c2 in range(2):
                                            DMA(P, 'sp', q_scr.ap()[(gch + c2) * 128:(gch + c2 + 1) * 128, row0:row0 + n], st[:, c2, 0:n], [stn], [('q', l)], store=True)
                                    elif is_ctx:
                                        for c2 in range(2):
                                            DMA(P, 'sp', kc_scr.ap()[(gch + c2) * 128:(gch + c2 + 1) * 128, b * CTX:b * CTX + n], st[:, c2, 0:n], [stn], [('kc', l)], store=True)
                                    else:
                                        for c2 in range(2):
                                            DMA(P, 'sp', k_loc[(gch + c2) * 128:(gch + c2 + 1) * 128, row0:row0 + n], st[:, c2, 0:n], [stn], [('kvl', l)], store=True)
                P.barrier()
                cut('p1')

                AG(kv_loc.ap(), kvg.ap(), [('kvl', l)], [('kvg', l)])
                if l == 0:
                    MEMSET(P, 'dve', zpad[:], 0.0, ['zpad'])
                    for g in range(CG):
                        DMA(P, 'sp', halo_loc.ap()[g * 128:(g + 1) * 128, 68:128], zpad[:], ['zpad'], ['halo_loc'], store=True)
                for b in range(2):
                    hl = halo_loc.ap()
                    DMA(P, 'sp', hl[:, b * 34:b * 34 + 16], g_scr.ap()[:, b * NTh:b * NTh + 16], [('ga', l)], ['halo_loc'], store=True)
                    DMA(P, 'sp', hl[:, b * 34 + 16:b * 34 + 32], g_scr.ap()[:, (b + 1) * NTh - 16:(b + 1) * NTh], [('ga', l)],
                        ['halo_loc'], store=True)
                    DMA(P, 'sp', hl[:, b * 34 + 32:b * 34 + 33], s_scr.ap()[:, b * NTh:b * NTh + 1], [('gc', l)], ['halo_loc'], store=True)
                    DMA(P, 'sp', hl[:, b * 34 + 33:b * 34 + 34], s_scr.ap()[:, (b + 1) * NTh - 1:(b + 1) * NTh], [('gc', l)],
                        ['halo_loc'], store=True)
                AG(halo_loc.ap(), halo_g.ap(), ['halo_loc'], ['halo_g'])
                for nm in ('out', 'gate', 'up', 'down'):
                    gather_w(nm, l)
                if not last:
                    gather_w('in', l + 1)

                cut('xchg')
                with ExitStack() as s1:
                    SEGM = max(NTh, CTX)
                    CH = min(512, SEGM)
                    hal = sb("cv_hal", [128, CG, 8, 128], F32, s1)
                    hL = sb("cv_hL", [128, 2, CG, 16], F32, s1)
                    hR = sb("cv_hR", [128, 2, CG, 16], F32, s1)
                    sLR = sb("cv_sLR", [128, 2, CG, 2], F32, s1)
                    gbuf = [sb("cv_gbuf%d" % i, [128, SEGM + 32], F32, s1) for i in range(2)]
                    zall = sb("cv_z", [128, CG, SEGM], F32, s1)
                    zsq = [sb("cv_zsq%d" % i, [128, CH], F32, s1) for i in range(2)]
                    mu = sb("cv_mu", [128, CH], F32, s1)
                    msq = sb("cv_msq", [128, CH], F32, s1)
                    rs = sb("cv_rs", [128, CH], F32, s1)
                    tt = [sb("cv_tt%d" % i, [128, CH], F32, s1) for i in range(2)]
                    yst = [sb("cv_yst%d" % i, [128, 2, CH], BF16, s1) for i in range(2)]
                    sbuf_ = [sb("cv_s%d" % i, [128, SEGM + 32], F32, s1) for i in range(2)]
                    gbt = [sb("cv_gb%d" % i, [128, SEGM], F32, s1) for i in range(2)]
                    pa = [sb("cv_pa%d" % i, [128, SEGM], F32, s1) for i in range(2)]
                    pb = sb("cv_pb", [128, SEGM], F32, s1)
                    ysh = [sb("cv_ysh%d" % i, [128, 2, SEGM], BF16, s1) for i in range(2)]
                    for g in range(CG):
                        src = halo_g.ap().rearrange("(r x) y -> x r y", r=8)[g * 128:(g + 1) * 128]
                        DMA(P, 'sp', hal[:, g, :, :], src, ['halo_g'], ['hal'])
                    cut('cvl')
                    for b in range(2):
                        for r in range(8):
                            o = b * 34
                            if r == 0:
                                TS(P, 'dve', hL[:, b, :, 0:15], hal[:, :, r, o + 17:o + 32], sel_t[:, r:r + 1], None, ALU.mult, None,
                                   ['hal', 'sel'], ['hL'])
                                TS(P, 'dve', hR[:, b, :, 0:15], hal[:, :, r, o:o + 15], sel_t[:, 8 + r:9 + r], None, ALU.mult, None,
                                   ['hal', 'sel'], ['hR'])
                                TS(P, 'dve', sLR[:, b, :, 0:1], hal[:, :, r, o + 33:o + 34], sel_t[:, r:r + 1], None, ALU.mult, None,
                                   ['hal', 'sel'], ['sLR'])
                                TS(P, 'dve', sLR[:, b, :, 1:2], hal[:, :, r, o + 32:o + 33], sel_t[:, 8 + r:9 + r], None, ALU.mult, None,
                                   ['hal', 'sel'], ['sLR'])
                            else:
                                STT(P, hL[:, b, :, 0:15], hal[:, :, r, o + 17:o + 32], sel_t[:, r:r + 1], hL[:, b, :, 0:15],
                                    ALU.mult, ALU.add, ['hal', 'sel', 'hL'], ['hL'])
                                STT(P, hR[:, b, :, 0:15], hal[:, :, r, o:o + 15], sel_t[:, 8 + r:9 + r], hR[:, b, :, 0:15],
                                    ALU.mult, ALU.add, ['hal', 'sel', 'hR'], ['hR'])
                                STT(P, sLR[:, b, :, 0:1], hal[:, :, r, o + 33:o + 34], sel_t[:, r:r + 1], sLR[:, b, :, 0:1],
                                    ALU.mult, ALU.add, ['hal', 'sel', 'sLR'], ['sLR'])
                                STT(P, sLR[:, b, :, 1:2], hal[:, :, r, o + 32:o + 33], sel_t[:, 8 + r:9 + r], sLR[:, b, :, 1:2],
                                    ALU.mult, ALU.add, ['hal', 'sel', 'sLR'], ['sLR'])
                    cut('cvh')
                    segs = [(b * NTh, NTh, b, False) for b in range(2)]
                    if not last:
                        segs += [(NT + b * CTX, CTX, b, True) for b in range(2)]
                    gi = 0
                    for (row0, n, b, is_ctx) in segs:
                        for g in range(CG):
                            gb_ = gbuf[gi % 2]
                            gbn = ('gbuf', gi % 2)
                            DMA(P, 'sp', gb_[:, 16:16 + n], g_scr.ap()[g * 128:(g + 1) * 128, row0:row0 + n], [('ga', l)], [gbn])
                            if is_ctx:
                                MEMSET(P, 'dve', gb_[:, 1:16], 0.0, [gbn])
                                MEMSET(P, 'dve', gb_[:, 16 + n:31 + n], 0.0, [gbn])
                            else:
                                COPY(P, 'dve', gb_[:, 1:16], hL[:, b, g, 0:15], ['hL'], [gbn])
                                COPY(P, 'dve', gb_[:, 16 + n:31 + n], hR[:, b, g, 0:15], ['hR'], [gbn])
                            zr = ('z', g)
                            TS(P, 'dve', zall[:, g, 0:n], gb_[:, 1:1 + n], wcf_c[:, l, g, 0:1], bcf_c[:, l, g:g + 1], ALU.mult, ALU.add,
                               [gbn, 'wcf', 'bcf'], [zr])
                            for k in range(1, 31):
                                STT(P, zall[:, g, 0:n], gb_[:, k + 1:k + 1 + n], wcf_c[:, l, g, k:k + 1], zall[:, g, 0:n], ALU.mult, ALU.add,
                                    [gbn, 'wcf', zr], [zr])
                            cut('cv1')
                            s_ = sbuf_[gi % 2]
                            sn = ('sbuf', gi % 2)
                            g2 = gbt[gi % 2]
                            g2n = ('gbt', gi % 2)
                            p_a = pa[gi % 2]
                            pan = ('pa', gi % 2)
                            y_ = ysh[gi % 2]
                            yn = ('ysh', gi % 2)
                            DMA(P, 'sp', s_[:, 16:16 + n], s_scr.ap()[g * 128:(g + 1) * 128, row0:row0 + n], [('gc', l)], [sn])
                            DMA(P, 'sp', g2[:, 0:n], gb_scr.ap()[g * 128:(g + 1) * 128, row0:row0 + n], [('gb', l)], [g2n])
                            if is_ctx:
                                MEMSET(P, 'dve', s_[:, 15:16], 0.0, [sn])
                                MEMSET(P, 'dve', s_[:, 16 + n:17 + n], 0.0, [sn])
                            else:
                                COPY(P, 'dve', s_[:, 15:16], sLR[:, b, g, 0:1], ['sLR'], [sn])
                                COPY(P, 'dve', s_[:, 16 + n:17 + n], sLR[:, b, g, 1:2], ['sLR'], [sn])
                            TS(P, 'dve', p_a[:, 0:n], s_[:, 15:15 + n], wsc_c[:, l, g, 0:1], None, ALU.mult, None, [sn, 'wsc'], [pan])
                            TS(P, 'dve', pb[:, 0:n], s_[:, 16:16 + n], wsc_c[:, l, g, 1:2], None, ALU.mult, None, [sn, 'wsc'], ['pb'])
                            TT(P, 'dve', p_a[:, 0:n], p_a[:, 0:n], pb[:, 0:n], ALU.add, [pan, 'pb'], [pan])
                            TS(P, 'dve', pb[:, 0:n], s_[:, 17:17 + n], wsc_c[:, l, g, 2:3], None, ALU.mult, None, [sn, 'wsc'], ['pb'])
                            TT(P, 'dve', p_a[:, 0:n], p_a[:, 0:n], pb[:, 0:n], ALU.add, [pan, 'pb'], [pan])
                            TT(P, 'dve', y_[:, 0, 0:n], p_a[:, 0:n], g2[:, 0:n], ALU.mult, [pan, g2n], [yn])
                            DMA(P, 'sp', mix_scr.ap()[g * 128:(g + 1) * 128, row0:row0 + n], y_[:, 0, 0:n], [yn], [('mix', l)], store=True)
                            gi += 1
                        cut('cv2')
                        zi = 0
                        for c0 in range(0, n, CH):
                            m = min(CH, n - c0)
                            for g in range(CG):
                                MM(P, ps[:, 0, 0:m], ones_f[:], zall[:, g, c0:c0 + m], g == 0, g == CG - 1, ['ones_f', ('z', g)], [PSR(0)])
                            for g in range(CG):
                                zq = zsq[zi % 2]
                                zqn = ('zsq', zi % 2)
                                zi += 1
                                TT(P, 'dve', zq[:, 0:m], zall[:, g, c0:c0 + m], zall[:, g, c0:c0 + m], ALU.mult, [('z', g)], [zqn])
                                MM(P, ps[:, 1, 0:m], ones_f[:], zq[:, 0:m], g == 0, g == CG - 1, ['ones_f', zqn], [PSR(1)])
                            ACTV(P, mu[:, 0:m], ps[:, 0, 0:m], AF.Copy, [PSR(0)], ['mu'], scale=1.0 / c.CFM_W)
                            TT(P, 'dve', msq[:, 0:m], mu[:, 0:m], mu[:, 0:m], ALU.mult, ['mu'], ['msq'])
                            STT(P, rs[:, 0:m], ps[:, 1, 0:m], 1.0 / c.CFM_W, msq[:, 0:m], ALU.mult, ALU.subtract, [PSR(1), 'msq'], ['rs'])
                            TS(P, 'dve', rs[:, 0:m], rs[:, 0:m], 1.0, EPS, ALU.mult, ALU.add, ['rs'], ['rs'])
                            ACTV(P, rs[:, 0:m], rs[:, 0:m], AF.Ln, ['rs'], ['rs'])
                            RSQ(P, rs[:, 0:m], ['rs'], ['rs'])
                            for g in range(CG):
                                t_ = tt[g % 2]
                                tn = ('tt', g % 2)
                                y_ = yst[g % 2]
                                yn = ('yst', g % 2)
                                TT(P, 'dve', t_[:, 0:m], zall[:, g, c0:c0 + m], mu[:, 0:m], ALU.subtract, [('z', g), 'mu'], [tn])
                                TT(P, 'dve', t_[:, 0:m], t_[:, 0:m], rs[:, 0:m], ALU.mult, [tn, 'rs'], [tn])
                                TS(P, 'dve', t_[:, 0:m], t_[:, 0:m], lng_c[:, l, g:g + 1], lnb_c[:, l, g:g + 1], ALU.mult, ALU.add, [tn, 'lng', 'lnb'], [tn])
                                ACTV(P, y_[:, 0, 0:m], t_[:, 0:m], AF.Silu, [tn], [yn])

                                ch = CG + H2 + g
                                DMA(P, 'sp', mix_scr.ap()[ch * 128:(ch + 1) * 128, row0 + c0:row0 + c0 + m], y_[:, 0, 0:m], [yn],
                                    [('mix', l)], store=True)
                P.barrier()
                cut('conv')

                with ExitStack() as s1:
                    NKEY = c.SEQ + CTX
                    NKT = NKEY // 128
                    LKT = c.SEQ // 128
                    kT = [sb("at_kT%d" % i, [128, 2, NKEY], BF16, s1) for i in range(2)]
                    vt = sb("at_vt", [128, NKT, 256], BF16, s1)
                    qT = [sb("at_qT%d" % i, [128, 2, NTh + CTX], BF16, s1) for i in range(2)]
                    pT = [sb("at_pT%d" % i, [128, 512], BF16, s1) for i in range(4)]
                    om = [sb("at_om%d" % i, [128, 2, 512], F32, s1) for i in range(2)]
                    rl = sb("at_rl", [128, 512], F32, s1)
                    att = sb("at_att", [128, 2, 512], F32, s1)
                    sq = sb("at_sq", [128, 2, 512], F32, s1)
                    rsd = sb("at_rsd", [128, 512], F32, s1)
                    ybf = [sb("at_y%d" % i, [128, 2, 512], BF16, s1) for i in range(2)]
                    scale = 128.0 ** -0.5
                    hi = 0
                    pi = 0
                    yi = 0
                    for b in range(2):
                        for h in range(H):
                            kt_ = kT[hi % 2]
                            ktn = ('kT', hi % 2)
                            qt_ = qT[hi % 2]
                            qtn = ('qT', hi % 2)
                            hi += 1
                            for r in range(8):
                                src = kg_rank(r)[2 * h * 128:(2 * h + 2) * 128, b * NTh:(b + 1) * NTh].rearrange("(m p) t -> p m t", p=128)
                                DMA(P, 'sp', kt_[:, :, r * NTh:(r + 1) * NTh], src, [('kvg', l)], [ktn])
                                src = vg_rank(r)[b * NTh:(b + 1) * NTh, h * 256:(h + 1) * 256].rearrange("(k p) x -> p k x", p=128)
                                DMA(P, 'sp', vt[:, r * (NTh // 128):(r + 1) * (NTh // 128), :], src, [('kvg', l)], ['vt'])
                            src = kc_scr.ap()[2 * h * 128:(2 * h + 2) * 128, b * CTX:(b + 1) * CTX].rearrange("(m p) t -> p m t", p=128)
                            DMA(P, 'sp', kt_[:, :, c.SEQ:NKEY], src, [('kc', l)], [ktn])
                            src = vc_scr.ap()[b * CTX:(b + 1) * CTX, h * 256:(h + 1) * 256].rearrange("(k p) x -> p k x", p=128)
                            DMA(P, 'sp', vt[:, LKT:NKT, :], src, [('vc', l)], ['vt'])
                            src = q_scr.ap()[2 * h * 128:(2 * h + 2) * 128, b * NTh:(b + 1) * NTh].rearrange("(m p) t -> p m t", p=128)
                            DMA(P, 'sp', qt_[:, :, 0:NTh], src, [('q', l)], [qtn])
                            if not last:
                                src = q_scr.ap()[2 * h * 128:(2 * h + 2) * 128, NT + b * CTX:NT + (b + 1) * CTX].rearrange(
                                    "(m p) t -> p m t", p=128)
                                DMA(P, 'sp', qt_[:, :, NTh:NTh + CTX], src, [('q', l)], [qtn])
                            qblocks = [(k * TB, TB, 0, NKT, b * NTh + k * TB) for k in range(NTh // TB)]
                            if not last:
                                qblocks.append((NTh, CTX, LKT, NKT, NT + b * CTX))
                            for (q0, nq, kt0, kt1, orow) in qblocks:
                                for m in range(2):
                                    kts = list(range(kt0, kt1))

                                    def S(i):
                                        kt = kts[i]
                                        bk = i % 3
                                        MM(P, ps[:, bk, 0:nq], kt_[:, m, kt * 128:(kt + 1) * 128], qt_[:, m, q0:q0 + nq], True, True,
                                           [ktn, qtn], [PSR(bk)])

                                    def AV(i, pi):
                                        kt = kts[i]
                                        bk = i % 3
                                        p_ = pT[pi % 4]
                                        pn = ('pT', pi % 4)
                                        ACTV(P, p_[:, 0:nq], ps[:, bk, 0:nq], AF.Exp, [PSR(bk)], [pn], scale=scale)
                                        first, lastk = (i == 0), (i == len(kts) - 1)
                                        MM(P, ps[:, 3, 0:nq], vt[:, kt, 0:128], p_[:, 0:nq], first, lastk, ['vt', pn], [PSR(3)])
                                        MM(P, ps[:, 4, 0:nq], vt[:, kt, 128:256], p_[:, 0:nq], first, lastk, ['vt', pn], [PSR(4)])
                                        MM(P, ps[:, 5, 0:nq], ones_b[:], p_[:, 0:nq], first, lastk, ['ones_b', pn], [PSR(5)])

                                    S(0)
                                    for i in range(len(kts)):
                                        if i + 1 < len(kts):
                                            S(i + 1)
                                        AV(i, pi)
                                        pi += 1
                                    RECIP(P, rl[:, 0:nq], ps[:, 5, 0:nq], [PSR(5)], ['rl'])
                                    omr = ('om', m)
                                    TT(P, 'dve', om[m][:, 0, 0:nq], ps[:, 3, 0:nq], rl[:, 0:nq], ALU.mult, [PSR(3), 'rl'], [omr])
                                    TT(P, 'dve', om[m][:, 1, 0:nq], ps[:, 4, 0:nq], rl[:, 0:nq], ALU.mult, [PSR(4), 'rl'], [omr])
                                STT(P, att[:, :, 0:nq], om[1][:, :, 0:nq], neglam[:, l:l + 1], om[0][:, :, 0:nq], ALU.mult, ALU.add,
                                    [('om', 0), ('om', 1), 'neglam'], ['att'])
                                TT(P, 'dve', sq[:, :, 0:nq], att[:, :, 0:nq], att[:, :, 0:nq], ALU.mult, ['att'], ['sq'])
                                MM(P, ps[:, 6, 0:nq], ones_f[:], sq[:, 0, 0:nq], True, False, ['ones_f', 'sq'], [PSR(6)])
                                MM(P, ps[:, 6, 0:nq], ones_f[:], sq[:, 1, 0:nq], False, True, ['ones_f', 'sq'], [PSR(6)])
                                TS(P, 'dve', rsd[:, 0:nq], ps[:, 6, 0:nq], 1.0 / 256, EPS, ALU.mult, ALU.add, [PSR(6)], ['rsd'])
                                ACTV(P, rsd[:, 0:nq], rsd[:, 0:nq], AF.Ln, ['rsd'], ['rsd'])
                                RSQ(P, rsd[:, 0:nq], ['rsd'], ['rsd'])
                                y_ = ybf[yi % 2]
                                yn = ('ybf', yi % 2)
                                yi += 1
                                for cc in range(2):
                                    TT(P, 'dve', sq[:, cc, 0:nq], att[:, cc, 0:nq], rsd[:, 0:nq], ALU.mult, ['att', 'rsd', 'sq'], ['sq'])
                                    TS(P, 'dve', y_[:, cc, 0:nq], sq[:, cc, 0:nq], subg_c[:, l, cc:cc + 1], None, ALU.mult, None, ['sq', 'subg'], [yn])
                                ch = CG + 2 * h
                                for c2 in range(2):
                                    DMA(P, 'sp', mix_scr.ap()[(ch + c2) * 128:(ch + c2 + 1) * 128, orow:orow + nq], y_[:, c2, 0:nq], [yn], [('mix', l)], store=True)
                P.barrier()
                cut('attn')

                wout = wview('out', l, D)
                wgt = wview('gate', l, c.FF)
                wup = wview('up', l, c.FF)
                wdn = wview('down', l, D)
                for (row0, n, j, b, is_ctx, tok0) in blocks:
                    if is_ctx and last:
                        continue
                    NTL = n // 128
                    with ExitStack() as s1:
                        mixT = sb("o_mixT", [128, KC, TBM], BF16, s1)
                        wo = [sb("o_w%d" % i, [128, KC, 256], BF16, s1) for i in range(2)]
                        hbk = sb("o_hb", [128, TBM // 128, D], F32, s1)
                        gbc = sb("o_gbc", [128, D], F32, s1)
                        dtmp = sb("o_dtmp", [128, 128], F32, s1)
                        tmp = [sb("o_tmp%d" % i, [128, 256], F32, s1) for i in range(2)]
                        bcast_cols(mvec(l, j, 2), gbc, ['modT'], ['gbc'], dtmp, 'dtmp')
                        DMA(P, 'sp', mixT[:, :, 0:n], mix_scr.ap()[:, row0:row0 + n].rearrange("(k p) t -> p k t", p=128), [('mix', l)], ['mixT'])
                        for t in range(NTL):
                            DMA(P, 'sp', hbk[:, t, :], hsrc(l, row0 + t * 128, 128), [('h', l)], [('hbk', t)])
                        ti = 0
                        for pn in range(D // 256):
                            w = wo[pn % 2]
                            wn = ('wo', pn % 2)
                            DMA(P, 'sp', w[:], wout[:, pn * 256:(pn + 1) * 256].rearrange("(k p) x -> p k x", p=128), [('wg', 'out', l)], [wn])
                            for t in range(NTL):
                                bk = t % 4
                                for k in range(KC):
                                    MM(P, ps[:, bk, 0:256], mixT[:, k, t * 128:(t + 1) * 128], w[:, k, :], k == 0, k == KC - 1,
                                       ['mixT', wn], [PSR(bk)])
                                tm = tmp[ti % 2]
                                tmn = ('otmp', ti % 2)
                                ti += 1
                                TT(P, 'dve', tm[:], ps[:, bk, 0:256], gbc[:, pn * 256:(pn + 1) * 256], ALU.mult, [PSR(bk), 'gbc'], [tmn])
                                TT(P, 'dve', hbk[:, t, pn * 256:(pn + 1) * 256], hbk[:, t, pn * 256:(pn + 1) * 256], tm[:], ALU.add,
                                   [('hbk', t), tmn], [('hbk', t)])
                        for t in range(NTL):
                            DMA(P, 'sp', h1_scr.ap()[row0 + t * 128:row0 + (t + 1) * 128, :], hbk[:, t, :], [('hbk', t)], [('h1', l)], store=True)
                    P.barrier()
                    cut('p5')

                    with ExitStack() as sB:
                        fT = sb("m_fT", [128, KC, TBM], BF16, sB)
                        gTb = sb("m_gT", [32, TBM], F32, sB)
                        acc = sb("m_acc", [128, TBM // 128, D], F32, sB)
                        with ExitStack() as s1:
                            hb = sb("r_hb", [128, D], F32, s1)
                            junk = sb("r_junk", [128, D], BF16, s1)
                            dst_junk[0], dst_junk[1] = junk, 'junk'
                            ss = sb("r_ss", [128, 1], F32, s1)
                            rt = sb("r_rt", [128, 1], F32, s1)
                            f32T = sb("r_f32T", [128, KC, 128], F32, s1)
                            wr_t = sb("r_wr", [128, KC, 36], F32, s1)
                            lg = sb("r_lg", [128, 36], F32, s1)
                            sm = sb("r_sm", [128, 64], F32, s1)
                            oh = sb("r_oh", [128, 4], F32, s1)
                            lsel = sb("r_lsel", [128, 8], F32, s1)
                            l2 = sb("r_l2", [128, 8], F32, s1)
                            mk = sb("r_mk", [128, 16], F32, s1)
                            wi_ = sb("r_wi", [128, 8], F32, s1)
                            gts = sb("r_gts", [128, 32], F32, s1)
                            DMA(P, 'sp', wr_t[:, :, 0:4], w_rg.ap()[l].rearrange("(k p) x -> p k x", p=128), (), ['wr_t'])
                            DMA(P, 'sp', wr_t[:, :, 4:36], w_re.ap()[l].rearrange("(k p) x -> p k x", p=128), (), ['wr_t'])
                            av, shv = a2[:, l * 3 + j, :], mvec(l, j, 3)
                            for t in range(NTL):
                                def wrt(k, pss, psr, t=t, av=av, shv=shv):
                                    if False:
                                        ACTV(P, f32T[:, k, :], pss, AF.Identity, [psr, 'a2', 'modT'], [('f32T', k)],
                                             bias=shv[:, k:k + 1], scale=av[:, k:k + 1])
                                    else:
                                        TS(P, 'dve', f32T[:, k, :], pss, av[:, k:k + 1], shv[:, k:k + 1], ALU.mult, ALU.add,
                                           [psr, 'a2', 'modT'], [('f32T', k)])
                                    COPY(P, 'dve', fT[:, k, t * 128:(t + 1) * 128], f32T[:, k, :], [('f32T', k)], ['fT'])
                                norm_tile(l, j, h1_scr.ap()[row0 + t * 128:row0 + (t + 1) * 128, :], hb, 'hb', av, shv, (ss, rt), wrt,
                                          [('h1', l)])
                                for k in range(KC):
                                    MM(P, ps[:, 6, 0:36], f32T[:, k, :], wr_t[:, k, :], k == 0, False, [('f32T', k), 'wr_t'], [PSR(6)])
                                MM(P, ps[:, 6, 0:36], ones_f[0:1, :], brow[0:1, l, :], False, True, ['ones_f', 'brow'], [PSR(6)])
                                COPY(P, 'dve', lg[:], ps[:, 6, 0:36], [PSR(6)], ['lg'])
                                R = ['lg', 'sm', 'oh', 'lsel', 'l2', 'mk', 'wi', 'gts']
                                RMAX(P, sm[:, 0:1], lg[:, 0:4], R, R)
                                TS(P, 'dve', oh[:], lg[:, 0:4], sm[:, 0:1], None, ALU.is_equal, None, R, R)
                                TS(P, 'dve', sm[:, 1:2], sm[:, 0:1], -1.0, None, ALU.mult, None, R, R)
                                TS(P, 'dve', sm[:, 4:8], lg[:, 0:4], sm[:, 0:1], None, ALU.subtract, None, R, R)
                                ACTV(P, sm[:, 4:8], sm[:, 4:8], AF.Exp, R, R, accum=sm[:, 2:3])
                                RECIP(P, sm[:, 3:4], sm[:, 2:3], R, R)
                                TS(P, 'dve', lsel[:], lg[:, 4:12], oh[:, 0:1], None, ALU.mult, None, R, R)
                                for g in range(1, 4):
                                    STT(P, lsel[:], lg[:, 4 + 8 * g:12 + 8 * g], oh[:, g:g + 1], lsel[:], ALU.mult, ALU.add, R, R)
                                RMAX(P, sm[:, 8:9], lsel[:], R, R)
                                TS(P, 'dve', mk[:, 0:8], lsel[:], sm[:, 8:9], None, ALU.is_equal, None, R, R)
                                STT(P, l2[:], mk[:, 0:8], -1e30, lsel[:], ALU.mult, ALU.add, R, R)
                                RMAX(P, sm[:, 9:10], l2[:], R, R)
                                TS(P, 'dve', mk[:, 8:16], l2[:], sm[:, 9:10], None, ALU.is_equal, None, R, R)
                                TT(P, 'dve', sm[:, 10:11], sm[:, 9:10], sm[:, 8:9], ALU.subtract, R, R)
                                ACTV(P, sm[:, 11:12], sm[:, 10:11], AF.Exp, R, R)
                                TS(P, 'dve', sm[:, 12:13], sm[:, 11:12], 1.0, None, ALU.add, None, R, R)
                                RECIP(P, sm[:, 12:13], sm[:, 12:13], R, R)
                                TT(P, 'dve', sm[:, 13:14], sm[:, 12:13], sm[:, 3:4], ALU.mult, R, R)
                                TT(P, 'dve', sm[:, 14:15], sm[:, 13:14], sm[:, 11:12], ALU.mult, R, R)
                                TS(P, 'dve', wi_[:], mk[:, 0:8], sm[:, 13:14], None, ALU.mult, None, R, R)
                                STT(P, wi_[:], mk[:, 8:16], sm[:, 14:15], wi_[:], ALU.mult, ALU.add, R, R)
                                for g in range(4):
                                    TS(P, 'dve', gts[:, 8 * g:8 * g + 8], wi_[:], oh[:, g:g + 1], None, ALU.mult, None, R, R)
                                TR(P, ps[0:32, 7, 0:128], gts[:], ident[:], R + ['ident'], [PSR(7)])
                                ACTV(P, gTb[0:32, t * 128:(t + 1) * 128], ps[0:32, 7, 0:128], AF.Copy, [PSR(7)], ['gTb'])
                        P.barrier()
                        cut('p6')

                        with ExitStack() as s1:
                            NHALF = max(1, FC // 2)
                            FH = FC // NHALF
                            WH = FH * 128
                            ring = [sb("e_ring%d" % i, [128, 2048 * 4], BF16, s1) for i in range(3)]
                            hid = sb("e_hid", [128, FC, TBM], BF16, s1)
                            gbcs = [sb("e_gbc%d" % i, [128, TBM], F32, s1) for i in range(2)]
                            sg = [sb("e_sg%d" % i, [128, TBM], F32, s1) for i in range(2)]
                            tu = [sb("e_tu%d" % i, [128, TBM], F32, s1) for i in range(2)]
                            sel_e = [sb("e_sel%d" % i, [32, 128], F32, s1) for i in range(2)]
                            ri = 0
                            ei = 0
                            for e_ in range(NE):
                                se = sel_e[e_ % 2]
                                sen = ('sel_e', e_ % 2)
                                gb_ = gbcs[e_ % 2]
                                gbn = ('gbcs', e_ % 2)
                                TS(P, 'dve', se[:], ones_f[0:32, :], ident[0:32, e_:e_ + 1], None, ALU.mult, None, ['ones_f', 'ident'], [sen])
                                MM(P, ps[:, 6, 0:n], se[:], gTb[0:32, 0:n], True, True, [sen, 'gTb'], [PSR(6)])
                                ACTV(P, gb_[:, 0:n], ps[:, 6, 0:n], AF.Copy, [PSR(6)], [gbn])
                                for hf in range(NHALF):
                                    wts = []
                                    for (wv, nm) in ((wgt, 'gate'), (wup, 'up')):
                                        rg_ = ring[ri % 3]
                                        rn = ('ring', ri % 3)
                                        ri += 1
                                        wt = rg_[:, 0:KC * WH].rearrange("p (k x) -> p k x", k=KC)
                                        DMA(P, 'sp', wt, wv[e_ * D:(e_ + 1) * D, hf * WH:(hf + 1) * WH].rearrange("(k p) x -> p k x", p=128),
                                            [('wg', nm, l)], [rn])
                                        wts.append((wt, rn))
                                    for fi in range(FH):
                                        for k in range(KC):
                                            MM(P, ps[:, fi, 0:n], wts[0][0][:, k, fi * 128:(fi + 1) * 128], fT[:, k, 0:n], k == 0, k == KC - 1,
                                               [wts[0][1], 'fT'], [PSR(fi)])
                                    for fi in range(FH):
                                        for k in range(KC):
                                            MM(P, ps[:, 2 + fi, 0:n], wts[1][0][:, k, fi * 128:(fi + 1) * 128], fT[:, k, 0:n], k == 0, k == KC - 1,
                                               [wts[1][1], 'fT'], [PSR(2 + fi)])
                                    for fi in range(FH):
                                        s_ = sg[ei % 2]
                                        sn = ('sg', ei % 2)
                                        u_ = tu[ei % 2]
                                        un = ('tu', ei % 2)
                                        ei += 1
                                        ACTV(P, s_[:, 0:n], ps[:, fi, 0:n], AF.Silu, [PSR(fi)], [sn])
                                        TT(P, 'dve', u_[:, 0:n], ps[:, 2 + fi, 0:n], gb_[:, 0:n], ALU.mult, [PSR(2 + fi), gbn], [un])
                                        TT(P, 'dve', hid[:, hf * FH + fi, 0:n], s_[:, 0:n], u_[:, 0:n], ALU.mult, [sn, un], ['hid'])
                                DCH = 512
                                for d0 in range(0, D, 2048):
                                    dw = min(2048, D - d0)
                                    rg_ = ring[ri % 3]
                                    rn = ('ring', ri % 3)
                                    ri += 1
                                    wd = rg_[:, 0:FC * dw].rearrange("p (f x) -> p f x", f=FC)
                                    DMA(P, 'sp', wd, wdn[e_ * c.FF:(e_ + 1) * c.FF, d0:d0 + dw].rearrange("(f p) x -> p f x", p=128),
                                        [('wg', 'down', l)], [rn])
                                    oi = 0
                                    for t in range(NTL):
                                        for dc in range(0, dw, DCH):
                                            bk = 4 + oi % 2
                                            oi += 1
                                            for f in range(FC):
                                                MM(P, ps[:, bk, 0:DCH], hid[:, f, t * 128:(t + 1) * 128], wd[:, f, dc:dc + DCH], f == 0, f == FC - 1,
                                                   ['hid', rn], [PSR(bk)])
                                            a_ = acc[:, t, d0 + dc:d0 + dc + DCH]
                                            if e_ == 0:
                                                COPY(P, 'dve', a_, ps[:, bk, 0:DCH], [PSR(bk)], [('acc', t)])
                                            else:
                                                TT(P, 'dve', a_, ps[:, bk, 0:DCH], a_, ALU.add, [PSR(bk), ('acc', t)], [('acc', t)])
                        P.barrier()
                        cut('moe')

                        with ExitStack() as s1:
                            hb2 = [sb("f_hb%d" % i, [128, D], F32, s1) for i in range(2)]
                            gbc = sb("f_gbc", [128, D], F32, s1)
                            gfb = sb("f_gfb", [128, D], F32, s1)
                            dtmp = sb("f_dtmp", [128, 128], F32, s1)
                            junk = sb("f_junk", [128, D], BF16, s1)
                            ss = sb("f_ss", [128, 1], F32, s1)
                            rt = sb("f_rt", [128, 1], F32, s1)
                            bcast_cols(mvec(l, j, 5), gbc, ['modT'], ['gbc'], dtmp, 'dtmp')
                            if last:
                                bcast_cols(gfin_c, gfb, ['gfin'], ['gfb'], dtmp, 'dtmp')
                            for t in range(NTL):
                                hb = hb2[t % 2]
                                hbn = ('hb2', t % 2)
                                DMA(P, 'sp', hb[:], h1_scr.ap()[row0 + t * 128:row0 + (t + 1) * 128, :], [('h1', l)], [hbn])
                                TT(P, 'dve', acc[:, t, :], acc[:, t, :], gbc[:], ALU.mult, [('acc', t), 'gbc'], [('acc', t)])
                                TT(P, 'dve', hb[:], hb[:], acc[:, t, :], ALU.add, [hbn, ('acc', t)], [hbn])
                                if not last:
                                    DMA(P, 'sp', h_scr.ap()[row0 + t * 128:row0 + (t + 1) * 128, :], hb[:], [hbn], [('h', l + 1)], store=True)
                                else:
                                    ACTV(P, junk[:], hb[:], AF.Square, [hbn], ['junk', 'ss'], accum=ss[:])
                                    TS(P, 'dve', rt[:], ss[:], 1.0 / D, EPS, ALU.mult, ALU.add, ['ss'], ['rt'])
                                    ACTV(P, rt[:], rt[:], AF.Ln, ['rt'], ['rt'])
                                    RSQ(P, rt[:], ['rt'], ['rt'])
                                    STT(P, hb[:], hb[:], rt[:, 0:1], gfb[:], ALU.mult, ALU.mult, [hbn, 'rt', 'gfb'], [hbn])
                                    DMA(P, 'sp', out_sh.ap()[row0 + t * 128:row0 + (t + 1) * 128, :], hb[:], [hbn], ['out'], store=True)
                        P.barrier()
                        cut('p7')
                        cut('blk%d' % blocks.index((row0, n, j, b, is_ctx, tok0)))
                        if is_ctx and b == 1:
                            cut('l%d' % l)

        except StopBuild:
            pass
        P.dead = False
        P.barrier(full=True)
        P.emit(nc, es)
        nc_ctx.__exit__(None, None, None)
    return nc, P


def host_inputs(c, inp):
    L, D, NTh, CTX = c.L, c.D, c.NTh, c.CTX
    f = lambda a: np.ascontiguousarray(np.asarray(a, dtype=np.float32))
    x = f(inp['x'])
    ctx = f(inp['ctx']).reshape(2 * CTX, D)
    cvec = np.concatenate([f(inp['c']), f(inp['c_ctx'])[None, :]], axis=0)
    ident = np.eye(128, dtype=np.float32)
    pm = np.zeros((128, 128), np.float32)
    for i in range(32):
        pm[32 + i, i] = -1.0
        pm[i, 32 + i] = 1.0
        pm[96 + i, 64 + i] = -1.0
        pm[64 + i, 96 + i] = 1.0
    w_gate = f(inp['w_gate']).reshape(L, 32, D, c.FF)
    w_up = f(inp['w_up']).reshape(L, 32, D, c.FF)
    w_down = f(inp['w_down']).reshape(L, 32, c.FF, D)
    w_in, w_out, w_ada, b_ada = f(inp['w_in']), f(inp['w_out']), f(inp['w_ada']), f(inp['b_ada'])
    axis_dim = 64
    inv = (10000.0 ** (-np.arange(0, axis_dim, 2, dtype=np.float32) / axis_dim)).astype(np.float32)
    maps = []
    for i in range(8):
        pos = np.arange(i * NTh, (i + 1) * NTh)
        row = (pos // c.GRID_W).astype(np.float32)
        col = (pos % c.GRID_W).astype(np.float32)
        ang_r = row[:, None] * inv[None, :]
        ang_c = col[:, None] * inv[None, :]
        ang = np.concatenate([ang_r, ang_r, ang_c, ang_c], axis=-1).astype(np.float32)
        sel = np.zeros((128, 16), np.float32)
        if i > 0:
            sel[:, i - 1] = 1.0
        if i < 7:
            sel[:, 8 + i + 1] = 1.0
        m = {
            'x_sh': np.ascontiguousarray(x[:, i * NTh:(i + 1) * NTh, :].reshape(2 * NTh, D)),
            'ctx_in': ctx, 'cvec': cvec, 'selLR': sel,
            'w_ada_sh': np.ascontiguousarray(w_ada[:, :, i * c.NS:(i + 1) * c.NS]),
            'b_ada_sh': np.ascontiguousarray(b_ada[:, i * c.NS:(i + 1) * c.NS]),
            'g_mix': f(inp['g_mix']), 'g_ffn': f(inp['g_ffn']), 'g_final': f(inp['g_final']),
            'w_in_sh': np.ascontiguousarray(w_in[:, i * (D // 8):(i + 1) * (D // 8), :]),
            'w_out_sh': np.ascontiguousarray(w_out[:, i * (D // 8):(i + 1) * (D // 8), :]),
            'w_gate_sh': np.ascontiguousarray(w_gate[:, 4 * i:4 * i + 4].reshape(L, 4 * D, c.FF)),
            'w_up_sh': np.ascontiguousarray(w_up[:, 4 * i:4 * i + 4].reshape(L, 4 * D, c.FF)),
            'w_down_sh': np.ascontiguousarray(w_down[:, 4 * i:4 * i + 4].reshape(L, 4 * c.FF, D)),
            'sc_w': f(inp['short_conv_w']), 'cf_w': f(inp['cfm_conv_w']), 'cf_b': f(inp['cfm_conv_b']),
            'ln_g': f(inp['cfm_ln_g']), 'ln_b': f(inp['cfm_ln_b']), 'lam_qk': f(inp['lam_qk']),
            'subln_g': f(inp['subln_g']), 'w_rg': f(inp['w_route_group']), 'b_rg': f(inp['b_route_group']),
            'w_re': f(inp['w_route_expert']), 'b_re': f(inp['b_route_expert']),
            'cosT': np.ascontiguousarray(np.cos(ang).T.astype(np.float32)),
            'sinT': np.ascontiguousarray(np.sin(ang).T.astype(np.float32)),
            'ident': ident, 'pm': pm,
        }
        maps.append(m)
    return maps


def run_cfg(c, inp, trace=False):
    nc, P = build(c)
    maps = host_inputs(c, inp)
    res = run_bass_kernel_spmd(nc, maps, core_ids=list(range(8)), trace=trace)
    out = np.empty((2, c.SEQ, c.D), np.float32)
    for i in range(8):
        o = res.results[i]['out_sh'].reshape(2, c.NTh, c.D)
        out[:, i * c.NTh:(i + 1) * c.NTh, :] = o
    return out, res


def kernel(**inputs):
    c = Cfg(4096, 8192)
    out, _ = run_cfg(c, inputs)
    return out
```
